# Optimizing a Trainium2 kernel written in Bass

```python
import math
import jax, jax.numpy as jnp
from jax import lax
import numpy as np

D_MODEL = 1024
BATCH = 4
SEQ = 8192
DEPTH = 4

CTX_LEN = 256
GRID_W = 64
N_MIXERS = 3
N_LAYERS_CONV = (DEPTH + 2) // 3
N_LAYERS_S5 = (DEPTH + 1) // 3
N_LAYERS_LRU = DEPTH // 3
CONV_WIDTH = 31
S5_GROUP = 16
S5_GROUPS = D_MODEL // S5_GROUP
S5_STATE = 64
SCAN_CHUNK = 128
LRU_WIDTH = D_MODEL
LRU_HEADS = 8
LRU_BLOCK = LRU_WIDTH // LRU_HEADS
LRU_CONV = 4
LRU_C = 8.0
N_EXPERTS = 16
EC_CAPACITY = 2
D_EXPERT = D_MODEL
EPS = 1e-6
POS_BASE = 10000.0

kernel_name = "hybrid_conv_s5_rglru_ecmoe_diffusion_trunk"

F32 = jnp.float32


def rmsnorm(x, g):
    xf = x.astype(F32)
    y = xf * lax.rsqrt(jnp.mean(xf * xf, axis=-1, keepdims=True) + EPS)
    return (y * g).astype(x.dtype)


def layernorm(x, g, b):
    xf = x.astype(F32)
    mu = jnp.mean(xf, axis=-1, keepdims=True)
    xc = xf - mu
    y = xc * lax.rsqrt(jnp.mean(xc * xc, axis=-1, keepdims=True) + EPS)
    return (y * g + b).astype(x.dtype)


def adaln(cond, w, b):
    return jnp.split(jax.nn.silu(cond) @ w + b, 6, axis=-1)


def depthwise_conv(x, w, pad):
    return lax.conv_general_dilated(
        x, w[:, None, :].astype(x.dtype), window_strides=(1,), padding=[pad],
        dimension_numbers=("NWC", "WIO", "NWC"), feature_group_count=x.shape[-1])


def grid_pos_embed(n, d):
    rows = n // GRID_W
    quarter = d // 4
    omega = 1.0 / (POS_BASE ** (jnp.arange(quarter, dtype=F32) / quarter))
    r = jnp.arange(rows, dtype=F32)[:, None] * omega
    cc = jnp.arange(GRID_W, dtype=F32)[:, None] * omega
    row_emb = jnp.concatenate([jnp.sin(r), jnp.cos(r)], axis=-1)
    col_emb = jnp.concatenate([jnp.sin(cc), jnp.cos(cc)], axis=-1)
    emb = jnp.concatenate([
        jnp.broadcast_to(row_emb[:, None, :], (rows, GRID_W, d // 2)),
        jnp.broadcast_to(col_emb[None, :, :], (rows, GRID_W, d // 2))], axis=-1)
    return emb.reshape(rows * GRID_W, d)


def conformer_conv(h, w_in, b_in, dw, dw_b, ln_g, ln_b, w_out, b_out):
    u = h @ w_in + b_in
    a, g = jnp.split(u, 2, axis=-1)
    u = a * jax.nn.sigmoid(g)
    u = depthwise_conv(u, dw, (CONV_WIDTH // 2, CONV_WIDTH // 2)) + dw_b
    u = layernorm(u, ln_g, ln_b)
    return jax.nn.silu(u) @ w_out + b_out


def s5_discretize(lam_re, lam_im, log_dt, b_re, b_im):
    lam_re = lam_re.astype(F32); lam_im = lam_im.astype(F32)
    dt = jnp.exp(log_dt.astype(F32))[:, None]
    mag = jnp.exp(lam_re * dt)
    ang = lam_im * dt
    lb_re = mag * jnp.cos(ang)
    lb_im = mag * jnp.sin(ang)
    nr = lb_re - 1.0
    ni = lb_im
    den = lam_re * lam_re + lam_im * lam_im
    coef_re = ((nr * lam_re + ni * lam_im) / den)[:, :, None]
    coef_im = ((ni * lam_re - nr * lam_im) / den)[:, :, None]
    b_re = b_re.astype(F32); b_im = b_im.astype(F32)
    bb_re = coef_re * b_re - coef_im * b_im
    bb_im = coef_re * b_im + coef_im * b_re
    return lb_re, lb_im, bb_re, bb_im


def s5_combine(e1, e2):
    a1r, a1i, b1r, b1i = e1
    a2r, a2i, b2r, b2i = e2
    return (a1r * a2r - a1i * a2i,
            a1r * a2i + a1i * a2r,
            a2r * b1r - a2i * b1i + b2r,
            a2r * b1i + a2i * b1r + b2i)


def s5_scan(u, lb_re, lb_im, bb_re, bb_im, c_re, c_im, h0, want_y):
    bsz, n, d = u.shape
    nc = n // SCAN_CHUNK
    uc = u.reshape(bsz, nc, SCAN_CHUNK, S5_GROUPS, S5_GROUP).transpose(1, 0, 2, 3, 4)

    def step(h, uk):
        hr0, hi0 = h
        bur = jnp.einsum("bsgk,gpk->bsgp", uk, bb_re)
        bui = jnp.einsum("bsgk,gpk->bsgp", uk, bb_im)
        ar = jnp.broadcast_to(lb_re, bur.shape)
        ai = jnp.broadcast_to(lb_im, bur.shape)
        cr, ci, sr, si = lax.associative_scan(s5_combine, (ar, ai, bur, bui), axis=1)
        hr = sr + cr * hr0[:, None] - ci * hi0[:, None]
        hi = si + cr * hi0[:, None] + ci * hr0[:, None]
        carry = (hr[:, -1], hi[:, -1])
        if want_y:
            y = (jnp.einsum("bsgp,gkp->bsgk", hr, c_re)
                 - jnp.einsum("bsgp,gkp->bsgk", hi, c_im))
            return carry, y
        return carry, None

    h_last, ys = lax.scan(step, h0, uc)
    y = ys.transpose(1, 0, 2, 3, 4).reshape(bsz, n, d) if want_y else None
    return y, h_last


def s5_glu(y, w, b, dtype):
    z = jax.nn.gelu(y).astype(dtype) @ w + b
    a, g = jnp.split(z, 2, axis=-1)
    return a * jax.nn.sigmoid(g)


def s5_mixer(h_lat, h_ctx, lam_re, lam_im, log_dt, b_re, b_im, c_re, c_im,
             d_skip, w_glu, b_glu, ctx_out):
    dtype = h_lat.dtype
    ul = h_lat.astype(F32)
    uc = h_ctx.astype(F32)
    dsk = d_skip.astype(F32)
    y_lat = dsk * ul
    y_ctx = dsk * uc if ctx_out else None
    bsz = h_ctx.shape[0]
    h0 = (jnp.zeros((bsz, S5_GROUPS, S5_STATE), F32),
          jnp.zeros((bsz, S5_GROUPS, S5_STATE), F32))
    for direction in range(2):
        lb_re, lb_im, bb_re, bb_im = s5_discretize(
            lam_re[direction], lam_im[direction], log_dt[direction],
            b_re[direction], b_im[direction])
        cre = c_re[direction].astype(F32)
        cim = c_im[direction].astype(F32)
        rev = direction == 1
        ucd = uc[:, ::-1] if rev else uc
        uld = ul[:, ::-1] if rev else ul
        yc, hc = s5_scan(ucd, lb_re, lb_im, bb_re, bb_im, cre, cim, h0, ctx_out)
        yl, _ = s5_scan(uld, lb_re, lb_im, bb_re, bb_im, cre, cim, hc, True)
        y_lat = y_lat + (yl[:, ::-1] if rev else yl)
        if ctx_out:
            y_ctx = y_ctx + (yc[:, ::-1] if rev else yc)
    out_lat = s5_glu(y_lat, w_glu, b_glu, dtype)
    out_ctx = s5_glu(y_ctx, w_glu, b_glu, dtype) if ctx_out else None
    return out_lat, out_ctx


def lru_gates(xb, w_a, b_a, w_i, b_i, lam):
    bsz, n, w = xb.shape
    xh = xb.reshape(bsz, n, LRU_HEADS, LRU_BLOCK)
    r = jax.nn.sigmoid((jnp.einsum("bshi,hij->bshj", xh, w_a).reshape(bsz, n, w) + b_a).astype(F32))
    ig = jax.nn.sigmoid((jnp.einsum("bshi,hij->bshj", xh, w_i).reshape(bsz, n, w) + b_i).astype(F32))
    log_a = -LRU_C * r * jax.nn.softplus(-lam.astype(F32))
    a = jnp.exp(log_a)
    b = jnp.sqrt(-jnp.expm1(2.0 * log_a)) * (ig * xb.astype(F32))
    return a, b


def linear_scan(a, b, h0):
    ca, cb = lax.associative_scan(
        lambda e1, e2: (e1[0] * e2[0], e2[0] * e1[1] + e2[1]), (a, b), axis=1)
    return cb + ca * h0[:, None]


def lru_mixer(h_lat, h_ctx, w_y, b_y, w_x, b_x, conv_w, conv_b, w_a, b_a, w_i, b_i,
              lam, w_out, b_out, ctx_out):
    dtype = h_lat.dtype
    pad = (LRU_CONV // 2, LRU_CONV - 1 - LRU_CONV // 2)

    def branch(h):
        return depthwise_conv(h @ w_x + b_x, conv_w, pad) + conv_b

    xl = branch(h_lat)
    xc = branch(h_ctx)
    bsz = h_ctx.shape[0]
    r_lat = jnp.zeros(xl.shape, F32)
    r_ctx = jnp.zeros(xc.shape, F32) if ctx_out else None
    for direction in range(2):
        al, bl = lru_gates(xl, w_a[direction], b_a[direction], w_i[direction], b_i[direction], lam[direction])
        ac, bc = lru_gates(xc, w_a[direction], b_a[direction], w_i[direction], b_i[direction], lam[direction])
        rev = direction == 1
        if rev:
            al, bl, ac, bc = al[:, ::-1], bl[:, ::-1], ac[:, ::-1], bc[:, ::-1]
        hc = linear_scan(ac, bc, jnp.zeros((bsz, LRU_WIDTH), F32))
        hl = linear_scan(al, bl, hc[:, -1])
        r_lat = r_lat + (hl[:, ::-1] if rev else hl)
        if ctx_out:
            r_ctx = r_ctx + (hc[:, ::-1] if rev else hc)

    def finish(h, r):
        gate = jax.nn.gelu((h @ w_y + b_y).astype(F32))
        return (r * gate).astype(dtype) @ w_out + b_out

    out_lat = finish(h_lat, r_lat)
    out_ctx = finish(h_ctx, r_ctx) if ctx_out else None
    return out_lat, out_ctx


def ec_moe(x, w_router, w1, w3, w2):
    bsz, n, d = x.shape
    cap = EC_CAPACITY * n // N_EXPERTS
    aff = jax.nn.softmax((x @ w_router).astype(F32), axis=-1)
    g, idx = lax.top_k(jnp.swapaxes(aff, 1, 2), cap)
    xs = jax.vmap(lambda xb, ib: xb[ib])(x, idx)
    h = jax.nn.silu(jnp.einsum("becd,edf->becf", xs, w1)) * jnp.einsum("becd,edf->becf", xs, w3)
    y = jnp.einsum("becf,efd->becd", h, w2) * g[..., None].astype(x.dtype)
    return jax.vmap(lambda yb, ib: jnp.zeros((n, d), y.dtype).at[ib.reshape(-1)].add(yb.reshape(-1, d)))(y, idx)


def setup_inputs(seed: int = 0) -> dict:
    key = jax.random.key(seed)
    split = jax.random.split(key, 64)
    keys = iter([split[i] for i in range(64)])

    def nrm(shape, scale):
        return scale * jax.random.normal(next(keys), shape, F32)

    D = D_MODEL
    NA, NB, NC = N_LAYERS_CONV, N_LAYERS_S5, N_LAYERS_LRU
    G, P, K = S5_GROUPS, S5_STATE, S5_GROUP
    W, H, BL = LRU_WIDTH, LRU_HEADS, LRU_BLOCK
    E, DE = N_EXPERTS, D_EXPERT
    inv_sqrt2 = 1.0 / math.sqrt(2.0)

    lam_im0 = jnp.pi * jnp.arange(P, dtype=F32)
    u_a = jax.random.uniform(next(keys), (NC, 2, W), F32, minval=0.9, maxval=0.999)
    s_a = u_a ** (1.0 / LRU_C)

    return {
        "x": nrm((BATCH, SEQ, D), 1.0),
        "c": nrm((BATCH, D), 1.0),
        "ctx": nrm((BATCH, CTX_LEN, D), 1.0),
        "c_ctx": nrm((D,), 1.0),
        "w_mod": nrm((DEPTH, D, 6 * D), 0.5 * D ** -0.5),
        "b_mod": nrm((DEPTH, 6 * D), 0.02),
        "g_mix": 1.0 + nrm((DEPTH, D), 0.02),
        "g_ffn": 1.0 + nrm((DEPTH, D), 0.02),
        "conv_w_in": nrm((NA, D, 2 * D), D ** -0.5),
        "conv_b_in": nrm((NA, 2 * D), 0.02),
        "conv_dw": nrm((NA, CONV_WIDTH, D), CONV_WIDTH ** -0.5),
        "conv_dw_b": nrm((NA, D), 0.02),
        "conv_ln_g": 1.0 + nrm((NA, D), 0.02),
        "conv_ln_b": nrm((NA, D), 0.02),
        "conv_w_out": nrm((NA, D, D), D ** -0.5),
        "conv_b_out": nrm((NA, D), 0.02),
        "s5_lam_re": -0.5 + nrm((NB, 2, G, P), 0.01),
        "s5_lam_im": lam_im0 + nrm((NB, 2, G, P), 0.01),
        "s5_log_dt": jax.random.uniform(next(keys), (NB, 2, G), F32,
                                        minval=math.log(1e-3), maxval=math.log(1e-1)),
        "s5_b_re": nrm((NB, 2, G, P, K), inv_sqrt2 * K ** -0.5),
        "s5_b_im": nrm((NB, 2, G, P, K), inv_sqrt2 * K ** -0.5),
        "s5_c_re": nrm((NB, 2, G, K, P), inv_sqrt2 * P ** -0.5),
        "s5_c_im": nrm((NB, 2, G, K, P), inv_sqrt2 * P ** -0.5),
        "s5_d": nrm((NB, D), 1.0),
        "s5_w_glu": nrm((NB, D, 2 * D), D ** -0.5),
        "s5_b_glu": nrm((NB, 2 * D), 0.02),
        "lru_w_y": nrm((NC, D, W), D ** -0.5),
        "lru_b_y": nrm((NC, W), 0.02),
        "lru_w_x": nrm((NC, D, W), D ** -0.5),
        "lru_b_x": nrm((NC, W), 0.02),
        "lru_conv_w": nrm((NC, LRU_CONV, W), LRU_CONV ** -0.5),
        "lru_conv_b": nrm((NC, W), 0.02),
        "lru_w_a": nrm((NC, 2, H, BL, BL), BL ** -0.5),
        "lru_b_a": nrm((NC, 2, W), 0.02),
        "lru_w_i": nrm((NC, 2, H, BL, BL), BL ** -0.5),
        "lru_b_i": nrm((NC, 2, W), 0.02),
        "lru_lam": jnp.log(s_a) - jnp.log1p(-s_a),
        "lru_w_out": nrm((NC, W, D), W ** -0.5),
        "lru_b_out": nrm((NC, D), 0.02),
        "moe_router": nrm((DEPTH, D, E), D ** -0.5),
        "moe_w1": nrm((DEPTH, E, D, DE), D ** -0.5),
        "moe_w3": nrm((DEPTH, E, D, DE), D ** -0.5),
        "moe_w2": nrm((DEPTH, E, DE, D), DE ** -0.5),
        "final_g": 1.0 + nrm((D,), 0.02),
    }


def reference(x, c, ctx, c_ctx, w_mod, b_mod, g_mix, g_ffn,
              conv_w_in, conv_b_in, conv_dw, conv_dw_b, conv_ln_g, conv_ln_b, conv_w_out, conv_b_out,
              s5_lam_re, s5_lam_im, s5_log_dt, s5_b_re, s5_b_im, s5_c_re, s5_c_im, s5_d, s5_w_glu, s5_b_glu,
              lru_w_y, lru_b_y, lru_w_x, lru_b_x, lru_conv_w, lru_conv_b, lru_w_a, lru_b_a, lru_w_i, lru_b_i,
              lru_lam, lru_w_out, lru_b_out,
              moe_router, moe_w1, moe_w3, moe_w2, final_g):
    reader_layers = [i for i in range(DEPTH) if i % N_MIXERS != 0]
    last_reader = max(reader_layers) if reader_layers else -1

    x_lat = x + grid_pos_embed(x.shape[1], x.shape[2]).astype(x.dtype)
    x_ctx = ctx
    for i in range(DEPTH):
        kind = i % N_MIXERS
        j = i // N_MIXERS
        ctx_in = i <= last_reader
        ctx_out = i < last_reader

        sh1, sc1, g1, sh2, sc2, g2 = [t[:, None, :] for t in adaln(c, w_mod[i], b_mod[i])]
        h_lat = rmsnorm(x_lat, g_mix[i]) * (1.0 + sc1) + sh1
        h_ctx = None
        if ctx_in:
            csh1, csc1, cg1, csh2, csc2, cg2 = [t[None, None, :] for t in adaln(c_ctx, w_mod[i], b_mod[i])]
            h_ctx = rmsnorm(x_ctx, g_mix[i]) * (1.0 + csc1) + csh1

        if kind == 0:
            conv_args = (conv_w_in[j], conv_b_in[j], conv_dw[j], conv_dw_b[j],
                         conv_ln_g[j], conv_ln_b[j], conv_w_out[j], conv_b_out[j])
            y_lat = conformer_conv(h_lat, *conv_args)
            y_ctx = conformer_conv(h_ctx, *conv_args) if ctx_out else None
        elif kind == 1:
            y_lat, y_ctx = s5_mixer(h_lat, h_ctx, s5_lam_re[j], s5_lam_im[j], s5_log_dt[j],
                                    s5_b_re[j], s5_b_im[j], s5_c_re[j], s5_c_im[j],
                                    s5_d[j], s5_w_glu[j], s5_b_glu[j], ctx_out)
        else:
            y_lat, y_ctx = lru_mixer(h_lat, h_ctx, lru_w_y[j], lru_b_y[j], lru_w_x[j], lru_b_x[j],
                                     lru_conv_w[j], lru_conv_b[j], lru_w_a[j], lru_b_a[j],
                                     lru_w_i[j], lru_b_i[j], lru_lam[j], lru_w_out[j], lru_b_out[j],
                                     ctx_out)

        x_lat = x_lat + g1 * y_lat
        m_lat = rmsnorm(x_lat, g_ffn[i]) * (1.0 + sc2) + sh2
        x_lat = x_lat + g2 * ec_moe(m_lat, moe_router[i], moe_w1[i], moe_w3[i], moe_w2[i])
        if ctx_out:
            x_ctx = x_ctx + cg1 * y_ctx
            m_ctx = rmsnorm(x_ctx, g_ffn[i]) * (1.0 + csc2) + csh2
            x_ctx = x_ctx + cg2 * ec_moe(m_ctx, moe_router[i], moe_w1[i], moe_w3[i], moe_w2[i])
    return rmsnorm(x_lat, final_g)
```

```python
import math
import contextlib
import numpy as np
import concourse.bass as bass
import concourse.mybir as mybir
from concourse.bass_utils import run_bass_kernel_spmd

F32 = mybir.dt.float32
BF16 = mybir.dt.bfloat16
I32 = mybir.dt.int32
AF = mybir.ActivationFunctionType
ALU = mybir.AluOpType
AX = mybir.AxisListType

D = 1024
NE = 16
EPS = 1e-6
ENGS = ("pe", "act", "dve", "pool", "sp")
NDMASEM = 48
BIG = 1.0e6


class Prog:
    def __init__(self, nc):
        self.nc = nc
        self.streams = {e: [] for e in ENGS}
        self.cnt = {e: 0 for e in ENGS}
        self.known = {e: {} for e in ENGS}
        self.last_w = {}
        self.reads = {}
        self.dma_cnt = [0] * NDMASEM
        self.dma_rr = 0
        self.n_ops = 0

    def _deps(self, eng, reads, writes):
        need = {}

        def add(dep):
            if dep is None:
                return
            k, v = dep
            if need.get(k, 0) < v:
                need[k] = v

        for r in reads:
            add(self.last_w.get(r))
        for w in writes:
            add(self.last_w.get(w))
            for k, v in self.reads.get(w, {}).items():
                add((k, v))
        out = []
        kn = self.known[eng]
        for k, v in need.items():
            if k == "pe" and eng == "pe":
                continue
            if kn.get(k, 0) >= v:
                continue
            kn[k] = v
            out.append((k, v))
        return out

    def _mark(self, tag, reads, writes):
        for r in reads:
            d = self.reads.setdefault(r, {})
            if d.get(tag[0], 0) < tag[1]:
                d[tag[0]] = tag[1]
        for w in writes:
            self.last_w[w] = tag
            self.reads[w] = {}

    def op(self, eng, fn, reads=(), writes=()):
        waits = self._deps(eng, reads, writes)
        self.cnt[eng] += 1
        tag = (eng, self.cnt[eng])
        self.streams[eng].append((fn, waits, (eng, 1)))
        self._mark(tag, reads, writes)
        self.n_ops += 1

    def dma(self, eng, fn, reads=(), writes=()):
        j = self.dma_rr
        self.dma_rr = (self.dma_rr + 1) % NDMASEM
        k = ("dma", j)
        waits = self._deps(eng, reads, writes)
        prev = self.dma_cnt[j]
        if prev > 0 and self.known[eng].get(k, 0) < prev:
            self.known[eng][k] = prev
            waits.append((k, prev))
        self.dma_cnt[j] += 16
        tag = (k, self.dma_cnt[j])
        self.streams[eng].append((fn, waits, (k, 16)))
        self._mark(tag, reads, writes)
        self.n_ops += 1

    def barrier(self):
        for eng in ENGS:
            waits = []
            for e in ENGS:
                if self.cnt[e] > 0 and e != eng and self.known[eng].get(e, 0) < self.cnt[e]:
                    waits.append((e, self.cnt[e]))
                    self.known[eng][e] = self.cnt[e]
            for j in range(NDMASEM):
                k = ("dma", j)
                if self.dma_cnt[j] > 0 and self.known[eng].get(k, 0) < self.dma_cnt[j]:
                    waits.append((k, self.dma_cnt[j]))
                    self.known[eng][k] = self.dma_cnt[j]
            if waits:
                self.streams[eng].append((None, waits, None))
        self.last_w = {}
        self.reads = {}

    def build(self):
        nc = self.nc
        with contextlib.ExitStack() as st:
            sems = {}
            for e in ENGS:
                sems[e] = st.enter_context(nc.semaphore("s_" + e))
            for j in range(NDMASEM):
                sems[("dma", j)] = st.enter_context(nc.semaphore("s_dma%d" % j))
            block = st.enter_context(nc.Block())

            def runner(ename):
                def run(engine):
                    for fn, waits, inc in self.streams[ename]:
                        for k, v in waits:
                            engine.wait_ge(sems[k], v)
                        if fn is not None:
                            ins = fn(engine)
                            ins.then_inc(sems[inc[0]], inc[1])
                return run

            block.tensor(runner("pe"))
            block.scalar(runner("act"))
            block.vector(runner("dve"))
            block.gpsimd(runner("pool"))
            block.sync(runner("sp"))


class Arena:
    def __init__(self, ap, n):
        self.ap = ap
        self.n = n
        self.off = 0
        self.base = 0
        self.uid = 0

    def alloc(self, shape, dt=F32):
        ne = 1
        for s in shape[1:]:
            ne *= s
        words = ne if dt in (F32, I32) else (ne + 1) // 2
        a = self.ap[:, self.off:self.off + words]
        self.off += words
        assert self.off <= self.n, ("arena overflow", self.off, self.n)
        if dt != F32:
            a = a.bitcast(dt)
        if len(shape) == 3:
            a = a.rearrange("p (a b) -> p a b", a=shape[1])
        elif len(shape) == 4:
            a = a.rearrange("p (a b c) -> p a b c", a=shape[1], b=shape[2])
        return a

    def mark(self):
        self.base = self.off

    def reset(self):
        self.off = self.base


def build_program(T, TC, nlayers=4):
    NT = T // 128
    NTC = TC // 128
    CAP = 2 * T // NE
    CAPC = 2 * TC // NE
    nc = bass.Bass("TRN2", target_bir_lowering=False)

    def din(name, shape, dt=F32):
        return nc.dram_tensor(name, list(shape), dt, kind="ExternalInput").ap()

    def dscr(name, shape, dt=F32):
        return nc.dram_tensor(name, list(shape), dt, kind="Internal").ap()

    x_in = din("x", [T, D])
    ctx_in = din("ctx", [TC, D])
    c2T = din("c2T", [128, 8, 2])
    w_mod = din("w_mod", [4, D, 6 * D])
    b_mod = din("b_mod", [4, 6 * D])
    g_mix = din("g_mix", [4, D])
    g_ffn = din("g_ffn", [4, D])
    final_g = din("final_g", [D])
    moe_router = din("moe_router", [4, D, NE])
    moe_w1 = din("moe_w1", [4, NE, D, D])
    moe_w3 = din("moe_w3", [4, NE, D, D])
    moe_w2 = din("moe_w2", [4, NE, D, D])
    conv_w_in = din("conv_w_in", [2, D, 2 * D])
    conv_b_in_pp = din("conv_b_in_pp", [2, 128, 16])
    conv_dw_pp = din("conv_dw_pp", [2, 128, 8, 31])
    conv_dw_b_pp = din("conv_dw_b_pp", [2, 128, 8])
    conv_ln_g_pp = din("conv_ln_g_pp", [2, 128, 8])
    conv_ln_b_pp = din("conv_ln_b_pp", [2, 128, 8])
    conv_w_out = din("conv_w_out", [2, D, D])
    conv_b_out = din("conv_b_out", [2, D])
    s5_lam_re = din("s5_lam_re", [2, 128, 32])
    s5_lam_im = din("s5_lam_im", [2, 128, 32])
    s5_log_dt = din("s5_log_dt", [2, 128, 32])
    s5_bw_re = din("s5_bw_re", [2, 32, 128, 128])
    s5_bw_im = din("s5_bw_im", [2, 32, 128, 128])
    s5_cw_re = din("s5_cw_re", [2, 32, 128, 128])
    s5_cw_im = din("s5_cw_im", [2, 32, 128, 128])
    s5_d_pp = din("s5_d_pp", [128, 8])
    s5_bn_re = din("s5_bn_re", [2, 32, 128, 16])
    s5_bn_im = din("s5_bn_im", [2, 32, 128, 16])
    s5_cn_re = din("s5_cn_re", [2, 32, 128, 16])
    s5_cn_im = din("s5_cn_im", [2, 32, 128, 16])
    s5_d_rep = din("s5_d_rep", [128, 64])
    s5_w_glu = din("s5_w_glu", [D, 2 * D])
    s5_b_glu = din("s5_b_glu", [2 * D])
    lru_w_y = din("lru_w_y", [D, D])
    lru_b_y_pp = din("lru_b_y_pp", [128, 8])
    lru_w_x = din("lru_w_x", [D, D])
    lru_b_x_pp = din("lru_b_x_pp", [128, 8])
    lru_conv_w_pp = din("lru_conv_w_pp", [128, 8, 4])
    lru_conv_b_pp = din("lru_conv_b_pp", [128, 8])
    lru_w_a = din("lru_w_a", [2, 8, 128, 128])
    lru_b_a_pp = din("lru_b_a_pp", [2, 128, 8])
    lru_w_i = din("lru_w_i", [2, 8, 128, 128])
    lru_b_i_pp = din("lru_b_i_pp", [2, 128, 8])
    lru_lam_pp = din("lru_lam_pp", [2, 128, 8])
    lru_w_out = din("lru_w_out", [D, D])
    lru_b_out = din("lru_b_out", [D])
    out = nc.dram_tensor("out", [T, D], F32, kind="ExternalOutput").ap()

    XS = {"l": dscr("xl", [T, D]), "c": dscr("xc", [TC, D])}
    MB = {"l": dscr("mbl", [T, D], BF16), "c": dscr("mbc", [TC, D], BF16)}
    LST = {"l": [dscr("lstl%d" % e, [CAP, 2], I32) for e in range(NE)], "c": [dscr("lstc%d" % e, [CAPC, 2], I32) for e in range(NE)]}
    FA = {"l": dscr("fal", [D, T]), "c": dscr("fac", [D, TC])}
    FB = {"l": dscr("fbl", [D, T]), "c": dscr("fbc", [D, TC])}
    FA16 = {"l": dscr("fa16l", [D, T], BF16), "c": dscr("fa16c", [D, TC], BF16)}
    FC = {"l": dscr("fcl", [D, T]), "c": dscr("fcc", [D, TC])}
    modd = dscr("modd", [2, 6, D])
    TS = {"l": T, "c": TC}
    NTS = {"l": NT, "c": NTC}
    CAPS = {"l": CAP, "c": CAPC}

    st = contextlib.ExitStack()
    with st:
        NAR = 52600
        arena_t = st.enter_context(nc.sbuf_tensor("arena", [128, NAR], F32))[:, :]
        PS = [st.enter_context(nc.psum_tensor("ps%d" % i, [128, 512], F32))[:, :] for i in range(8)]
        A = Arena(arena_t, NAR)
        P = Prog(nc)

        def TT(eng, o, a, b, op, r, w):
            P.op(eng, lambda e: e.tensor_tensor(out=o, in0=a, in1=b, op=op), r, w)

        def TSC(eng, o, a, s1, op0, r, w, s2=None, op1=None):
            if op1 is None:
                P.op(eng, lambda e: e.tensor_scalar(out=o, in0=a, scalar1=s1, scalar2=None, op0=op0), r, w)
            else:
                P.op(eng, lambda e: e.tensor_scalar(out=o, in0=a, scalar1=s1, scalar2=s2, op0=op0, op1=op1), r, w)

        def STT(eng, o, a, s, b, op0, op1, r, w):
            P.op(eng, lambda e: e.scalar_tensor_tensor(out=o, in0=a, scalar=s, in1=b, op0=op0, op1=op1), r, w)

        def ACT(o, i, func, r, w, bias=None, scale=1.0, accum=None):
            kw = {}
            if bias is not None:
                kw["bias"] = bias
            if accum is not None:
                kw["accum_out"] = accum
            P.op("act", lambda e: e.activation(out=o, in_=i, func=func, scale=scale, **kw), r, w)

        def CPY(eng, o, i, r, w):
            if eng == "act":
                P.op("act", lambda e: e.activation(out=o, in_=i, func=AF.Copy), r, w)
            else:
                P.op(eng, lambda e: e.tensor_copy(out=o, in_=i), r, w)

        def MSET(eng, o, v, w):
            P.op(eng, lambda e: e.memset(o, v), (), w)

        def MM(o, l, rh, start, stop, r, w):
            P.op("pe", lambda e: e.matmul(o, lhsT=l, rhs=rh, start=start, stop=stop), r, w)

        def DMA(q, o, i, r, w, **kw):
            P.dma(q, lambda e: e.dma_start(out=o, in_=i, **kw), r, w)

        def RECIP(o, i, r, w):
            P.op("dve", lambda e: e.reciprocal(out=o, in_=i), r, w)

        psk = lambda k: ("ps", k)
        _regs = {}

        def getreg(e, v):
            if v not in _regs:
                _regs[v] = e.to_reg(v)
            return _regs[v]

        ident = A.alloc([128, 128])
        ones = A.alloc([128, 128])
        Umat = A.alloc([128, 128])
        tokid_std = A.alloc([128, max(NT, NTC)], I32)
        tokid_l5 = A.alloc([128, NT], I32)
        tokid_c5 = A.alloc([128, 8], I32)
        AFFC5 = A.alloc([128, NE, 8])
        rmA = A.alloc([128, 1]); rmB = A.alloc([128, 1])
        RCFG = {}
        AFF = {"l": A.alloc([128, NE, NT]), "c": A.alloc([128, NE, NTC])}
        wr_t = A.alloc([128, 8, NE])
        csil = A.alloc([128, 8, 2])
        identb = A.alloc([128, 128], BF16)
        A.mark()
        MSET("pool", ones, 1.0, ["ones"])
        MSET("pool", ident, 1.0, ["ident"])
        P.op("pool", lambda e: e.affine_select(out=ident, in_=ident, pattern=[[-1, 128]], compare_op=ALU.is_equal,
                                               fill=0.0, base=0, channel_multiplier=1), ["ident"], ["ident"])
        MSET("pool", Umat, 1.0, ["Umat"])
        P.op("pool", lambda e: e.affine_select(out=Umat, in_=Umat, pattern=[[1, 128]], compare_op=ALU.is_gt,
                                               fill=0.0, base=0, channel_multiplier=-1), ["Umat"], ["Umat"])
        P.op("pool", lambda e: e.iota(tokid_std, pattern=[[128, max(NT, NTC)]], base=0, channel_multiplier=1), (), ["tokid"])
        P.op("pool", lambda e: e.iota(tokid_l5, pattern=[[1024, NT // 8], [1, 8]], base=0, channel_multiplier=8), (), ["tokid"])
        P.op("pool", lambda e: e.iota(tokid_c5, pattern=[[1, 8]], base=0, channel_multiplier=8), (), ["tokid"])
        MSET("pool", rmA, 1.0, ["rm"])
        P.op("pool", lambda e: e.affine_select(out=rmA, in_=rmA, pattern=[[0, 1]], compare_op=ALU.is_ge, fill=0.0, base=63, channel_multiplier=-1), ["rm"], ["rm"])
        TSC("pool", rmB, rmA, -1.0, ALU.mult, ["rm"], ["rm"], s2=1.0, op1=ALU.add)
        CPY("dve", identb, ident, ["ident"], ["identb"])
        DMA("sp", csil, c2T, (), ["csil"])
        ACT(csil, csil, AF.Silu, ["csil"], ["csil"])
        P.barrier()

        def transposes_to(src, rows, dst_fn, src_key, dst_key, nchunks=8, evac="act", pb=(0, 1)):
            for g in range(nchunks // 4):
                b = pb[g % 2]
                for j in range(4):
                    c = g * 4 + j
                    P.op("pe", lambda e, c=c, j=j, b=b: e.transpose(out=PS[b][:, j * 128:j * 128 + rows],
                                                                    in_=src[:rows, c * 128:(c + 1) * 128],
                                                                    identity=ident[:rows, :rows]),
                         [src_key, "ident"], [psk(b)])
                CPY(evac, dst_fn(g * 4, 4), PS[b][:, :].rearrange("p (a b) -> p a b", a=4)[:, :, :rows], [psk(b)], [dst_key])

        def norm_mod(xt, Ab, Bb, ho, ss, kx, kh, kss, rows=128):
            ACT(ho[:rows], xt[:rows], AF.Square, [kx], [kh, kss], accum=ss[:rows])
            TSC("dve", ss[:rows], ss[:rows], 1.0 / D, ALU.mult, [kss], [kss], s2=EPS, op1=ALU.add)
            ACT(ss[:rows], ss[:rows], AF.Sqrt, [kss], [kss])
            RECIP(ss[:rows], ss[:rows], [kss], [kss])
            STT("dve", ho[:rows], xt[:rows], ss[:rows, 0:1], Ab[:rows], ALU.mult, ALU.mult, [kx, kss, "modb"], [kh])
            if Bb is not None:
                TT("dve", ho[:rows], ho[:rows], Bb[:rows], ALU.add, [kh, "modb"], [kh])

        def load_bcast(dst, src_row, key):
            DMA("sp", dst, src_row.partition_broadcast(128), (), [key])

        def modulation(L):
            A.reset()
            wch = [A.alloc([128, 8, 512]) for _ in range(2)]
            brow = A.alloc([1, 6 * D])
            mrow = A.alloc([2, 6 * D])
            gm = A.alloc([2, D])
            gf = A.alloc([2, D])
            DMA("sp", brow[0:1], b_mod[L:L + 1, :], (), ["brow"])
            DMA("sp", gm[0:2], g_mix[L].partition_broadcast(2), (), ["gm"])
            DMA("sp", gf[0:2], g_ffn[L].partition_broadcast(2), (), ["gf"])
            wv = w_mod[L].rearrange("(a p) n -> p a n", p=128)
            for nb in range(12):
                wb = wch[nb % 2]
                DMA("sp", wb, wv[:, :, nb * 512:(nb + 1) * 512], (), [("wch", nb % 2)])
                b = 2 + nb % 2
                for c in range(8):
                    MM(PS[b][0:2, :], csil[:, c, :], wb[:, c, :], c == 0, False, [("wch", nb % 2), "csil"], [psk(b)])
                MM(PS[b][0:2, :], ones[0:1, 0:2], brow[0:1, nb * 512:(nb + 1) * 512], False, True, ["ones", "brow"], [psk(b)])
                CPY("act", mrow[0:2, nb * 512:(nb + 1) * 512], PS[b][0:2, :], [psk(b)], ["mrow"])
            STT("dve", mrow[0:2, D:2 * D], mrow[0:2, D:2 * D], 1.0, gm[0:2], ALU.add, ALU.mult, ["mrow", "gm"], ["mrow"])
            STT("dve", mrow[0:2, 4 * D:5 * D], mrow[0:2, 4 * D:5 * D], 1.0, gf[0:2], ALU.add, ALU.mult, ["mrow", "gf"], ["mrow"])
            DMA("sp", modd.rearrange("s k d -> s (k d)"), mrow[0:2, :], ["mrow"], ["modd"])
            P.barrier()

        MOD_SH1, MOD_A1, MOD_G1, MOD_SH2, MOD_A2, MOD_G2 = range(6)
        SIDX = {"l": 0, "c": 1}

        class PM:
            pass

        def pm_setup(L, s):
            pm = PM()
            pm.G1 = A.alloc([128, D]); pm.A2 = A.alloc([128, D]); pm.B2 = A.alloc([128, D])
            load_bcast(pm.G1, modd[SIDX[s], MOD_G1], "modb")
            load_bcast(pm.A2, modd[SIDX[s], MOD_A2], "modb")
            load_bcast(pm.B2, modd[SIDX[s], MOD_SH2], "modb")
            pm.xt = [A.alloc([128, D]) for _ in range(2)]
            pm.m = [A.alloc([128, D]) for _ in range(2)]
            pm.mT = A.alloc([128, 8, 128])
            pm.mb = [A.alloc([128, D], BF16) for _ in range(2)]
            pm.ss = A.alloc([128, 4])
            pm.ex = A.alloc([128, NE])
            DMA("sp", wr_t, moe_router[L].rearrange("(a p) e -> p a e", p=128), (), ["wr"])
            pm.n = 0
            return pm

        def post_mixer(pm, s, i, y, ykey, xrows=None, mrows=None, rr=128, affcol=None):
            k = pm.n % 2
            pm.n += 1
            xt, m = pm.xt[k], pm.m[k]
            kx, km = ("pmx", k), ("pmm", k)
            if xrows is None:
                rows = slice(i * 128, (i + 1) * 128)
                xrows = XS[s][rows, :]
                mrows = MB[s][rows, :]
            if affcol is None:
                affcol = AFF[s][:, :, i]
            DMA("sp", xt[:rr], xrows, (), [kx])
            TT("dve", y[:rr], y[:rr], pm.G1[:rr], ALU.mult, [ykey, "modb"], [ykey])
            TT("dve", xt[:rr], xt[:rr], y[:rr], ALU.add, [kx, ykey], [kx])
            DMA("sp", xrows, xt[:rr], [kx], [("xs", s, i)])
            norm_mod(xt, pm.A2, pm.B2, m, pm.ss[:, 0:1], kx, km, "pmss", rows=rr)
            CPY("act", pm.mb[k][:rr], m[:rr], [km], [("pmb", k)])
            DMA("sp", mrows, pm.mb[k][:rr], [("pmb", k)], [("ms", s, i)])
            transposes_to(m, rr, lambda c0, n: pm.mT[:, c0:c0 + n, :rr], km, "pmT", evac="act", pb=(0, 1))
            for c in range(8):
                MM(PS[7][:rr, 0:NE], pm.mT[:, c, :rr], wr_t[:, c, :], c == 0, c == 7, ["pmT", "wr"], [psk(7)])
            P.op("dve", lambda e: e.reduce_max(out=pm.ss[:rr, 1:2], in_=PS[7][:rr, 0:NE], axis=AX.X), [psk(7)], ["pmss2"])
            TSC("dve", pm.ss[:rr, 1:2], pm.ss[:rr, 1:2], -1.0, ALU.mult, ["pmss2"], ["pmss2"])
            ACT(pm.ex[:rr], PS[7][:rr, 0:NE], AF.Exp, [psk(7), "pmss2"], ["pmex", "pmss3"], bias=pm.ss[:rr, 1:2], accum=pm.ss[:rr, 2:3])
            RECIP(pm.ss[:rr, 2:3], pm.ss[:rr, 2:3], ["pmss3"], ["pmss3"])
            TSC("dve", affcol[:rr], pm.ex[:rr], pm.ss[:rr, 2:3], ALU.mult, ["pmex", "pmss3"], [("aff", s)])

        def conv_mixer(L, j, strs):
            A.reset()
            win = A.alloc([128, 8, 2 * D], BF16)
            bin_pp = A.alloc([128, 16])
            A1 = {}; B1 = {}
            for s in strs:
                A1[s] = A.alloc([128, D]); B1[s] = A.alloc([128, D])
                load_bcast(A1[s], modd[SIDX[s], MOD_A1], "modb")
                load_bcast(B1[s], modd[SIDX[s], MOD_SH1], "modb")
            xt = [A.alloc([128, D]) for _ in range(2)]
            h = [A.alloc([128, D]) for _ in range(2)]
            ss = A.alloc([128, 2])
            hT = [A.alloc([128, 8, 512], BF16) for _ in range(2)]
            sg = [A.alloc([128, 512]) for _ in range(2)]
            ub = [A.alloc([128, 512], BF16) for _ in range(2)]
            wv = conv_w_in[j].rearrange("(a p) n -> p a n", p=128)
            for c in range(8):
                DMA("pool", win[:, c, :], wv[:, c, :], (), ["win"])
            DMA("sp", bin_pp, conv_b_in_pp[j], (), ["binpp"])
            n = 0
            nb = 0
            for s in strs:
                Ts = TS[s]
                BW = min(512, Ts)
                for blk in range(Ts // BW):
                    hb = hT[nb % 2]
                    for ti in range(BW // 128):
                        i = blk * (BW // 128) + ti
                        k = n % 2
                        n += 1
                        DMA("sp", xt[k], XS[s][i * 128:(i + 1) * 128, :], (), [("xt", k)])
                        norm_mod(xt[k], A1[s], B1[s], h[k], ss[:, k:k + 1], ("xt", k), ("h", k), ("ss", k))
                        transposes_to(h[k], 128, lambda c0, nn, hb=hb, ti=ti: hb[:, c0:c0 + nn, ti * 128:(ti + 1) * 128],
                                      ("h", k), ("hT", nb % 2), evac="act", pb=(0, 1))
                    for fo in range(8):
                        pa, pg = 2 + (fo % 2) * 2, 3 + (fo % 2) * 2
                        for c in range(8):
                            MM(PS[pa][:, :BW], win[:, c, fo * 128:(fo + 1) * 128], hb[:, c, :BW], c == 0, c == 7, ["win", ("hT", nb % 2)], [psk(pa)])
                        for c in range(8):
                            MM(PS[pg][:, :BW], win[:, c, D + fo * 128:D + (fo + 1) * 128], hb[:, c, :BW], c == 0, c == 7, ["win", ("hT", nb % 2)], [psk(pg)])
                        q = fo % 2
                        ACT(sg[q][:, :BW], PS[pg][:, :BW], AF.Sigmoid, [psk(pg), "binpp"], [("sg", q)], bias=bin_pp[:, 8 + fo:9 + fo])
                        STT("dve", ub[q][:, :BW], PS[pa][:, :BW], bin_pp[:, fo:fo + 1], sg[q][:, :BW], ALU.add, ALU.mult, [psk(pa), ("sg", q), "binpp"], [("ub", q)])
                        DMA("sp", FA16[s][fo * 128:(fo + 1) * 128, blk * BW:(blk + 1) * BW], ub[q][:, :BW], [("ub", q)], [("fa", s)])
                    nb += 1
            P.barrier()
            A.reset()
            wout = A.alloc([128, 8, D], BF16)
            lng = A.alloc([128, 8]); lnb = A.alloc([128, 8])
            bout = A.alloc([128, D])
            wv = conv_w_out[j].rearrange("(a p) n -> p a n", p=128)
            for c in range(8):
                DMA("pool", wout[:, c, :], wv[:, c, :], (), ["wout"])
            DMA("sp", lng, conv_ln_g_pp[j], (), ["ln"])
            DMA("sp", lnb, conv_ln_b_pp[j], (), ["ln"])
            load_bcast(bout, conv_b_out[j], "bout")
            dw = A.alloc([128, 8, 31]); dwb = A.alloc([128, 8])
            DMA("sp", dw, conv_dw_pp[j], (), ["dw"])
            DMA("sp", dwb, conv_dw_b_pp[j], (), ["dw"])
            Dg = A.alloc([128, 8 * 31, 128], BF16)
            for c in range(8):
                for jj in range(31):
                    TSC("dve", Dg[:, c * 31 + jj, :], ident, dw[:, c, jj:jj + 1], ALU.mult, ["ident", "dw"], ["Dg"])
            u16 = A.alloc([128, 8, 512 + 30], BF16)
            v = [A.alloc([128, 8, 512])]
            sq = A.alloc([128, 8, 512])
            mean = A.alloc([128, 512]); var = A.alloc([128, 512])
            zT = [A.alloc([128, 8, 512], BF16) for _ in range(2)]
            ysb = [A.alloc([128, D]) for _ in range(2)]
            base = A.off
            nb = 0
            ny = 0
            for s in strs:
                A.off = base
                pm = pm_setup(L, s)
                Ts = TS[s]
                BW = min(512, Ts)
                fbv = FA16[s].rearrange("(a p) t -> p a t", p=128)
                for blk in range(Ts // BW):
                    q = nb % 2
                    nb += 1
                    vv = v[0]
                    kv, kz = ("v", 0), ("zT", q)
                    nblk_ = Ts // BW
                    t0 = blk * BW
                    if blk == 0:
                        MSET("pool", u16[:, :, 0:15], 0.0, ["u16"])
                    else:
                        DMA("sp", u16[:, :, 0:15], fbv[:, :, t0 - 15:t0], (), ["u16"])
                    if blk == nblk_ - 1:
                        MSET("pool", u16[:, :, 15 + BW:30 + BW], 0.0, ["u16"])
                    else:
                        DMA("sp", u16[:, :, 15 + BW:30 + BW], fbv[:, :, t0 + BW:t0 + BW + 15], (), ["u16"])
                    DMA("sp", u16[:, :, 15:15 + BW], fbv[:, :, t0:t0 + BW], (), ["u16"])
                    for c in range(8):
                        pc = (2, 3, 6)[c % 3]
                        for jj in range(31):
                            MM(PS[pc][:, :BW], Dg[:, c * 31 + jj, :], u16[:, c, jj:jj + BW], jj == 0, jj == 30, ["Dg", "u16"], [psk(pc)])
                        ACT(vv[:, c, :BW], PS[pc][:, :BW], AF.Identity, [psk(pc), "dw"], [kv], bias=dwb[:, c:c + 1])
                    ACT(sq[:, :, :BW], vv[:, :, :BW], AF.Square, [kv], ["sq"])
                    for c in range(8):
                        MM(PS[2][:, :BW], ones, vv[:, c, :BW], c == 0, c == 7, ["ones", kv], [psk(2)])
                    for c in range(8):
                        MM(PS[3][:, :BW], ones, sq[:, c, :BW], c == 0, c == 7, ["ones", "sq"], [psk(3)])
                    ACT(mean[:, :BW], PS[2][:, :BW], AF.Copy, [psk(2)], ["mean"], scale=1.0 / D)
                    TT("dve", var[:, :BW], mean[:, :BW], mean[:, :BW], ALU.mult, ["mean"], ["var"])
                    STT("dve", var[:, :BW], PS[3][:, :BW], 1.0 / D, var[:, :BW], ALU.mult, ALU.subtract, [psk(3), "var"], ["var"])
                    TSC("dve", var[:, :BW], var[:, :BW], EPS, ALU.add, ["var"], ["var"])
                    ACT(var[:, :BW], var[:, :BW], AF.Sqrt, ["var"], ["var"])
                    RECIP(var[:, :BW], var[:, :BW], ["var"], ["var"])
                    for c in range(8):
                        eng = "pool" if c % 2 else "dve"
                        TT(eng, sq[:, c, :BW], vv[:, c, :BW], mean[:, :BW], ALU.subtract, [kv, "mean", "sq"], [("sqc", c)])
                        TT(eng, sq[:, c, :BW], sq[:, c, :BW], var[:, :BW], ALU.mult, [("sqc", c), "var"], [("sqc", c)])
                        ACT(zT[q][:, c, :BW], sq[:, c, :BW], AF.Silu, [("sqc", c), "ln"], [kz], bias=lnb[:, c:c + 1], scale=lng[:, c:c + 1])
                    for c in range(8):
                        P.reads.setdefault("sq", {})
                    P.op("act", lambda e: e.activation(out=mean[:, 0:1], in_=mean[:, 0:1], func=AF.Copy), [("sqc", c) for c in range(8)] + ["mean"], ["sq", "mean"])
                    for ti in range(BW // 128):
                        i = blk * (BW // 128) + ti
                        yk = ny % 2
                        ny += 1
                        for nh in range(2):
                            b = 4 + nh
                            for c in range(8):
                                MM(PS[b], zT[q][:, c, ti * 128:(ti + 1) * 128], wout[:, c, nh * 512:(nh + 1) * 512], c == 0, c == 7, [kz, "wout"], [psk(b)])
                            TT("dve", ysb[yk][:, nh * 512:(nh + 1) * 512], PS[b], bout[:, nh * 512:(nh + 1) * 512], ALU.add, [psk(b), "bout"], [("ysb", yk)])
                        post_mixer(pm, s, i, ysb[yk], ("ysb", yk))
            P.barrier()

        def s5_mixer(L, strs, ctx_out):
            A.reset()
            A1 = {}; B1 = {}
            for s in strs:
                A1[s] = A.alloc([128, D]); B1[s] = A.alloc([128, D])
                load_bcast(A1[s], modd[SIDX[s], MOD_A1], "modb")
                load_bcast(B1[s], modd[SIDX[s], MOD_SH1], "modb")
            xt = [A.alloc([128, D]) for _ in range(2)]
            h = [A.alloc([128, D]) for _ in range(2)]
            ss = A.alloc([128, 2])
            hT = [A.alloc([128, 8, 512]) for _ in range(2)]
            n = 0
            nb = 0
            for s in strs:
                Ts = TS[s]
                BW = min(512, Ts)
                fav = FA[s].rearrange("(a p) t -> p a t", p=128)
                for blk in range(Ts // BW):
                    hb = hT[nb % 2]
                    for ti in range(BW // 128):
                        i = blk * (BW // 128) + ti
                        k = n % 2
                        n += 1
                        DMA("sp", xt[k], XS[s][i * 128:(i + 1) * 128, :], (), [("xt", k)])
                        norm_mod(xt[k], A1[s], B1[s], h[k], ss[:, k:k + 1], ("xt", k), ("h", k), ("ss", k))
                        transposes_to(h[k], 128, lambda c0, nn, hb=hb, ti=ti: hb[:, c0:c0 + nn, ti * 128:(ti + 1) * 128],
                                      ("h", k), ("hT", nb % 2), evac="act", pb=(0, 1))
                    DMA("sp", fav[:, :, blk * BW:(blk + 1) * BW], hb[:, :, :BW], [("hT", nb % 2)], [("fa", s)])
                    nb += 1
            P.barrier()
            A.reset()
            TBM = 2048
            NLV = 11
            lre = A.alloc([128, 32]); lim = A.alloc([128, 32]); ldt = A.alloc([128, 32])
            t1 = A.alloc([128, 32]); t2 = A.alloc([128, 32]); t3 = A.alloc([128, 32]); t4 = A.alloc([128, 32])
            ti32 = A.alloc([128, 32], I32)
            cre = A.alloc([128, 32]); cim = A.alloc([128, 32]); ncim = A.alloc([128, 32])
            pwr = A.alloc([128, NLV + 1, 32]); pwi = A.alloc([128, NLV + 1, 32]); npwi = A.alloc([128, NLV + 1, 32])
            bwr = A.alloc([128, 128]); bwi = A.alloc([128, 128])
            mre = A.alloc([128, 128]); mim = A.alloc([128, 128])
            LBr = A.alloc([128, 4, 128]); LBi = A.alloc([128, 4, 128])
            CWr = A.alloc([128, 4, 128]); CWi = A.alloc([128, 4, 128])
            ucb = [A.alloc([128, TBM]) for _ in range(2)]
            KA = [A.alloc([128, TBM]), A.alloc([128, TBM])]
            KB = [A.alloc([128, TBM]), A.alloc([128, TBM])]
            ysb = A.alloc([128, TBM])
            car = A.alloc([128, 4, 2])
            TWO_PI = 2.0 * math.pi
            for d in range(2):
                DMA("sp", lre, s5_lam_re[d], (), ["lre"])
                DMA("sp", lim, s5_lam_im[d], (), ["lim"])
                DMA("sp", ldt, s5_log_dt[d], (), ["ldt"])
                S = "s5s"
                ACT(ldt, ldt, AF.Exp, ["ldt"], ["ldt"])
                TT("dve", t1, lre, ldt, ALU.mult, ["lre", "ldt"], [S])
                ACT(t1, t1, AF.Exp, [S], [S])
                TT("dve", t2, lim, ldt, ALU.mult, ["lim", "ldt", S], [S])
                TSC("dve", t3, t2, 1.0 / TWO_PI, ALU.mult, [S], [S])
                CPY("dve", ti32, t3, [S], [S])
                CPY("dve", t3, ti32, [S], [S])
                STT("dve", t2, t3, -TWO_PI, t2, ALU.mult, ALU.add, [S], [S])
                TSC("dve", t3, t2, math.pi, ALU.is_gt, [S], [S])
                STT("dve", t2, t3, -TWO_PI, t2, ALU.mult, ALU.add, [S], [S])
                TSC("dve", t3, t2, -math.pi, ALU.is_lt, [S], [S])
                STT("dve", t2, t3, TWO_PI, t2, ALU.mult, ALU.add, [S], [S])
                ACT(t4, t2, AF.Sin, [S], [S])
                TSC("dve", t2, t2, math.pi / 2, ALU.add, [S], [S])
                TSC("dve", t3, t2, math.pi, ALU.is_gt, [S], [S])
                STT("dve", t2, t3, -TWO_PI, t2, ALU.mult, ALU.add, [S], [S])
                ACT(t3, t2, AF.Sin, [S], [S])
                TT("dve", pwr[:, 0, :], t1, t3, ALU.mult, [S], [S])
                TT("dve", pwi[:, 0, :], t1, t4, ALU.mult, [S], [S])
                for lv in range(NLV):
                    TT("dve", t1, pwr[:, lv, :], pwr[:, lv, :], ALU.mult, [S], [S])
                    TT("dve", t2, pwi[:, lv, :], pwi[:, lv, :], ALU.mult, [S], [S])
                    TT("dve", pwr[:, lv + 1, :], t1, t2, ALU.subtract, [S], [S])
                    TT("dve", t1, pwr[:, lv, :], pwi[:, lv, :], ALU.mult, [S], [S])
                    TSC("dve", pwi[:, lv + 1, :], t1, 2.0, ALU.mult, [S], [S])
                TSC("dve", npwi, pwi, -1.0, ALU.mult, [S], [S])
                TSC("dve", t1, pwr[:, 0, :], -1.0, ALU.add, [S], [S])
                TT("dve", t2, lre, lre, ALU.mult, [S, "lre"], [S])
                TT("dve", t3, lim, lim, ALU.mult, [S, "lim"], [S])
                TT("dve", t2, t2, t3, ALU.add, [S], [S])
                RECIP(t2, t2, [S], [S])
                TT("dve", t3, t1, lre, ALU.mult, [S], [S])
                TT("dve", t4, pwi[:, 0, :], lim, ALU.mult, [S], [S])
                TT("dve", t3, t3, t4, ALU.add, [S], [S])
                TT("dve", cre, t3, t2, ALU.mult, [S], [S])
                TT("dve", t3, pwi[:, 0, :], lre, ALU.mult, [S], [S])
                TT("dve", t4, t1, lim, ALU.mult, [S], [S])
                TT("dve", t3, t3, t4, ALU.subtract, [S], [S])
                TT("dve", cim, t3, t2, ALU.mult, [S], [S])
                TSC("dve", ncim, cim, -1.0, ALU.mult, [S], [S])
                nu = 0
                for c in range(8):
                    for ti in range(4):
                        it = 4 * c + ti
                        DMA("sp", bwr, s5_bw_re[d, it], (), ["bwr"])
                        DMA("sp", bwi, s5_bw_im[d, it], (), ["bwi"])
                        TSC("dve", mre, bwr, cre[:, it:it + 1], ALU.mult, ["bwr", S], ["mre"])
                        STT("dve", mre, bwi, ncim[:, it:it + 1], mre, ALU.mult, ALU.add, ["bwi", S, "mre"], ["mre"])
                        TSC("dve", mim, bwi, cre[:, it:it + 1], ALU.mult, ["bwi", S], ["mim"])
                        STT("dve", mim, bwr, cim[:, it:it + 1], mim, ALU.mult, ALU.add, ["bwr", S, "mim"], ["mim"])
                        P.op("pe", lambda e: e.transpose(out=PS[0][:, 0:128], in_=mre, identity=ident), ["mre", "ident"], [psk(0)])
                        CPY("act", LBr[:, ti, :], PS[0][:, 0:128], [psk(0)], ["LB"])
                        P.op("pe", lambda e: e.transpose(out=PS[1][:, 0:128], in_=mim, identity=ident), ["mim", "ident"], [psk(1)])
                        CPY("act", LBi[:, ti, :], PS[1][:, 0:128], [psk(1)], ["LB"])
                        DMA("sp", CWr[:, ti, :], s5_cw_re[d, it], (), ["CW"])
                        DMA("sp", CWi[:, ti, :], s5_cw_im[d, it], (), ["CW"])
                    TSC("dve", CWi, CWi, -1.0, ALU.mult, ["CW"], ["CW"])
                    first = True
                    for s in (("c", "l") if "c" in strs else ("l",)):
                        Ts = TS[s]
                        TB = min(TBM, Ts)
                        nlv = int(round(math.log2(TB)))
                        nblk = Ts // TB
                        need_y = (s == "l") or ctx_out
                        order = range(nblk) if d == 0 else range(nblk - 1, -1, -1)
                        for blk in order:
                            t0 = blk * TB
                            uk = nu % 2
                            nu += 1
                            u = ucb[uk]
                            DMA("sp", u[:, :TB], FA[s][c * 128:(c + 1) * 128, t0:t0 + TB], (), [("ucb", uk)])
                            ncb = (TB + 511) // 512
                            for ti in range(4):
                                it = 4 * c + ti
                                for cb in range(ncb):
                                    w = min(512, TB)
                                    cs = slice(cb * 512, cb * 512 + w)
                                    MM(PS[0][:, :w], LBr[:, ti, :], u[:, cs], True, True, ["LB", ("ucb", uk)], [psk(0)])
                                    CPY("act", KA[0][:, cs], PS[0][:, :w], [psk(0)], ["KA0"])
                                    MM(PS[1][:, :w], LBi[:, ti, :], u[:, cs], True, True, ["LB", ("ucb", uk)], [psk(1)])
                                    CPY("act", KA[1][:, cs], PS[1][:, :w], [psk(1)], ["KA1"])
                                ecol = 0 if d == 0 else TB - 1
                                if not first:
                                    ec = slice(ecol, ecol + 1)
                                    STT("dve", KA[0][:, ec], car[:, ti, 0:1], pwr[:, 0, it:it + 1], KA[0][:, ec], ALU.mult, ALU.add, ["car", S, "KA0"], ["KA0"])
                                    STT("dve", KA[0][:, ec], car[:, ti, 1:2], npwi[:, 0, it:it + 1], KA[0][:, ec], ALU.mult, ALU.add, ["car", S, "KA0"], ["KA0"])
                                    STT("dve", KA[1][:, ec], car[:, ti, 1:2], pwr[:, 0, it:it + 1], KA[1][:, ec], ALU.mult, ALU.add, ["car", S, "KA1"], ["KA1"])
                                    STT("dve", KA[1][:, ec], car[:, ti, 0:1], pwi[:, 0, it:it + 1], KA[1][:, ec], ALU.mult, ALU.add, ["car", S, "KA1"], ["KA1"])
                                src, dst = KA, KB
                                sk, dk = ("KA0", "KA1"), ("KB0", "KB1")
                                for lv in range(nlv):
                                    sh = 1 << lv
                                    if d == 0:
                                        S0, S1, SC = slice(0, TB - sh), slice(sh, TB), slice(0, sh)
                                    else:
                                        S0, S1, SC = slice(sh, TB), slice(0, TB - sh), slice(TB - sh, TB)
                                    pr = pwr[:, lv, it:it + 1]; pi_ = pwi[:, lv, it:it + 1]; npi = npwi[:, lv, it:it + 1]
                                    STT("dve", dst[0][:, S1], src[0][:, S0], pr, src[0][:, S1], ALU.mult, ALU.add, [sk[0], S], [dk[0]])
                                    STT("dve", dst[0][:, S1], src[1][:, S0], npi, dst[0][:, S1], ALU.mult, ALU.add, [sk[1], dk[0], S], [dk[0]])
                                    STT("dve", dst[1][:, S1], src[1][:, S0], pr, src[1][:, S1], ALU.mult, ALU.add, [sk[1], S], [dk[1]])
                                    STT("dve", dst[1][:, S1], src[0][:, S0], pi_, dst[1][:, S1], ALU.mult, ALU.add, [sk[0], dk[1], S], [dk[1]])
                                    CPY("act", dst[0][:, SC], src[0][:, SC], [sk[0], dk[0]], [dk[0]])
                                    CPY("act", dst[1][:, SC], src[1][:, SC], [sk[1], dk[1]], [dk[1]])
                                    src, dst = dst, src
                                    sk, dk = dk, sk
                                lcol = TB - 1 if d == 0 else 0
                                CPY("act", car[:, ti, 0:1], src[0][:, lcol:lcol + 1], [sk[0], "car"], ["car"])
                                CPY("act", car[:, ti, 1:2], src[1][:, lcol:lcol + 1], [sk[1], "car"], ["car"])
                                if need_y:
                                    for cb in range(ncb):
                                        w = min(512, TB)
                                        cs = slice(cb * 512, cb * 512 + w)
                                        MM(PS[4 + cb][:, :w], CWr[:, ti, :], src[0][:, cs], ti == 0, False, ["CW", sk[0]], [psk(4 + cb)])
                                        MM(PS[4 + cb][:, :w], CWi[:, ti, :], src[1][:, cs], False, ti == 3, ["CW", sk[1]], [psk(4 + cb)])
                                if src is not KA:
                                    pass
                            if need_y:
                                for cb in range(ncb):
                                    w = min(512, TB)
                                    CPY("act", ysb[:, cb * 512:cb * 512 + w], PS[4 + cb][:, :w], [psk(4 + cb)], ["ysb5"])
                                if d == 0:
                                    DMA("sp", FB[s][c * 128:(c + 1) * 128, t0:t0 + TB], ysb[:, :TB], ["ysb5"], [("fb", s)])
                                else:
                                    DMA("pool", FB[s][c * 128:(c + 1) * 128, t0:t0 + TB], ysb[:, :TB], ["ysb5"], [("fb", s)], accum_op=ALU.add)
                            first = False
                P.barrier()
            A.reset()
            wg = A.alloc([128, 8, 2 * D], BF16)
            dsk = A.alloc([128, 8])
            bg = A.alloc([128, 2 * D])
            wv = s5_w_glu.rearrange("(a p) n -> p a n", p=128)
            for c in range(8):
                DMA("pool", wg[:, c, :], wv[:, c, :], (), ["wg"])
            DMA("sp", dsk, s5_d_pp, (), ["dsk"])
            load_bcast(bg, s5_b_glu, "bg")
            uu = A.alloc([128, 8, 512]); yy = A.alloc([128, 8, 512]); x2 = A.alloc([128, 8, 512])
            gT = [A.alloc([128, 8, 512], BF16) for _ in range(2)]
            ysb = [A.alloc([128, D]) for _ in range(2)]
            tg = A.alloc([128, 512])
            base = A.off
            nb = 0
            ny = 0
            for s in (strs if ctx_out else ["l"]):
                A.off = base
                pm = pm_setup(L, s)
                Ts = TS[s]
                BW = min(512, Ts)
                fav = FA[s].rearrange("(a p) t -> p a t", p=128)
                fbv = FB[s].rearrange("(a p) t -> p a t", p=128)
                for blk in range(Ts // BW):
                    q = nb % 2
                    nb += 1
                    DMA("sp", uu[:, :, :BW], fav[:, :, blk * BW:(blk + 1) * BW], (), ["uu"])
                    DMA("sp", yy[:, :, :BW], fbv[:, :, blk * BW:(blk + 1) * BW], (), ["yy"])
                    for c in range(8):
                        STT("dve", yy[:, c, :BW], uu[:, c, :BW], dsk[:, c:c + 1], yy[:, c, :BW], ALU.mult, ALU.add, ["uu", "yy", "dsk"], ["yy"])
                    gelu_to(yy[:, :, :BW], x2[:, :, :BW], uu[:, :, :BW], gT[q][:, :, :BW], "yy", "x2", "uu", ("gT", q))
                    for ti in range(BW // 128):
                        i = blk * (BW // 128) + ti
                        yk = ny % 2
                        ny += 1
                        for nbk in range(4):
                            b = 2 + nbk
                            for c in range(8):
                                MM(PS[b], gT[q][:, c, ti * 128:(ti + 1) * 128], wg[:, c, nbk * 512:(nbk + 1) * 512], c == 0, c == 7, [("gT", q), "wg"], [psk(b)])
                        for nh in range(2):
                            TT("dve", tg, PS[4 + nh], bg[:, D + nh * 512:D + (nh + 1) * 512], ALU.add, [psk(4 + nh), "bg"], ["tg"])
                            ACT(tg, tg, AF.Sigmoid, ["tg"], ["tg"])
                            ysl = ysb[yk][:, nh * 512:(nh + 1) * 512]
                            TT("dve", ysl, PS[2 + nh], bg[:, nh * 512:(nh + 1) * 512], ALU.add, [psk(2 + nh), "bg"], [("ysb", yk)])
                            TT("dve", ysl, ysl, tg, ALU.mult, [("ysb", yk), "tg"], [("ysb", yk)])
                        post_mixer(pm, s, i, ysb[yk], ("ysb", yk))
            P.barrier()


        def s5_mixer2(L, strs, ctx_out):
            NCS = {s_: TS[s_] // 8 for s_ in strs}
            fag = {s_: FA[s_].rearrange("a t -> (a t)").rearrange("(g p c) -> p g c", g=64, p=128) for s_ in strs}
            fbg = {s_: FB[s_].rearrange("a t -> (a t)").rearrange("(g p c) -> p g c", g=64, p=128) for s_ in strs}
            A.reset()
            A1 = {}; B1 = {}
            for s in strs:
                A1[s] = A.alloc([128, D]); B1[s] = A.alloc([128, D])
                load_bcast(A1[s], modd[SIDX[s], MOD_A1], "modb")
                load_bcast(B1[s], modd[SIDX[s], MOD_SH1], "modb")
            xt = [A.alloc([128, D]) for _ in range(2)]
            junk = A.alloc([128, D])
            ss = A.alloc([128, 2])
            Ht = A.alloc([128, 64, 8, 16])
            Ub = [A.alloc([128, 4, 128]) for _ in range(2)]
            n = 0
            for s in strs:
                NC = NCS[s]
                xv = XS[s].rearrange("(c s) d -> s c d", s=8)
                for j in range((NC + 127) // 128):
                    rows = min(128, NC - j * 128)
                    for s8 in range(8):
                        k = n % 2
                        n += 1
                        kx, kss = ("xt", k), ("ss", k)
                        DMA("sp", xt[k][:rows], xv[s8, j * 128:j * 128 + rows, :], (), [kx])
                        ssk = ss[:, k:k + 1]
                        ACT(junk[:rows], xt[k][:rows], AF.Square, [kx], ["junk", kss], accum=ssk[:rows])
                        TSC("dve", ssk[:rows], ssk[:rows], 1.0 / D, ALU.mult, [kss], [kss], s2=EPS, op1=ALU.add)
                        ACT(ssk[:rows], ssk[:rows], AF.Sqrt, [kss], [kss])
                        RECIP(ssk[:rows], ssk[:rows], [kss], [kss])
                        hv = Ht[:rows, :, s8, :]
                        v3 = lambda ap_: ap_.rearrange("p (g k) -> p g k", g=64)
                        STT("dve", hv, v3(xt[k][:rows]), ssk[:rows, 0:1], v3(A1[s][:rows]), ALU.mult, ALU.mult, [kx, kss, "modb"], ["Ht"])
                        TT("dve", hv, hv, v3(B1[s][:rows]), ALU.add, ["Ht", "modb"], ["Ht"])
                    for g4 in range(16):
                        b = g4 % 2
                        for jj in range(4):
                            g = g4 * 4 + jj
                            P.op("pe", lambda e, g=g, jj=jj, b=b, rows=rows: e.transpose(out=PS[b][:, jj * 128:jj * 128 + rows],
                                 in_=Ht[:rows, g, :, :].rearrange("p s k -> p (s k)"), identity=ident[:rows, :rows]), ["Ht", "ident"], [psk(b)])
                        CPY("act" if b else "dve", Ub[b][:, :, :rows], PS[b].rearrange("p (a b) -> p a b", a=4)[:, :, :rows], [psk(b)], [("Ub", b)])
                        DMA("sp", fag[s][:, g4 * 4:(g4 + 1) * 4, j * 128:j * 128 + rows], Ub[b][:, :, :rows], [("Ub", b)], [("fa", s)])
            P.barrier()
            A.reset()
            NCL = NCS["l"]
            NCC = NCS.get("c", 0)
            NLVMAX = int(round(math.log2(NCL)))
            lre = A.alloc([128, 32]); lim = A.alloc([128, 32]); ldt = A.alloc([128, 32])
            t1 = A.alloc([128, 32]); t2 = A.alloc([128, 32]); t3 = A.alloc([128, 32]); t4 = A.alloc([128, 32])
            ti32 = A.alloc([128, 32], I32)
            cre = [A.alloc([128, 32]) for _ in range(2)]; cim = [A.alloc([128, 32]) for _ in range(2)]; ncim = [A.alloc([128, 32]) for _ in range(2)]
            Pp_r = A.alloc([128, 9, 32]); Pp_i = A.alloc([128, 9, 32])
            Pn_r = A.alloc([128, 8, 32]); Pn_i = A.alloc([128, 8, 32])
            Tn_r = [A.alloc([128, 32, 8]) for _ in range(2)]; Tn_i = [A.alloc([128, 32, 8]) for _ in range(2)]
            Tp_r = [A.alloc([128, 32, 8]) for _ in range(2)]; Tp_i = [A.alloc([128, 32, 8]) for _ in range(2)]
            L7r = [A.alloc([128, 32]) for _ in range(2)]; L7i = [A.alloc([128, 32]) for _ in range(2)]; nL7i = [A.alloc([128, 32]) for _ in range(2)]
            L1r = [A.alloc([128, 32]) for _ in range(2)]; L1i = [A.alloc([128, 32]) for _ in range(2)]
            nL1r = [A.alloc([128, 32]) for _ in range(2)]; nL1i = [A.alloc([128, 32]) for _ in range(2)]
            Qr = [A.alloc([128, NLVMAX + 1, 32]) for _ in range(2)]; Qi = [A.alloc([128, NLVMAX + 1, 32]) for _ in range(2)]; nQi = [A.alloc([128, NLVMAX + 1, 32]) for _ in range(2)]
            maskd = [A.alloc([128, 128]) for _ in range(2)]
            drep = A.alloc([128, 64])
            DMA("sp", drep, s5_d_rep, (), ["drep"])
            TWO_PI = 2.0 * math.pi
            S = "s5s"
            for d in range(2):
                MSET("pool", maskd[d], 1.0, [("mask", d)])
                if d == 0:
                    P.op("pool", lambda e, d=d: e.affine_select(out=maskd[d], in_=maskd[d], pattern=[[16, 8], [0, 16]], compare_op=ALU.is_ge, fill=0.0, base=15, channel_multiplier=-1), [("mask", d)], [("mask", d)])
                else:
                    P.op("pool", lambda e, d=d: e.affine_select(out=maskd[d], in_=maskd[d], pattern=[[-16, 8], [0, 16]], compare_op=ALU.is_ge, fill=0.0, base=0, channel_multiplier=1), [("mask", d)], [("mask", d)])
                DMA("sp", lre, s5_lam_re[d], [S], ["lre"])
                DMA("sp", lim, s5_lam_im[d], [S], ["lim"])
                DMA("sp", ldt, s5_log_dt[d], [S], ["ldt"])
                ACT(ldt, ldt, AF.Exp, ["ldt"], ["ldt"])
                TT("dve", t1, lre, ldt, ALU.mult, ["lre", "ldt"], [S])
                ACT(t1, t1, AF.Exp, [S], [S])
                TT("dve", t2, lim, ldt, ALU.mult, ["lim", "ldt", S], [S])
                TSC("dve", t3, t2, 1.0 / TWO_PI, ALU.mult, [S], [S])
                CPY("dve", ti32, t3, [S], [S])
                CPY("dve", t3, ti32, [S], [S])
                STT("dve", t2, t3, -TWO_PI, t2, ALU.mult, ALU.add, [S], [S])
                TSC("dve", t3, t2, math.pi, ALU.is_gt, [S], [S])
                STT("dve", t2, t3, -TWO_PI, t2, ALU.mult, ALU.add, [S], [S])
                TSC("dve", t3, t2, -math.pi, ALU.is_lt, [S], [S])
                STT("dve", t2, t3, TWO_PI, t2, ALU.mult, ALU.add, [S], [S])
                ACT(t4, t2, AF.Sin, [S], [S])
                TSC("dve", t2, t2, math.pi / 2, ALU.add, [S], [S])
                TSC("dve", t3, t2, math.pi, ALU.is_gt, [S], [S])
                STT("dve", t2, t3, -TWO_PI, t2, ALU.mult, ALU.add, [S], [S])
                ACT(t3, t2, AF.Sin, [S], [S])
                MSET("dve", Pp_r[:, 0, :], 1.0, [S]); MSET("dve", Pp_i[:, 0, :], 0.0, [S])
                TT("dve", Pp_r[:, 1, :], t1, t3, ALU.mult, [S], [S])
                TT("dve", Pp_i[:, 1, :], t1, t4, ALU.mult, [S], [S])

                def cmul(or_, oi_, ar, ai, br, bi):
                    TT("dve", t1, ar, br, ALU.mult, [S], [S])
                    TT("dve", t2, ai, bi, ALU.mult, [S], [S])
                    TT("dve", t3, ar, bi, ALU.mult, [S], [S])
                    TT("dve", t4, ai, br, ALU.mult, [S], [S])
                    TT("dve", or_, t1, t2, ALU.subtract, [S], [S])
                    TT("dve", oi_, t3, t4, ALU.add, [S], [S])

                for jx in range(2, 9):
                    cmul(Pp_r[:, jx, :], Pp_i[:, jx, :], Pp_r[:, jx - 1, :], Pp_i[:, jx - 1, :], Pp_r[:, 1, :], Pp_i[:, 1, :])
                MSET("dve", Pn_r[:, 0, :], 1.0, [S]); MSET("dve", Pn_i[:, 0, :], 0.0, [S])
                TT("dve", t1, Pp_r[:, 1, :], Pp_r[:, 1, :], ALU.mult, [S], [S])
                TT("dve", t2, Pp_i[:, 1, :], Pp_i[:, 1, :], ALU.mult, [S], [S])
                TT("dve", t1, t1, t2, ALU.add, [S], [S])
                RECIP(t1, t1, [S], [S])
                TT("dve", Pn_r[:, 1, :], Pp_r[:, 1, :], t1, ALU.mult, [S], [S])
                STT("dve", Pn_i[:, 1, :], Pp_i[:, 1, :], -1.0, t1, ALU.mult, ALU.mult, [S], [S])
                for jx in range(2, 8):
                    cmul(Pn_r[:, jx, :], Pn_i[:, jx, :], Pn_r[:, jx - 1, :], Pn_i[:, jx - 1, :], Pn_r[:, 1, :], Pn_i[:, 1, :])
                for jx in range(8):
                    jj = jx if d == 0 else 7 - jx
                    CPY("dve", Tn_r[d][:, :, jx], Pn_r[:, jj, :], [S], [S]); CPY("dve", Tn_i[d][:, :, jx], Pn_i[:, jj, :], [S], [S])
                    CPY("dve", Tp_r[d][:, :, jx], Pp_r[:, jj, :], [S], [S]); CPY("dve", Tp_i[d][:, :, jx], Pp_i[:, jj, :], [S], [S])
                CPY("dve", L7r[d], Pp_r[:, 7, :], [S], [S]); CPY("dve", L7i[d], Pp_i[:, 7, :], [S], [S])
                TSC("dve", nL7i[d], Pp_i[:, 7, :], -1.0, ALU.mult, [S], [S])
                CPY("dve", L1r[d], Pp_r[:, 1, :], [S], [S]); CPY("dve", L1i[d], Pp_i[:, 1, :], [S], [S])
                TSC("dve", nL1r[d], Pp_r[:, 1, :], -1.0, ALU.mult, [S], [S]); TSC("dve", nL1i[d], Pp_i[:, 1, :], -1.0, ALU.mult, [S], [S])
                CPY("dve", Qr[d][:, 0, :], Pp_r[:, 8, :], [S], [S]); CPY("dve", Qi[d][:, 0, :], Pp_i[:, 8, :], [S], [S])
                for lv in range(NLVMAX):
                    TT("dve", t1, Qr[d][:, lv, :], Qr[d][:, lv, :], ALU.mult, [S], [S])
                    TT("dve", t2, Qi[d][:, lv, :], Qi[d][:, lv, :], ALU.mult, [S], [S])
                    TT("dve", Qr[d][:, lv + 1, :], t1, t2, ALU.subtract, [S], [S])
                    TT("dve", t1, Qr[d][:, lv, :], Qi[d][:, lv, :], ALU.mult, [S], [S])
                    TSC("dve", Qi[d][:, lv + 1, :], t1, 2.0, ALU.mult, [S], [S])
                TSC("dve", nQi[d], Qi[d], -1.0, ALU.mult, [S], [S])
                TSC("dve", t1, Pp_r[:, 1, :], -1.0, ALU.add, [S], [S])
                TT("dve", t2, lre, lre, ALU.mult, [S, "lre"], [S])
                TT("dve", t3, lim, lim, ALU.mult, [S, "lim"], [S])
                TT("dve", t2, t2, t3, ALU.add, [S], [S])
                RECIP(t2, t2, [S], [S])
                TT("dve", t3, t1, lre, ALU.mult, [S], [S])
                TT("dve", t4, Pp_i[:, 1, :], lim, ALU.mult, [S], [S])
                TT("dve", t3, t3, t4, ALU.add, [S], [S])
                TT("dve", cre[d], t3, t2, ALU.mult, [S], [S])
                TT("dve", t3, Pp_i[:, 1, :], lre, ALU.mult, [S], [S])
                TT("dve", t4, t1, lim, ALU.mult, [S], [S])
                TT("dve", t3, t3, t4, ALU.subtract, [S], [S])
                TT("dve", cim[d], t3, t2, ALU.mult, [S], [S])
                TSC("dve", ncim[d], cim[d], -1.0, ALU.mult, [S, "lre", "lim"], [S])
            OPN = ("TzA", "TzB", "WZAr", "WZAi", "WZBr", "WZBi", "WYAr", "WYAi", "WYBr", "WYBi")
            OPS = [[{nm: A.alloc([128, 128]) for nm in OPN} for _ in range(4)] for _ in range(2)]
            SB = {}
            for s in strs:
                SB[s] = [[[A.alloc([128, NCS[s] + 1]) for _ in range(2)] for _ in range(4)] for _ in range(2)]
            bn = [A.alloc([128, 16]) for _ in range(2)]; cn = [A.alloc([128, 16]) for _ in range(2)]
            Bb = [A.alloc([128, 16]) for _ in range(2)]
            VBr = A.alloc([128, 8, 16]); VBi = A.alloc([128, 8, 16]); nVBi = A.alloc([128, 8, 16])
            VCr = A.alloc([128, 8, 16]); VCi = A.alloc([128, 8, 16])
            VCm = [A.alloc([128, 128]) for _ in range(4)]
            wt = [A.alloc([128, 128]) for _ in range(4)]
            Ug = [A.alloc([128, NCL]) for _ in range(4)]
            KPAD = NCL // 2
            KA = [A.alloc([128, NCL + KPAD]), A.alloc([128, NCL + KPAD])]
            KB = [A.alloc([128, NCL + KPAD]), A.alloc([128, NCL + KPAD])]
            car = A.alloc([128, 2, 4, 2])
            yt = [A.alloc([128, 512]) for _ in range(2)]
            gx2 = A.alloc([128, 512]); gtm = A.alloc([128, 512])
            go = [A.alloc([128, 512]) for _ in range(2)]
            f2 = lambda ap_: ap_.rearrange("p a b -> p (a b)")
            nug = 0
            ngo = 0
            for c8 in range(8):
                for d in range(2):
                    for ti in range(4):
                        it = 4 * c8 + ti
                        O = OPS[d][ti]
                        ko = ("ops", d, ti)
                        DMA("sp", bn[0], s5_bn_re[d, it], (), ["bn"]); DMA("sp", bn[1], s5_bn_im[d, it], (), ["bn"])
                        DMA("sp", cn[0], s5_cn_re[d, it], (), ["cn"]); DMA("sp", cn[1], s5_cn_im[d, it], (), ["cn"])
                        W_ = "s5w"
                        TSC("dve", Bb[0], bn[0], cre[d][:, it:it + 1], ALU.mult, ["bn", S], [W_])
                        STT("dve", Bb[0], bn[1], ncim[d][:, it:it + 1], Bb[0], ALU.mult, ALU.add, ["bn", S, W_], [W_])
                        TSC("dve", Bb[1], bn[1], cre[d][:, it:it + 1], ALU.mult, ["bn", S, W_], [W_])
                        STT("dve", Bb[1], bn[0], cim[d][:, it:it + 1], Bb[1], ALU.mult, ALU.add, ["bn", S, W_], [W_])
                        bc_k = lambda ap_: ap_.rearrange("p (o k) -> p o k", o=1).to_broadcast([128, 8, 16])
                        bc_s = lambda ap_: ap_.rearrange("p (s o) -> p s o", o=1).to_broadcast([128, 8, 16])
                        tA = wt[0].rearrange("p (a b) -> p a b", a=8); tB = wt[1].rearrange("p (a b) -> p a b", a=8)
                        TT("dve", VBr, bc_k(Bb[0]), bc_s(Tn_r[d][:, it, :]), ALU.mult, [W_, S], [W_])
                        TT("dve", tA, bc_k(Bb[1]), bc_s(Tn_i[d][:, it, :]), ALU.mult, [W_, S], [W_])
                        TT("dve", VBr, VBr, tA, ALU.subtract, [W_], [W_])
                        TT("dve", VBi, bc_k(Bb[0]), bc_s(Tn_i[d][:, it, :]), ALU.mult, [W_, S], [W_])
                        TT("dve", tA, bc_k(Bb[1]), bc_s(Tn_r[d][:, it, :]), ALU.mult, [W_, S], [W_])
                        TT("dve", VBi, VBi, tA, ALU.add, [W_], [W_])
                        TSC("dve", nVBi, VBi, -1.0, ALU.mult, [W_], [W_])
                        TT("dve", VCr, bc_k(cn[0]), bc_s(Tp_r[d][:, it, :]), ALU.mult, ["cn", W_, S], [W_])
                        TT("dve", tA, bc_k(cn[1]), bc_s(Tp_i[d][:, it, :]), ALU.mult, ["cn", W_, S], [W_])
                        TT("dve", VCr, VCr, tA, ALU.subtract, [W_], [W_])
                        TT("dve", VCi, bc_k(cn[0]), bc_s(Tp_i[d][:, it, :]), ALU.mult, ["cn", W_, S], [W_])
                        TT("dve", tA, bc_k(cn[1]), bc_s(Tp_r[d][:, it, :]), ALU.mult, ["cn", W_, S], [W_])
                        TT("dve", VCi, VCi, tA, ALU.add, [W_], [W_])
                        TSC("dve", VCm[0], f2(VCr), rmA[:, 0:1], ALU.mult, [W_, "rm"], [W_])
                        TSC("dve", VCm[1], f2(VCi), rmA[:, 0:1], ALU.mult, [W_, "rm"], [W_])
                        TSC("dve", VCm[2], f2(VCr), rmB[:, 0:1], ALU.mult, [W_, "rm"], [W_])
                        TSC("dve", VCm[3], f2(VCi), rmB[:, 0:1], ALU.mult, [W_, "rm"], [W_])
                        for gi, nm in ((0, "TzA"), (1, "TzB")):
                            MM(PS[0][:, 0:128], f2(VBr), VCm[2 * gi], True, False, [W_], [psk(0)])
                            MM(PS[0][:, 0:128], f2(nVBi), VCm[2 * gi + 1], False, True, [W_], [psk(0)])
                            TT("dve", O[nm], PS[0][:, 0:128], maskd[d], ALU.mult, [psk(0), ("mask", d)], [ko])
                        TSC("dve", wt[2], f2(VBr), L7r[d][:, it:it + 1], ALU.mult, [W_, S], [W_])
                        STT("dve", wt[2], f2(VBi), nL7i[d][:, it:it + 1], wt[2], ALU.mult, ALU.add, [W_, S], [W_])
                        TSC("dve", wt[3], f2(VBr), L7i[d][:, it:it + 1], ALU.mult, [W_, S], [W_])
                        STT("dve", wt[3], f2(VBi), L7r[d][:, it:it + 1], wt[3], ALU.mult, ALU.add, [W_, S], [W_])
                        for q_, (na, nb_) in ((2, ("WZAr", "WZBr")), (3, ("WZAi", "WZBi"))):
                            P.op("pe", lambda e, q_=q_: e.transpose(out=PS[1][:, 0:128], in_=wt[q_], identity=ident), [W_, "ident"], [psk(1)])
                            MSET("pool", O[na][:, 64:128], 0.0, [ko]); MSET("pool", O[nb_][:, 0:64], 0.0, [ko])
                            CPY("act", O[na][:, 0:64], PS[1][:, 0:64], [psk(1)], [ko])
                            CPY("act", O[nb_][:, 64:128], PS[1][:, 64:128], [psk(1)], [ko])
                        TSC("dve", wt[0], f2(VCr), L1r[d][:, it:it + 1], ALU.mult, [W_, S], [W_])
                        STT("dve", wt[0], f2(VCi), nL1i[d][:, it:it + 1], wt[0], ALU.mult, ALU.add, [W_, S], [W_])
                        TSC("dve", wt[1], f2(VCr), nL1i[d][:, it:it + 1], ALU.mult, [W_, S], [W_])
                        STT("dve", wt[1], f2(VCi), nL1r[d][:, it:it + 1], wt[1], ALU.mult, ALU.add, [W_, S], [W_])
                        TSC("dve", O["WYAr"], wt[0], rmA[:, 0:1], ALU.mult, [W_, "rm"], [ko]); TSC("dve", O["WYBr"], wt[0], rmB[:, 0:1], ALU.mult, [W_, "rm"], [ko])
                        TSC("dve", O["WYAi"], wt[1], rmA[:, 0:1], ALU.mult, [W_, "rm"], [ko]); TSC("dve", O["WYBi"], wt[1], rmB[:, 0:1], ALU.mult, [W_, "rm"], [ko])
                for si, s in enumerate(("c", "l") if "c" in strs else ("l",)):
                    NC = NCS[s]
                    nlv = int(round(math.log2(NC)))
                    first = (si == 0)
                    for ti in range(4):
                        it = 4 * c8 + ti
                        ua = Ug[nug % 4]; ka_ = ("Ug", nug % 4); nug += 1
                        ub_ = Ug[nug % 4]; kb_ = ("Ug", nug % 4); nug += 1
                        DMA("sp", ua[:, :NC], fag[s][:, 2 * it, :], (), [ka_])
                        DMA("sp", ub_[:, :NC], fag[s][:, 2 * it + 1, :], (), [kb_])
                        for d in range(2):
                            O = OPS[d][ti]
                            ko = ("ops", d, ti)
                            msh = max(NC // 2, 1)
                            D0 = KPAD if d == 0 else 0
                            PZ = slice(KPAD - msh, KPAD) if d == 0 else slice(NC, NC + msh)
                            for bi_, (buf, kk) in enumerate(((KA[0], "KA0"), (KA[1], "KA1"), (KB[0], "KB0"), (KB[1], "KB1"))):
                                MSET("pool", buf[:, PZ], 0.0, [kk])
                            for cb in range((NC + 511) // 512):
                                w = min(512, NC - cb * 512)
                                cs = slice(cb * 512, cb * 512 + w)
                                ds_ = slice(D0 + cb * 512, D0 + cb * 512 + w)
                                MM(PS[2][:, :w], O["WZAr"], ua[:, cs], True, False, [ko, ka_], [psk(2)])
                                MM(PS[2][:, :w], O["WZBr"], ub_[:, cs], False, True, [ko, kb_], [psk(2)])
                                CPY("act", KA[0][:, ds_], PS[2][:, :w], [psk(2)], ["KA0"])
                                MM(PS[3][:, :w], O["WZAi"], ua[:, cs], True, False, [ko, ka_], [psk(3)])
                                MM(PS[3][:, :w], O["WZBi"], ub_[:, cs], False, True, [ko, kb_], [psk(3)])
                                CPY("act", KA[1][:, ds_], PS[3][:, :w], [psk(3)], ["KA1"])
                            ecol = D0 if d == 0 else D0 + NC - 1
                            cr_ = car[:, d, ti, 0:1]; ci_ = car[:, d, ti, 1:2]
                            kc = ("car", d, ti)
                            if not first:
                                ec = slice(ecol, ecol + 1)
                                STT("dve", KA[0][:, ec], cr_, Qr[d][:, 0, it:it + 1], KA[0][:, ec], ALU.mult, ALU.add, [kc, S, "KA0"], ["KA0"])
                                STT("dve", KA[0][:, ec], ci_, nQi[d][:, 0, it:it + 1], KA[0][:, ec], ALU.mult, ALU.add, [kc, S, "KA0"], ["KA0"])
                                STT("dve", KA[1][:, ec], ci_, Qr[d][:, 0, it:it + 1], KA[1][:, ec], ALU.mult, ALU.add, [kc, S, "KA1"], ["KA1"])
                                STT("dve", KA[1][:, ec], cr_, Qi[d][:, 0, it:it + 1], KA[1][:, ec], ALU.mult, ALU.add, [kc, S, "KA1"], ["KA1"])
                            src, dst = KA, KB
                            sk, dk = ("KA0", "KA1"), ("KB0", "KB1")
                            S1 = slice(D0, D0 + NC)
                            for lv in range(nlv):
                                sh = 1 << lv
                                S0 = slice(D0 - sh, D0 - sh + NC) if d == 0 else slice(D0 + sh, D0 + sh + NC)
                                pr = Qr[d][:, lv, it:it + 1]; pi_ = Qi[d][:, lv, it:it + 1]; npi = nQi[d][:, lv, it:it + 1]
                                STT("dve", dst[0][:, S1], src[0][:, S0], pr, src[0][:, S1], ALU.mult, ALU.add, [sk[0], S], [dk[0]])
                                STT("dve", dst[1][:, S1], src[1][:, S0], pr, src[1][:, S1], ALU.mult, ALU.add, [sk[1], S], [dk[1]])
                                STT("dve", dst[0][:, S1], src[1][:, S0], npi, dst[0][:, S1], ALU.mult, ALU.add, [sk[1], dk[0], S], [dk[0]])
                                STT("dve", dst[1][:, S1], src[0][:, S0], pi_, dst[1][:, S1], ALU.mult, ALU.add, [sk[0], dk[1], S], [dk[1]])
                                src, dst = dst, src
                                sk, dk = dk, sk
                            sbr, sbi = SB[s][d][ti]
                            ksb = ("SB", s, d, ti)
                            off = 1 if d == 0 else 0
                            icol = 0 if d == 0 else NC
                            CPY("act", sbr[:, off:off + NC], src[0][:, D0:D0 + NC], [sk[0]], [ksb])
                            CPY("act", sbi[:, off:off + NC], src[1][:, D0:D0 + NC], [sk[1]], [ksb])
                            if first:
                                MSET("pool", sbr[:, icol:icol + 1], 0.0, [ksb]); MSET("pool", sbi[:, icol:icol + 1], 0.0, [ksb])
                            else:
                                CPY("act", sbr[:, icol:icol + 1], cr_, [kc], [ksb]); CPY("act", sbi[:, icol:icol + 1], ci_, [kc], [ksb])
                            lcol = D0 + NC - 1 if d == 0 else D0
                            CPY("act", cr_, src[0][:, lcol:lcol + 1], [sk[0], ksb], [kc])
                            CPY("act", ci_, src[1][:, lcol:lcol + 1], [sk[1], ksb], [kc])
                    if s == "l" or ctx_out:
                        for gl in range(8):
                            g = 8 * c8 + gl
                            ti = gl // 2
                            AB = "A" if gl % 2 == 0 else "B"
                            ug_ = Ug[nug % 4]; ku = ("Ug", nug % 4); nug += 1
                            DMA("sp", ug_[:, :NC], fag[s][:, g, :], (), [ku])
                            for cb in range((NC + 511) // 512):
                                w = min(512, NC - cb * 512)
                                c0 = cb * 512
                                pb_ = 4 + (ngo % 2)
                                first_mm = True
                                for d in range(2):
                                    O = OPS[d][ti]
                                    ko = ("ops", d, ti)
                                    sbr, sbi = SB[s][d][ti]
                                    ksb = ("SB", s, d, ti)
                                    so = c0 if d == 0 else c0 + 1
                                    MM(PS[pb_][:, :w], O["Tz" + AB], ug_[:, c0:c0 + w], first_mm, False, [ko, ku], [psk(pb_)])
                                    first_mm = False
                                    MM(PS[pb_][:, :w], O["WY" + AB + "r"], sbr[:, so:so + w], False, False, [ko, ksb], [psk(pb_)])
                                    MM(PS[pb_][:, :w], O["WY" + AB + "i"], sbi[:, so:so + w], False, d == 1, [ko, ksb], [psk(pb_)])
                                q = ngo % 2
                                ngo += 1
                                STT("dve", yt[q][:, :w], ug_[:, c0:c0 + w], drep[:, g:g + 1], PS[pb_][:, :w], ALU.mult, ALU.add, [ku, "drep", psk(pb_)], [("yt", q)])
                                gelu_to(yt[q][:, :w], gx2[:, :w], gtm[:, :w], go[q][:, :w], ("yt", q), "gx2", "gtm", ("go", q))
                                DMA("sp", fbg[s][:, g, c0:c0 + w], go[q][:, :w], [("go", q)], [("fb", s)])
            P.barrier()
            A.reset()
            wg = A.alloc([128, 8, 2 * D], BF16)
            bg = A.alloc([128, 2 * D])
            wv = s5_w_glu.rearrange("(a p) n -> p a n", p=128)
            for c in range(8):
                DMA("pool", wg[:, c, :], wv[:, c, :], (), ["wg"])
            load_bcast(bg, s5_b_glu, "bg")
            Gb = A.alloc([128, 64, 128])
            Gtok = A.alloc([128, 8, D], BF16)
            gT = [A.alloc([128, 8, 128], BF16) for _ in range(2)]
            ysb = [A.alloc([128, D]) for _ in range(2)]
            tg = A.alloc([128, 512])
            base = A.off
            ny = 0
            ngt = 0
            for s in (strs if ctx_out else ["l"]):
                A.off = base
                pm = pm_setup(L, s)
                NC = NCS[s]
                xv = XS[s].rearrange("(c s) d -> s c d", s=8)
                mv = MB[s].rearrange("(c s) d -> s c d", s=8)
                if s == "c":
                    MSET("dve", AFFC5, -1.0, [("aff", s)])
                for j in range((NC + 127) // 128):
                    rows = min(128, NC - j * 128)
                    DMA("sp", Gb[:, :, :rows], fbg[s][:, :, j * 128:j * 128 + rows], (), ["Gb"])
                    for g4 in range(16):
                        b = g4 % 2
                        for jj in range(4):
                            g = g4 * 4 + jj
                            P.op("pe", lambda e, g=g, jj=jj, b=b, rows=rows: e.transpose(out=PS[b][:rows, jj * 128:(jj + 1) * 128],
                                 in_=Gb[:, g, :rows], identity=ident), ["Gb", "ident"], [psk(b)])
                        for jj in range(4):
                            g = g4 * 4 + jj
                            CPY("act" if jj % 2 else "dve", Gtok[:rows, :, 16 * g:16 * g + 16],
                                PS[b][:rows, jj * 128:(jj + 1) * 128].rearrange("p (t k) -> p t k", t=8), [psk(b)], ["Gtok"])
                    for tau in range(8):
                        q = ngt % 2
                        ngt += 1
                        for g2_ in range(2):
                            b = g2_
                            pb16 = PS[b].bitcast(BF16)
                            for jj in range(4):
                                c = g2_ * 4 + jj
                                P.op("pe", lambda e, c=c, jj=jj, pb16=pb16, rows=rows, tau=tau: e.transpose(out=pb16[:, jj * 128:jj * 128 + rows],
                                     in_=Gtok[:rows, tau, c * 128:(c + 1) * 128], identity=identb[:rows, :rows]), ["Gtok", "identb"], [psk(b)])
                            CPY("act" if g2_ else "dve", gT[q][:, g2_ * 4:g2_ * 4 + 4, :rows],
                                pb16[:, 0:512].rearrange("p (a b) -> p a b", a=4)[:, :, :rows], [psk(b)], [("gT", q)])
                        yk = ny % 2
                        ny += 1
                        for nbk in range(4):
                            b = 2 + nbk
                            for c in range(8):
                                MM(PS[b][:rows, :], gT[q][:, c, :rows], wg[:, c, nbk * 512:(nbk + 1) * 512], c == 0, c == 7, [("gT", q), "wg"], [psk(b)])
                        for nh in range(2):
                            TT("dve", tg[:rows], PS[4 + nh][:rows, :], bg[:rows, D + nh * 512:D + (nh + 1) * 512], ALU.add, [psk(4 + nh), "bg"], ["tg"])
                            ACT(tg[:rows], tg[:rows], AF.Sigmoid, ["tg"], ["tg"])
                            ysl = ysb[yk][:rows, nh * 512:(nh + 1) * 512]
                            TT("dve", ysl, PS[2 + nh][:rows, :], bg[:rows, nh * 512:(nh + 1) * 512], ALU.add, [psk(2 + nh), "bg"], [("ysb", yk)])
                            TT("dve", ysl, ysl, tg[:rows], ALU.mult, [("ysb", yk), "tg"], [("ysb", yk)])
                        if s == "l":
                            affcol = AFF["l"][:, :, 8 * j + tau]
                        else:
                            affcol = AFFC5[:, :, tau]
                        post_mixer(pm, s, ("s5", j, tau), ysb[yk], ("ysb", yk),
                                   xrows=xv[tau, j * 128:j * 128 + rows, :], mrows=mv[tau, j * 128:j * 128 + rows, :], rr=rows, affcol=affcol)
            RCFG["l"] = (AFF["l"], NT, tokid_l5)
            if ctx_out and "c" in strs:
                RCFG["c"] = (AFFC5, 8, tokid_c5)
            P.barrier()

        def gelu_to(y, x2, tmp, outp, ky, kx2, ktmp, kout):
            ACT(x2, y, AF.Square, [ky], [kx2])
            TSC("dve", x2, x2, 0.044715, ALU.mult, [kx2], [kx2], s2=1.0, op1=ALU.add)
            TT("dve", x2, x2, y, ALU.mult, [kx2, ky], [kx2])
            ACT(tmp, x2, AF.Sigmoid, [kx2], [ktmp], scale=1.5957691216057308)
            TT("dve", outp, tmp, y, ALU.mult, [ktmp, ky], [kout])

        def lru_mixer(L, strs, ctx_out):
            A.reset()
            wx = A.alloc([128, 8, D], BF16); wy = A.alloc([128, 8, D], BF16)
            bx = A.alloc([128, 8]); by = A.alloc([128, 8])
            for nm, wt, src_w in (("wx", wx, lru_w_x), ("wy", wy, lru_w_y)):
                wv = src_w.rearrange("(a p) n -> p a n", p=128)
                for c in range(8):
                    DMA("pool", wt[:, c, :], wv[:, c, :], (), [nm])
            DMA("sp", bx, lru_b_x_pp, (), ["bx"])
            DMA("sp", by, lru_b_y_pp, (), ["by"])
            A1 = {}; B1 = {}
            for s in strs:
                A1[s] = A.alloc([128, D]); B1[s] = A.alloc([128, D])
                load_bcast(A1[s], modd[SIDX[s], MOD_A1], "modb")
                load_bcast(B1[s], modd[SIDX[s], MOD_SH1], "modb")
            xt = [A.alloc([128, D]) for _ in range(2)]
            h = [A.alloc([128, D]) for _ in range(2)]
            ss = A.alloc([128, 2])
            hT = [A.alloc([128, 8, 512], BF16) for _ in range(2)]
            xo = [A.alloc([128, 512]) for _ in range(2)]
            go = [A.alloc([128, 512]) for _ in range(2)]
            gx2 = A.alloc([128, 512]); gtm = A.alloc([128, 512]); gy = A.alloc([128, 512])
            n = 0
            nb = 0
            for s in strs:
                Ts = TS[s]
                BW = min(512, Ts)
                for blk in range(Ts // BW):
                    hb = hT[nb % 2]
                    for ti in range(BW // 128):
                        i = blk * (BW // 128) + ti
                        k = n % 2
                        n += 1
                        DMA("sp", xt[k], XS[s][i * 128:(i + 1) * 128, :], (), [("xt", k)])
                        norm_mod(xt[k], A1[s], B1[s], h[k], ss[:, k:k + 1], ("xt", k), ("h", k), ("ss", k))
                        transposes_to(h[k], 128, lambda c0, nn, hb=hb, ti=ti: hb[:, c0:c0 + nn, ti * 128:(ti + 1) * 128],
                                      ("h", k), ("hT", nb % 2), evac="act", pb=(0, 1))
                    for oc in range(8):
                        q = oc % 2
                        pa, pg = 2 + q * 2, 3 + q * 2
                        for c in range(8):
                            MM(PS[pa][:, :BW], wx[:, c, oc * 128:(oc + 1) * 128], hb[:, c, :BW], c == 0, c == 7, ["wx", ("hT", nb % 2)], [psk(pa)])
                        ACT(xo[q][:, :BW], PS[pa][:, :BW], AF.Identity, [psk(pa), "bx"], [("xo", q)], bias=bx[:, oc:oc + 1])
                        DMA("sp", FA[s][oc * 128:(oc + 1) * 128, blk * BW:(blk + 1) * BW], xo[q][:, :BW], [("xo", q)], [("fa", s)])
                        if s == "l":
                            for c in range(8):
                                MM(PS[pg][:, :BW], wy[:, c, oc * 128:(oc + 1) * 128], hb[:, c, :BW], c == 0, c == 7, ["wy", ("hT", nb % 2)], [psk(pg)])
                            ACT(gy[:, :BW], PS[pg][:, :BW], AF.Identity, [psk(pg), "by"], ["gy"], bias=by[:, oc:oc + 1])
                            gelu_to(gy[:, :BW], gx2[:, :BW], gtm[:, :BW], go[q][:, :BW], "gy", "gx2", "gtm", ("go", q))
                            DMA("sp", FC[s][oc * 128:(oc + 1) * 128, blk * BW:(blk + 1) * BW], go[q][:, :BW], [("go", q)], [("fc", s)])
                    nb += 1
            P.barrier()
            A.reset()
            TBM = 2048
            cw = A.alloc([128, 8, 4]); cb_ = A.alloc([128, 8])
            ba = A.alloc([128, 2, 8]); bi = A.alloc([128, 2, 8]); nsp = A.alloc([128, 2, 8])
            wa = A.alloc([128, 128]); wi = A.alloc([128, 128])
            DMA("sp", cw, lru_conv_w_pp, (), ["cw"])
            DMA("sp", cb_, lru_conv_b_pp, (), ["cw"])
            for d in range(2):
                DMA("sp", ba[:, d, :], lru_b_a_pp[d], (), ["ba"])
                DMA("sp", bi[:, d, :], lru_b_i_pp[d], (), ["bi"])
                DMA("sp", nsp[:, d, :], lru_lam_pp[d], (), ["nsp"])
            ACT(nsp, nsp, AF.Exp, ["nsp"], ["nsp"], scale=-1.0)
            ACT(nsp, nsp, AF.Ln, ["nsp"], ["nsp"], bias=1.0)
            TSC("dve", nsp, nsp, -8.0, ALU.mult, ["nsp"], ["nsp"])
            xb = [A.alloc([128, TBM + 3]) for _ in range(2)]
            xc2 = [A.alloc([128, TBM]) for _ in range(2)]
            av2 = [A.alloc([128, TBM]) for _ in range(2)]; bv2 = [A.alloc([128, TBM]) for _ in range(2)]; ig2 = [A.alloc([128, TBM]) for _ in range(2)]
            hb2 = [A.alloc([128, TBM]) for _ in range(2)]
            car = A.alloc([128, 1])
            nx = 0
            nh2 = 0
            for d in range(2):
                for c in range(8):
                    DMA("sp", wa, lru_w_a[d, c], (), ["wa"])
                    DMA("sp", wi, lru_w_i[d, c], (), ["wi"])
                    first = True
                    for s in (("c", "l") if "c" in strs else ("l",)):
                        Ts = TS[s]
                        TB = min(TBM, Ts)
                        nblk = Ts // TB
                        need_y = (s == "l") or ctx_out
                        order = range(nblk) if d == 0 else range(nblk - 1, -1, -1)
                        for blk in order:
                            t0 = blk * TB
                            k = nx % 2
                            nx += 1
                            xbk = xb[k]
                            kx = ("xb", k)
                            xc, av, bv, ig = xc2[k], av2[k], bv2[k], ig2[k]
                            kxc, kav, kbv, kig = ("xc", k), ("av", k), ("bv", k), ("ig", k)
                            rowsl = slice(c * 128, (c + 1) * 128)
                            if blk == 0:
                                MSET("dve", xbk[:, 0:2], 0.0, [kx])
                            else:
                                DMA("sp", xbk[:, 0:2], FA[s][rowsl, t0 - 2:t0], (), [kx])
                            if blk == nblk - 1:
                                MSET("dve", xbk[:, 2 + TB:3 + TB], 0.0, [kx])
                            else:
                                DMA("sp", xbk[:, 2 + TB:3 + TB], FA[s][rowsl, t0 + TB:t0 + TB + 1], (), [kx], allow_slow_non_contiguous=True)
                            DMA("sp", xbk[:, 2:2 + TB], FA[s][rowsl, t0:t0 + TB], (), [kx])
                            TSC("dve", xc[:, :TB], xbk[:, 0:TB], cw[:, c, 0:1], ALU.mult, [kx, "cw"], [kxc], s2=cb_[:, c:c + 1], op1=ALU.add)
                            for jj in range(1, 4):
                                STT("dve", xc[:, :TB], xbk[:, jj:jj + TB], cw[:, c, jj:jj + 1], xc[:, :TB], ALU.mult, ALU.add, [kx, kxc, "cw"], [kxc])
                            ncb = (TB + 511) // 512
                            for cbk in range(ncb):
                                w = min(512, TB)
                                cs = slice(cbk * 512, cbk * 512 + w)
                                pa, pg = 2 + (cbk % 2) * 2, 3 + (cbk % 2) * 2
                                MM(PS[pa][:, :w], wa, xc[:, cs], True, True, ["wa", kxc], [psk(pa)])
                                MM(PS[pg][:, :w], wi, xc[:, cs], True, True, ["wi", kxc], [psk(pg)])
                                ACT(av[:, cs], PS[pa][:, :w], AF.Sigmoid, [psk(pa), "ba"], [kav], bias=ba[:, d, c:c + 1])
                                ACT(ig[:, cs], PS[pg][:, :w], AF.Sigmoid, [psk(pg), "bi"], [kig], bias=bi[:, d, c:c + 1])
                            ACT(av[:, :TB], av[:, :TB], AF.Exp, [kav, "nsp"], [kav], scale=nsp[:, d, c:c + 1])
                            TT("dve", bv[:, :TB], av[:, :TB], av[:, :TB], ALU.mult, [kav], [kbv])
                            ACT(bv[:, :TB], bv[:, :TB], AF.Sqrt, [kbv], [kbv], bias=1.0, scale=-1.0)
                            TT("dve", ig[:, :TB], ig[:, :TB], xc[:, :TB], ALU.mult, [kig, kxc], [kig])
                            TT("dve", bv[:, :TB], bv[:, :TB], ig[:, :TB], ALU.mult, [kbv, kig], [kbv])
                            hk = nh2 % 2
                            nh2 += 1
                            hh = hb2[hk]
                            init = 0.0 if first else car[:, 0:1]
                            if d == 0:
                                P.op("dve", lambda e, hh=hh, init=init, TB=TB, av=av, bv=bv: e.tensor_tensor_scan(out=hh[:, :TB], data0=av[:, :TB], data1=bv[:, :TB], initial=init, op0=ALU.mult, op1=ALU.add),
                                     [kav, kbv, "car"], [("hb2", hk)])
                                lcol = TB - 1
                            else:
                                P.op("dve", lambda e, hh=hh, init=init, TB=TB, av=av, bv=bv: e.tensor_tensor_scan(out=hh[:, TB - 1::-1] if False else hh[:, :TB][:, ::-1], data0=av[:, :TB][:, ::-1], data1=bv[:, :TB][:, ::-1], initial=init, op0=ALU.mult, op1=ALU.add),
                                     [kav, kbv, "car"], [("hb2", hk)])
                                lcol = 0
                            CPY("dve", car[:, 0:1], hh[:, lcol:lcol + 1], [("hb2", hk), "car"], ["car"])
                            first = False
                            if need_y:
                                if d == 0:
                                    DMA("sp", FB[s][rowsl, t0:t0 + TB], hh[:, :TB], [("hb2", hk)], [("fb", s)])
                                else:
                                    DMA("pool", FB[s][rowsl, t0:t0 + TB], hh[:, :TB], [("hb2", hk)], [("fb", s)], accum_op=ALU.add)
                P.barrier()
            A.reset()
            wo = A.alloc([128, 8, D], BF16)
            bout = A.alloc([128, D])
            wv = lru_w_out.rearrange("(a p) n -> p a n", p=128)
            for c in range(8):
                DMA("pool", wo[:, c, :], wv[:, c, :], (), ["wo"])
            load_bcast(bout, lru_b_out, "bout")
            rr = A.alloc([128, 8, 512]); gg = A.alloc([128, 8, 512])
            zT = [A.alloc([128, 8, 512], BF16) for _ in range(2)]
            ysb = [A.alloc([128, D]) for _ in range(2)]
            base = A.off
            nb = 0
            ny = 0
            for s in (strs if ctx_out else ["l"]):
                A.off = base
                pm = pm_setup(L, s)
                Ts = TS[s]
                BW = min(512, Ts)
                fbv = FB[s].rearrange("(a p) t -> p a t", p=128)
                fcv = FC[s].rearrange("(a p) t -> p a t", p=128)
                for blk in range(Ts // BW):
                    q = nb % 2
                    nb += 1
                    DMA("sp", rr[:, :, :BW], fbv[:, :, blk * BW:(blk + 1) * BW], (), ["rr"])
                    DMA("sp", gg[:, :, :BW], fcv[:, :, blk * BW:(blk + 1) * BW], (), ["gg"])
                    TT("dve", zT[q][:, :, :BW], rr[:, :, :BW], gg[:, :, :BW], ALU.mult, ["rr", "gg"], [("zT", q)])
                    for ti in range(BW // 128):
                        i = blk * (BW // 128) + ti
                        yk = ny % 2
                        ny += 1
                        for nh in range(2):
                            b = 4 + nh
                            for c in range(8):
                                MM(PS[b], zT[q][:, c, ti * 128:(ti + 1) * 128], wo[:, c, nh * 512:(nh + 1) * 512], c == 0, c == 7, [("zT", q), "wo"], [psk(b)])
                            TT("dve", ysb[yk][:, nh * 512:(nh + 1) * 512], PS[b], bout[:, nh * 512:(nh + 1) * 512], ALU.add, [psk(b), "bout"], [("ysb", yk)])
                        post_mixer(pm, s, i, ysb[yk], ("ysb", yk))
            P.barrier()

        def moe(L, strs):
            def route(s):
                A.reset()
                cap = CAPS[s]
                aff, nt, tokid = RCFG.get(s, (AFF[s], NTS[s], tokid_std))
                lo = A.alloc([128, NE]); hi = A.alloc([128, NE]); mid = A.alloc([128, NE]); tq = A.alloc([128, NE])
                ge = A.alloc([128, NE], I32); nge = A.alloc([128, NE], I32)
                cnt = A.alloc([128, NE])
                cmp_ = A.alloc([128, NE, nt])
                R = "rt"
                MSET("dve", lo, 0.0, [R]); MSET("dve", hi, 1.0, [R]); MSET("dve", mid, 0.5, [R])
                for it in range(34):
                    TT("dve", cmp_, aff, mid.rearrange("p (e o) -> p e o", o=1).to_broadcast([128, NE, nt]), ALU.is_gt, [R, ("aff", s)], [R])
                    P.op("dve", lambda e: e.reduce_sum(out=cnt, in_=cmp_, axis=AX.X), [R], [R])
                    MM(PS[0][:, 0:NE], ones, cnt, True, True, ["ones", R], [psk(0)])
                    TSC("dve", ge, PS[0][:, 0:NE], cap - 0.5, ALU.is_gt, [psk(0), R], [R])
                    TSC("dve", nge, PS[0][:, 0:NE], cap - 0.5, ALU.is_lt, [psk(0), R], [R])
                    P.op("dve", lambda e: e.copy_predicated(lo, ge, mid), [R], [R])
                    P.op("dve", lambda e: e.copy_predicated(hi, nge, mid), [R], [R])
                    TSC("dve", tq, lo, 0.5, ALU.mult, [R], [R])
                    STT("dve", mid, hi, 0.5, tq, ALU.mult, ALU.add, [R], [R])
                sel = cmp_
                TT("dve", sel, aff, lo.rearrange("p (e o) -> p e o", o=1).to_broadcast([128, NE, nt]), ALU.is_gt, [R, ("aff", s)], [R])
                onesb = A.alloc([128, NE * nt]); csum = A.alloc([128, NE, nt])
                base_ = A.alloc([128, NE]); tot = A.alloc([128, NE]); adj = A.alloc([128, NE])
                posf = A.alloc([128, NE, nt]); posi = A.alloc([128, NE, nt], I32)
                pair = A.alloc([128, NE, nt, 2])
                pair_i = pair.bitcast(I32)
                posi2 = posi.rearrange("p e t -> p (e t)")
                pair2 = pair_i.rearrange("p e t c -> p (e t c)")
                MSET("dve", onesb, 1.0, [R])
                P.op("dve", lambda e: e.tensor_tensor_scan(out=csum.rearrange("p e t -> p (e t)"), data0=onesb, data1=sel.rearrange("p e t -> p (e t)"), initial=0.0, op0=ALU.mult, op1=ALU.add), [R], [R])
                MSET("dve", base_[:, 0:1], 0.0, [R])
                CPY("dve", base_[:, 1:NE], csum[:, 0:NE - 1, nt - 1], [R], [R])
                TT("dve", tot, csum[:, :, nt - 1], base_, ALU.subtract, [R], [R])
                MM(PS[1][:, 0:NE], Umat, tot, True, True, ["Umat", R], [psk(1)])
                TT("dve", adj, PS[1][:, 0:NE], base_, ALU.subtract, [psk(1), R], [R])
                TSC("dve", adj, adj, -1.0, ALU.add, [R], [R])
                TT("dve", posf, csum, adj.rearrange("p (e o) -> p e o", o=1).to_broadcast([128, NE, nt]), ALU.add, [R], [R])
                STT("dve", posf, posf, -BIG, sel, ALU.add, ALU.mult, [R], [R])
                TSC("dve", posf, posf, BIG, ALU.add, [R], [R])
                CPY("dve", posi, posf, [R], [R])
                CPY("dve", pair_i[:, :, :, 0], tokid[:, 0:nt].rearrange("p (o t) -> p o t", o=1).to_broadcast([128, NE, nt]), [R, "tokid"], [R])
                CPY("dve", pair[:, :, :, 1], aff, [R, ("aff", s)], [R])
                for e_ in range(NE):
                    for jt in range(nt):
                        P.dma("pool", lambda e, e_=e_, jt=jt: e.indirect_dma_start(
                            out=LST[s][e_][:, :], out_offset=bass.IndirectOffsetOnAxis(ap=posi2[:, e_ * nt + jt:e_ * nt + jt + 1], axis=0),
                            in_=pair2[:, 2 * (e_ * nt + jt):2 * (e_ * nt + jt) + 2], in_offset=None, bounds_check=getreg(e, cap - 1), oob_is_err=False),
                            [R], [("lst", s, e_, jt)])
                P.barrier()
            for s in strs:
                route(s)
            A.reset()
            NSLOT = 6
            W = [A.alloc([128, 8, D], BF16) for _ in range(NSLOT)]
            has_c = "c" in strs
            NCOL = CAP + (CAPC if has_c else 0)
            xs = A.alloc([128, 8, D], BF16)
            xsc = A.alloc([128, D], BF16)
            xsT = A.alloc([128, 8, NCOL], BF16)
            hT = A.alloc([128, 8, NCOL], BF16)
            ysb = [A.alloc([128, D]) for _ in range(2)]
            sa = [A.alloc([128, 512]) for _ in range(2)]
            G2 = {}
            lt = {}
            for s in strs:
                G2[s] = A.alloc([128, D])
                load_bcast(G2[s], modd[SIDX[s], MOD_G2], ("g2", s))
                lt[s] = [A.alloc([128, 8, 2], I32) for _ in range(2)]
            wsrc = (moe_w1, moe_w3, moe_w2)
            tiles = [("l", jt, min(CAP, 128), jt * 128) for jt in range((CAP + 127) // 128)]
            if has_c:
                tiles.append(("c", 0, CAPC, CAP))

            def xs_of(s, jt):
                return xs[:, jt, :] if s == "l" else xsc

            def load_w(e_):
                for m in range(3):
                    slot = (3 * e_ + m) % NSLOT
                    wv = wsrc[m][L, e_].rearrange("(a p) n -> p a n", p=128)
                    for c in range(8):
                        DMA("pool", W[slot][:, c, :], wv[:, c, :], (), [("W", slot)])

            def gathers(e_):
                for s in strs:
                    cap = CAPS[s]
                    rows = min(cap, 128)
                    ntile = (cap + 127) // 128
                    DMA("sp", lt[s][e_ % 2][:rows, :ntile, :], LST[s][e_].rearrange("(j p) c -> p j c", p=rows), (), [("lt", s, e_ % 2)])
                for (s, jt, rows, c0) in tiles:
                    P.dma("pool", lambda e, s=s, jt=jt, rows=rows, e_=e_: e.indirect_dma_start(
                        out=xs_of(s, jt)[:rows, :], out_offset=None, in_=MB[s],
                        in_offset=bass.IndirectOffsetOnAxis(ap=lt[s][e_ % 2][:rows, jt, 0:1], axis=0),
                        bounds_check=getreg(e, TS[s] - 1), oob_is_err=False),
                        [("lt", s, e_ % 2)], ["INDIRECT", ("xs", s, jt)])

            load_w(0)
            gathers(0)
            load_w(1)
            ny = 0
            nsa = 0
            ntr = 0
            for e_ in range(NE):
                w1, w3, w2 = (3 * e_) % NSLOT, (3 * e_ + 1) % NSLOT, (3 * e_ + 2) % NSLOT
                for (s, jt, rows, c0) in tiles:
                    src = xs_of(s, jt)
                    for g in range(2):
                        b = ntr % 2
                        ntr += 1
                        pb16 = PS[b].bitcast(BF16)
                        for j in range(4):
                            c = g * 4 + j
                            P.op("pe", lambda e, c=c, j=j, pb16=pb16, src=src, rows=rows: e.transpose(
                                out=pb16[:, j * 128:j * 128 + rows], in_=src[:rows, c * 128:(c + 1) * 128], identity=identb[:rows, :rows]),
                                [("xs", s, jt), "identb"], [psk(b)])
                        CPY("act" if ntr % 2 else "dve", xsT[:, g * 4:g * 4 + 4, c0:c0 + rows],
                            pb16[:, 0:512].rearrange("p (a b) -> p a b", a=4)[:, :, :rows], [psk(b)], ["xsT"])
                if e_ + 1 < NE:
                    gathers(e_ + 1)
                for fc in range(8):
                    for hb_ in range((NCOL + 511) // 512):
                        w = min(512, NCOL - hb_ * 512)
                        cs = slice(hb_ * 512, hb_ * 512 + w)
                        q = nsa % 2
                        nsa += 1
                        pa, pb_ = 2 + q * 2, 3 + q * 2
                        for c in range(8):
                            MM(PS[pa][:, :w], W[w1][:, c, fc * 128:(fc + 1) * 128], xsT[:, c, cs], c == 0, c == 7, [("W", w1), "xsT"], [psk(pa)])
                        for c in range(8):
                            MM(PS[pb_][:, :w], W[w3][:, c, fc * 128:(fc + 1) * 128], xsT[:, c, cs], c == 0, c == 7, [("W", w3), "xsT"], [psk(pb_)])
                        ACT(sa[q][:, :w], PS[pa][:, :w], AF.Silu, [psk(pa)], [("sa", q)])
                        TT("dve", hT[:, fc, cs], sa[q][:, :w], PS[pb_][:, :w], ALU.mult, [("sa", q), psk(pb_)], ["hT"])
                for (s, jt, rows, c0) in tiles:
                    yk = ny % 2
                    ny += 1
                    ltf = lt[s][e_ % 2].bitcast(F32)
                    for nh in range(2):
                        b = 6 + nh
                        for fc in range(8):
                            MM(PS[b][:rows, :], hT[:, fc, c0:c0 + rows], W[w2][:, fc, nh * 512:(nh + 1) * 512], fc == 0, fc == 7, ["hT", ("W", w2)], [psk(b)])
                        STT("dve", ysb[yk][:rows, nh * 512:(nh + 1) * 512], PS[b][:rows, :], ltf[:rows, jt, 1:2], G2[s][:rows, nh * 512:(nh + 1) * 512],
                            ALU.mult, ALU.mult, [psk(b), ("lt", s, e_ % 2), ("g2", s)], [("ysbm", yk)])
                    P.dma("pool", lambda e, s=s, jt=jt, rows=rows, yk=yk, e_=e_: e.indirect_dma_start(
                        out=XS[s], out_offset=bass.IndirectOffsetOnAxis(ap=lt[s][e_ % 2][:rows, jt, 0:1], axis=0),
                        in_=ysb[yk][:rows, :], in_offset=None, compute_op=ALU.add, bounds_check=getreg(e, TS[s] - 1), oob_is_err=True),
                        [("ysbm", yk), ("lt", s, e_ % 2)], ["INDIRECT", ("xsd", s)])
                if e_ + 2 < NE:
                    load_w(e_ + 2)
            P.barrier()


        A.reset()
        xt = [A.alloc([128, D]) for _ in range(2)]
        om = A.alloc([128, 256]); posc = A.alloc([128, 512])
        prow = [A.alloc([128, 512]) for _ in range(2)]
        arg = A.alloc([128, 256]); tq_ = A.alloc([128, 256]); tqi = A.alloc([128, 256], I32)
        pf = A.alloc([128, 1]); phi = A.alloc([128, 1]); colv = A.alloc([128, 1]); rowv = A.alloc([128, 2])
        TWO_PI0 = 2.0 * math.pi
        G0 = "st0"
        P.op("pool", lambda e: e.iota(om, pattern=[[1, 256]], base=0, channel_multiplier=0, allow_small_or_imprecise_dtypes=True), (), [G0])
        P.op("pool", lambda e: e.iota(pf, pattern=[[0, 1]], base=0, channel_multiplier=1, allow_small_or_imprecise_dtypes=True), (), [G0])
        ACT(om, om, AF.Exp, [G0], [G0], scale=-math.log(10000.0) / 256.0)
        TSC("dve", phi, pf, 63.5, ALU.is_gt, [G0], [G0])
        STT("dve", colv, phi, -64.0, pf, ALU.mult, ALU.add, [G0], [G0])

        def sincos(dst, scal, kd):
            TSC("dve", arg, om, scal, ALU.mult, [G0, ("rowv", 0), ("rowv", 1)], [G0])
            TSC("dve", tq_, arg, 1.0 / TWO_PI0, ALU.mult, [G0], [G0])
            CPY("dve", tqi, tq_, [G0], [G0])
            CPY("dve", tq_, tqi, [G0], [G0])
            STT("dve", arg, tq_, -TWO_PI0, arg, ALU.mult, ALU.add, [G0], [G0])
            TSC("dve", tq_, arg, math.pi, ALU.is_gt, [G0], [G0])
            STT("dve", arg, tq_, -TWO_PI0, arg, ALU.mult, ALU.add, [G0], [G0])
            TSC("dve", tq_, arg, -math.pi, ALU.is_lt, [G0], [G0])
            STT("dve", arg, tq_, TWO_PI0, arg, ALU.mult, ALU.add, [G0], [G0])
            ACT(dst[:, 0:256], arg, AF.Sin, [G0], [kd])
            TSC("dve", arg, arg, math.pi / 2, ALU.add, [G0, kd], [G0])
            TSC("dve", tq_, arg, math.pi, ALU.is_gt, [G0], [G0])
            STT("dve", arg, tq_, -TWO_PI0, arg, ALU.mult, ALU.add, [G0], [G0])
            ACT(dst[:, 256:512], arg, AF.Sin, [G0], [kd])

        sincos(posc, colv[:, 0:1], "posc")
        for i in range(NT):
            k = i % 2
            DMA("sp", xt[k], x_in[i * 128:(i + 1) * 128, :], (), [("xt", k)])
            TSC("dve", rowv[:, k:k + 1], phi, float(2 * i), ALU.add, [G0], [("rowv", k)])
            sincos(prow[k], rowv[:, k:k + 1], ("prow", k))
            TT("dve", xt[k][:, 0:512], xt[k][:, 0:512], prow[k], ALU.add, [("xt", k), ("prow", k)], [("xt", k)])
            TT("dve", xt[k][:, 512:1024], xt[k][:, 512:1024], posc, ALU.add, [("xt", k), "posc"], [("xt", k)])
            DMA("sp", XS["l"][i * 128:(i + 1) * 128, :], xt[k], [("xt", k)], [("xs", "l", i)])
        for i in range(NTC):
            k = i % 2
            DMA("sp", xt[k], ctx_in[i * 128:(i + 1) * 128, :], (), [("xt", k)])
            DMA("sp", XS["c"][i * 128:(i + 1) * 128, :], xt[k], [("xt", k)], [("xs", "c", i)])
        P.barrier()

        for L in range(nlayers):
            kind = L % 3
            j = L // 3
            ctx_inL = L <= 2
            ctx_out = L < 2
            strs = ["l"] + (["c"] if ctx_inL else [])
            modulation(L)
            if kind == 0:
                conv_mixer(L, j, ["l"] + (["c"] if ctx_out else []))
            elif kind == 1:
                s5_mixer2(L, strs, ctx_out)
            else:
                lru_mixer(L, strs, ctx_out)
            moe(L, ["l"] + (["c"] if ctx_out else []))
            RCFG.clear()

        A.reset()
        fg = A.alloc([128, D])
        load_bcast(fg, final_g, "modb")
        xt = [A.alloc([128, D]) for _ in range(2)]
        ho = [A.alloc([128, D]) for _ in range(2)]
        ss = A.alloc([128, 2])
        for i in range(NT):
            k = i % 2
            DMA("sp", xt[k], XS["l"][i * 128:(i + 1) * 128, :], (), [("xt", k)])
            norm_mod(xt[k], fg, None, ho[k], ss[:, k:k + 1], ("xt", k), ("h", k), ("ss", k))
            DMA("sp", out[i * 128:(i + 1) * 128, :], ho[k], [("h", k)], [("out", i)])
        P.barrier()
        P.build()
    return nc, P


def _pp(v):
    v = np.asarray(v)
    return np.ascontiguousarray(v.reshape(-1, 128).T)


def _grid_pos(n, d):
    rows = n // 64
    quarter = d // 4
    omega = (1.0 / (10000.0 ** (np.arange(quarter, dtype=np.float32) / np.float32(quarter)))).astype(np.float32)
    r = np.arange(rows, dtype=np.float32)[:, None] * omega
    cc = np.arange(64, dtype=np.float32)[:, None] * omega
    row_emb = np.concatenate([np.sin(r), np.cos(r)], axis=-1)
    col_emb = np.concatenate([np.sin(cc), np.cos(cc)], axis=-1)
    emb = np.concatenate([np.broadcast_to(row_emb[:, None, :], (rows, 64, d // 2)),
                          np.broadcast_to(col_emb[None, :, :], (rows, 64, d // 2))], axis=-1)
    return np.ascontiguousarray(emb.reshape(rows * 64, d).astype(np.float32))


def prepare_shared(inp):
    f = lambda k: np.ascontiguousarray(np.asarray(inp[k], dtype=np.float32))
    sh = {}
    for k in ("w_mod", "b_mod", "g_mix", "g_ffn", "final_g", "moe_router", "moe_w1", "moe_w3", "moe_w2",
              "conv_w_in", "conv_w_out", "conv_b_out", "lru_w_y", "lru_w_x", "lru_w_out", "lru_b_out"):
        a = f(k)
        if k.startswith("lru_") :
            a = a[0]
        sh[k] = np.ascontiguousarray(a)
    na = f("conv_b_in").shape[0]
    def pad2(a):
        if a.shape[0] == 2:
            return a
        return np.ascontiguousarray(np.concatenate([a, a], axis=0)[:2])
    sh["conv_w_in"] = pad2(sh["conv_w_in"]); sh["conv_w_out"] = pad2(sh["conv_w_out"]); sh["conv_b_out"] = pad2(sh["conv_b_out"])
    sh["conv_b_in_pp"] = pad2(np.stack([_pp(v) for v in f("conv_b_in")]))
    sh["conv_dw_pp"] = pad2(np.stack([np.ascontiguousarray(w.T.reshape(8, 128, 31).transpose(1, 0, 2)) for w in f("conv_dw")]))
    sh["conv_dw_b_pp"] = pad2(np.stack([_pp(v) for v in f("conv_dw_b")]))
    sh["conv_ln_g_pp"] = pad2(np.stack([_pp(v) for v in f("conv_ln_g")]))
    sh["conv_ln_b_pp"] = pad2(np.stack([_pp(v) for v in f("conv_ln_b")]))
    lam_re = f("s5_lam_re")[0]; lam_im = f("s5_lam_im")[0]; log_dt = f("s5_log_dt")[0]
    sh["s5_lam_re"] = np.stack([_pp(lam_re[d].reshape(-1)) for d in range(2)])
    sh["s5_lam_im"] = np.stack([_pp(lam_im[d].reshape(-1)) for d in range(2)])
    sh["s5_log_dt"] = np.stack([_pp(np.repeat(log_dt[d], 64)) for d in range(2)])
    b_re = f("s5_b_re")[0]; b_im = f("s5_b_im")[0]; c_re = f("s5_c_re")[0]; c_im = f("s5_c_im")[0]
    def blockB(b):
        o = np.zeros((2, 32, 128, 128), np.float32)
        for g in range(64):
            it, gl = g // 2, g % 2
            o[:, it, gl * 64:(gl + 1) * 64, (g % 8) * 16:(g % 8) * 16 + 16] = b[:, g]
        return o
    def blockC(c):
        o = np.zeros((2, 32, 128, 128), np.float32)
        for g in range(64):
            it, gl = g // 2, g % 2
            o[:, it, gl * 64:(gl + 1) * 64, (g % 8) * 16:(g % 8) * 16 + 16] = c[:, g].transpose(0, 2, 1)
        return o
    sh["s5_bw_re"] = blockB(b_re); sh["s5_bw_im"] = blockB(b_im)
    sh["s5_cw_re"] = blockC(c_re); sh["s5_cw_im"] = blockC(c_im)
    sh["s5_d_pp"] = _pp(f("s5_d")[0])
    sh["s5_bn_re"] = np.ascontiguousarray(b_re.reshape(2, 32, 128, 16)); sh["s5_bn_im"] = np.ascontiguousarray(b_im.reshape(2, 32, 128, 16))
    sh["s5_cn_re"] = np.ascontiguousarray(c_re.transpose(0, 1, 3, 2).reshape(2, 32, 128, 16)); sh["s5_cn_im"] = np.ascontiguousarray(c_im.transpose(0, 1, 3, 2).reshape(2, 32, 128, 16))
    sh["s5_d_rep"] = np.ascontiguousarray(np.tile(f("s5_d")[0].reshape(64, 1, 16), (1, 8, 1)).reshape(64, 128).T)
    sh["s5_w_glu"] = f("s5_w_glu")[0]
    sh["s5_b_glu"] = f("s5_b_glu")[0]
    sh["lru_b_y_pp"] = _pp(f("lru_b_y")[0]); sh["lru_b_x_pp"] = _pp(f("lru_b_x")[0])
    sh["lru_conv_w_pp"] = np.ascontiguousarray(f("lru_conv_w")[0].T.reshape(8, 128, 4).transpose(1, 0, 2))
    sh["lru_conv_b_pp"] = _pp(f("lru_conv_b")[0])
    sh["lru_w_a"] = f("lru_w_a")[0]; sh["lru_w_i"] = f("lru_w_i")[0]
    sh["lru_b_a_pp"] = np.stack([_pp(v) for v in f("lru_b_a")[0]])
    sh["lru_b_i_pp"] = np.stack([_pp(v) for v in f("lru_b_i")[0]])
    sh["lru_lam_pp"] = np.stack([_pp(v) for v in f("lru_lam")[0]])
    return sh


def run_module(inp, nlayers=4):
    x = np.asarray(inp["x"], dtype=np.float32)
    B, T, _ = x.shape
    ctx = np.asarray(inp["ctx"], dtype=np.float32)
    TC = ctx.shape[1]
    c = np.asarray(inp["c"], dtype=np.float32)
    c_ctx = np.asarray(inp["c_ctx"], dtype=np.float32)
    sh = prepare_shared(inp)
    nc, P = build_program(T, TC, nlayers)
    in_maps = []
    for b in range(B):
        m = dict(sh)
        m["x"] = np.ascontiguousarray(x[b])
        m["ctx"] = np.ascontiguousarray(ctx[b])
        m["c2T"] = np.ascontiguousarray(np.stack([_pp(c[b]), _pp(c_ctx)], axis=-1))
        in_maps.append(m)
    res = run_bass_kernel_spmd(nc, in_maps, core_ids=list(range(B)))
    return np.stack([np.asarray(r["out"]) for r in res.results], axis=0).astype(np.float32)


def kernel(**inputs):
    return run_module(inputs, 4)
```

```python
import math
import contextlib
import numpy as np
import concourse.bass as bass
import concourse.mybir as mybir
from concourse.bass_utils import run_bass_kernel_spmd

F32 = mybir.dt.float32
BF16 = mybir.dt.bfloat16
I32 = mybir.dt.int32
AF = mybir.ActivationFunctionType
ALU = mybir.AluOpType
AX = mybir.AxisListType

D = 1024
NE = 16
EPS = 1e-6
ENGS = ("pe", "act", "dve", "pool", "sp")
NDMASEM = 24
BIG = 1.0e6


class Prog:
    def __init__(self, nc):
        self.nc = nc
        self.streams = {e: [] for e in ENGS}
        self.cnt = {e: 0 for e in ENGS}
        self.known = {e: {} for e in ENGS}
        self.last_w = {}
        self.reads = {}
        self.dma_cnt = [0] * NDMASEM
        self.dma_rr = 0
        self.n_ops = 0

    def _deps(self, eng, reads, writes):
        need = {}

        def add(dep):
            if dep is None:
                return
            k, v = dep
            if need.get(k, 0) < v:
                need[k] = v

        for r in reads:
            add(self.last_w.get(r))
        for w in writes:
            add(self.last_w.get(w))
            for k, v in self.reads.get(w, {}).items():
                add((k, v))
        out = []
        kn = self.known[eng]
        for k, v in need.items():
            if k == "pe" and eng == "pe":
                continue
            if kn.get(k, 0) >= v:
                continue
            kn[k] = v
            out.append((k, v))
        return out

    def _mark(self, tag, reads, writes):
        for r in reads:
            d = self.reads.setdefault(r, {})
            if d.get(tag[0], 0) < tag[1]:
                d[tag[0]] = tag[1]
        for w in writes:
            self.last_w[w] = tag
            self.reads[w] = {}

    def op(self, eng, fn, reads=(), writes=()):
        waits = self._deps(eng, reads, writes)
        self.cnt[eng] += 1
        tag = (eng, self.cnt[eng])
        self.streams[eng].append((fn, waits, (eng, 1)))
        self._mark(tag, reads, writes)
        self.n_ops += 1

    def dma(self, eng, fn, reads=(), writes=()):
        j = self.dma_rr
        self.dma_rr = (self.dma_rr + 1) % NDMASEM
        k = ("dma", j)
        waits = self._deps(eng, reads, writes)
        prev = self.dma_cnt[j]
        if prev > 0 and self.known[eng].get(k, 0) < prev:
            self.known[eng][k] = prev
            waits.append((k, prev))
        self.dma_cnt[j] += 16
        tag = (k, self.dma_cnt[j])
        self.streams[eng].append((fn, waits, (k, 16)))
        self._mark(tag, reads, writes)
        self.n_ops += 1

    def barrier(self):
        for eng in ENGS:
            waits = []
            for e in ENGS:
                if self.cnt[e] > 0 and e != eng and self.known[eng].get(e, 0) < self.cnt[e]:
                    waits.append((e, self.cnt[e]))
                    self.known[eng][e] = self.cnt[e]
            for j in range(NDMASEM):
                k = ("dma", j)
                if self.dma_cnt[j] > 0 and self.known[eng].get(k, 0) < self.dma_cnt[j]:
                    waits.append((k, self.dma_cnt[j]))
                    self.known[eng][k] = self.dma_cnt[j]
            if waits:
                self.streams[eng].append((None, waits, None))
        self.last_w = {}
        self.reads = {}

    def build(self):
        nc = self.nc
        with contextlib.ExitStack() as st:
            sems = {}
            for e in ENGS:
                sems[e] = st.enter_context(nc.semaphore("s_" + e))
            for j in range(NDMASEM):
                sems[("dma", j)] = st.enter_context(nc.semaphore("s_dma%d" % j))
            block = st.enter_context(nc.Block())

            def runner(ename):
                def run(engine):
                    for fn, waits, inc in self.streams[ename]:
                        for k, v in waits:
                            engine.wait_ge(sems[k], v)
                        if fn is not None:
                            ins = fn(engine)
                            ins.then_inc(sems[inc[0]], inc[1])
                return run

            block.tensor(runner("pe"))
            block.scalar(runner("act"))
            block.vector(runner("dve"))
            block.gpsimd(runner("pool"))
            block.sync(runner("sp"))


class Arena:
    def __init__(self, ap, n):
        self.ap = ap
        self.n = n
        self.off = 0
        self.base = 0
        self.uid = 0

    def alloc(self, shape, dt=F32):
        ne = 1
        for s in shape[1:]:
            ne *= s
        words = ne if dt in (F32, I32) else (ne + 1) // 2
        a = self.ap[:, self.off:self.off + words]
        self.off += words
        assert self.off <= self.n, ("arena overflow", self.off, self.n)
        if dt != F32:
            a = a.bitcast(dt)
        if len(shape) == 3:
            a = a.rearrange("p (a b) -> p a b", a=shape[1])
        elif len(shape) == 4:
            a = a.rearrange("p (a b c) -> p a b c", a=shape[1], b=shape[2])
        return a

    def mark(self):
        self.base = self.off

    def reset(self):
        self.off = self.base


def build_program(T, TC, nlayers=4):
    NT = T // 128
    NTC = TC // 128
    CAP = 2 * T // NE
    CAPC = 2 * TC // NE
    nc = bass.Bass("TRN2", target_bir_lowering=False)

    def din(name, shape, dt=F32):
        return nc.dram_tensor(name, list(shape), dt, kind="ExternalInput").ap()

    def dscr(name, shape, dt=F32):
        return nc.dram_tensor(name, list(shape), dt, kind="Internal").ap()

    x_in = din("x", [T, D])
    ctx_in = din("ctx", [TC, D])
    c2T = din("c2T", [128, 8, 2])
    w_mod = din("w_mod", [4, D, 6 * D])
    b_mod = din("b_mod", [4, 6 * D])
    g_mix = din("g_mix", [4, D])
    g_ffn = din("g_ffn", [4, D])
    final_g = din("final_g", [D])
    moe_router = din("moe_router", [4, D, NE])
    moe_w1 = din("moe_w1", [4, NE, D, D])
    moe_w3 = din("moe_w3", [4, NE, D, D])
    moe_w2 = din("moe_w2", [4, NE, D, D])
    conv_w_in = din("conv_w_in", [2, D, 2 * D])
    conv_b_in_pp = din("conv_b_in_pp", [2, 128, 16])
    conv_dw_pp = din("conv_dw_pp", [2, 128, 8, 31])
    conv_dw_b_pp = din("conv_dw_b_pp", [2, 128, 8])
    conv_ln_g_pp = din("conv_ln_g_pp", [2, 128, 8])
    conv_ln_b_pp = din("conv_ln_b_pp", [2, 128, 8])
    conv_w_out = din("conv_w_out", [2, D, D])
    conv_b_out = din("conv_b_out", [2, D])
    s5_lam_re = din("s5_lam_re", [2, 128, 32])
    s5_lam_im = din("s5_lam_im", [2, 128, 32])
    s5_log_dt = din("s5_log_dt", [2, 128, 32])
    s5_bw_re = din("s5_bw_re", [2, 32, 128, 128])
    s5_bw_im = din("s5_bw_im", [2, 32, 128, 128])
    s5_cw_re = din("s5_cw_re", [2, 32, 128, 128])
    s5_cw_im = din("s5_cw_im", [2, 32, 128, 128])
    s5_d_pp = din("s5_d_pp", [128, 8])
    s5_bn_re = din("s5_bn_re", [2, 32, 128, 16])
    s5_bn_im = din("s5_bn_im", [2, 32, 128, 16])
    s5_cn_re = din("s5_cn_re", [2, 32, 128, 16])
    s5_cn_im = din("s5_cn_im", [2, 32, 128, 16])
    s5_d_rep = din("s5_d_rep", [128, 64])
    s5_w_glu = din("s5_w_glu", [D, 2 * D])
    s5_b_glu = din("s5_b_glu", [2 * D])
    lru_w_y = din("lru_w_y", [D, D])
    lru_b_y_pp = din("lru_b_y_pp", [128, 8])
    lru_w_x = din("lru_w_x", [D, D])
    lru_b_x_pp = din("lru_b_x_pp", [128, 8])
    lru_conv_w_pp = din("lru_conv_w_pp", [128, 8, 4])
    lru_conv_b_pp = din("lru_conv_b_pp", [128, 8])
    lru_w_a = din("lru_w_a", [2, 8, 128, 128])
    lru_b_a_pp = din("lru_b_a_pp", [2, 128, 8])
    lru_w_i = din("lru_w_i", [2, 8, 128, 128])
    lru_b_i_pp = din("lru_b_i_pp", [2, 128, 8])
    lru_lam_pp = din("lru_lam_pp", [2, 128, 8])
    lru_w_out = din("lru_w_out", [D, D])
    lru_b_out = din("lru_b_out", [D])
    out = nc.dram_tensor("out", [T, D], F32, kind="ExternalOutput").ap()

    XS = {"l": dscr("xl", [T, D]), "c": dscr("xc", [TC, D])}
    MB = {"l": dscr("mbl", [T, D], BF16), "c": dscr("mbc", [TC, D], BF16)}
    LST = {"l": [dscr("lstl%d" % e, [CAP, 2], I32) for e in range(NE)], "c": [dscr("lstc%d" % e, [CAPC, 2], I32) for e in range(NE)]}
    FA = {"l": dscr("fal", [D, T]), "c": dscr("fac", [D, TC])}
    FB = {"l": dscr("fbl", [D, T]), "c": dscr("fbc", [D, TC])}
    FA16 = {"l": dscr("fa16l", [D, T], BF16), "c": dscr("fa16c", [D, TC], BF16)}
    FC = {"l": dscr("fcl", [D, T]), "c": dscr("fcc", [D, TC])}
    modd = dscr("modd", [2, 6, D])
    TS = {"l": T, "c": TC}
    NTS = {"l": NT, "c": NTC}
    CAPS = {"l": CAP, "c": CAPC}

    st = contextlib.ExitStack()
    with st:
        NAR = 52600
        arena_t = st.enter_context(nc.sbuf_tensor("arena", [128, NAR], F32))[:, :]
        PS = [st.enter_context(nc.psum_tensor("ps%d" % i, [128, 512], F32))[:, :] for i in range(8)]
        A = Arena(arena_t, NAR)
        P = Prog(nc)

        def TT(eng, o, a, b, op, r, w):
            P.op(eng, lambda e: e.tensor_tensor(out=o, in0=a, in1=b, op=op), r, w)

        def TSC(eng, o, a, s1, op0, r, w, s2=None, op1=None):
            if op1 is None:
                P.op(eng, lambda e: e.tensor_scalar(out=o, in0=a, scalar1=s1, scalar2=None, op0=op0), r, w)
            else:
                P.op(eng, lambda e: e.tensor_scalar(out=o, in0=a, scalar1=s1, scalar2=s2, op0=op0, op1=op1), r, w)

        def STT(eng, o, a, s, b, op0, op1, r, w):
            P.op(eng, lambda e: e.scalar_tensor_tensor(out=o, in0=a, scalar=s, in1=b, op0=op0, op1=op1), r, w)

        def ACT(o, i, func, r, w, bias=None, scale=1.0, accum=None):
            kw = {}
            if bias is not None:
                kw["bias"] = bias
            if accum is not None:
                kw["accum_out"] = accum
            P.op("act", lambda e: e.activation(out=o, in_=i, func=func, scale=scale, **kw), r, w)

        def CPY(eng, o, i, r, w):
            if eng == "act":
                P.op("act", lambda e: e.activation(out=o, in_=i, func=AF.Copy), r, w)
            else:
                P.op(eng, lambda e: e.tensor_copy(out=o, in_=i), r, w)

        def MSET(eng, o, v, w):
            P.op(eng, lambda e: e.memset(o, v), (), w)

        def MM(o, l, rh, start, stop, r, w):
            P.op("pe", lambda e: e.matmul(o, lhsT=l, rhs=rh, start=start, stop=stop), r, w)

        def DMA(q, o, i, r, w, **kw):
            P.dma(q, lambda e: e.dma_start(out=o, in_=i, **kw), r, w)

        def RECIP(o, i, r, w):
            P.op("dve", lambda e: e.reciprocal(out=o, in_=i), r, w)

        psk = lambda k: ("ps", k)
        _regs = {}

        def getreg(e, v):
            if v not in _regs:
                _regs[v] = e.to_reg(v)
            return _regs[v]

        ident = A.alloc([128, 128])
        ones = A.alloc([128, 128])
        Umat = A.alloc([128, 128])
        tokid_std = A.alloc([128, max(NT, NTC)], I32)
        tokid_l5 = A.alloc([128, NT], I32)
        tokid_c5 = A.alloc([128, 8], I32)
        AFFC5 = A.alloc([128, NE, 8])
        rmA = A.alloc([128, 1]); rmB = A.alloc([128, 1])
        RCFG = {}
        AFF = {"l": A.alloc([128, NE, NT]), "c": A.alloc([128, NE, NTC])}
        wr_t = A.alloc([128, 8, NE])
        csil = A.alloc([128, 8, 2])
        identb = A.alloc([128, 128], BF16)
        A.mark()
        MSET("pool", ones, 1.0, ["ones"])
        MSET("pool", ident, 1.0, ["ident"])
        P.op("pool", lambda e: e.affine_select(out=ident, in_=ident, pattern=[[-1, 128]], compare_op=ALU.is_equal,
                                               fill=0.0, base=0, channel_multiplier=1), ["ident"], ["ident"])
        MSET("pool", Umat, 1.0, ["Umat"])
        P.op("pool", lambda e: e.affine_select(out=Umat, in_=Umat, pattern=[[1, 128]], compare_op=ALU.is_gt,
                                               fill=0.0, base=0, channel_multiplier=-1), ["Umat"], ["Umat"])
        P.op("pool", lambda e: e.iota(tokid_std, pattern=[[128, max(NT, NTC)]], base=0, channel_multiplier=1), (), ["tokid"])
        P.op("pool", lambda e: e.iota(tokid_l5, pattern=[[1024, NT // 8], [1, 8]], base=0, channel_multiplier=8), (), ["tokid"])
        P.op("pool", lambda e: e.iota(tokid_c5, pattern=[[1, 8]], base=0, channel_multiplier=8), (), ["tokid"])
        MSET("pool", rmA, 1.0, ["rm"])
        P.op("pool", lambda e: e.affine_select(out=rmA, in_=rmA, pattern=[[0, 1]], compare_op=ALU.is_ge, fill=0.0, base=63, channel_multiplier=-1), ["rm"], ["rm"])
        TSC("pool", rmB, rmA, -1.0, ALU.mult, ["rm"], ["rm"], s2=1.0, op1=ALU.add)
        CPY("dve", identb, ident, ["ident"], ["identb"])
        DMA("sp", csil, c2T, (), ["csil"])
        ACT(csil, csil, AF.Silu, ["csil"], ["csil"])
        P.barrier()

        def transposes_to(src, rows, dst_fn, src_key, dst_key, nchunks=8, evac="act", pb=(0, 1)):
            for g in range(nchunks // 4):
                b = pb[g % 2]
                for j in range(4):
                    c = g * 4 + j
                    P.op("pe", lambda e, c=c, j=j, b=b: e.transpose(out=PS[b][:, j * 128:j * 128 + rows],
                                                                    in_=src[:rows, c * 128:(c + 1) * 128],
                                                                    identity=ident[:rows, :rows]),
                         [src_key, "ident"], [psk(b)])
                CPY(evac, dst_fn(g * 4, 4), PS[b][:, :].rearrange("p (a b) -> p a b", a=4)[:, :, :rows], [psk(b)], [dst_key])

        def norm_mod(xt, Ab, Bb, ho, ss, kx, kh, kss, rows=128):
            ACT(ho[:rows], xt[:rows], AF.Square, [kx], [kh, kss], accum=ss[:rows])
            TSC("dve", ss[:rows], ss[:rows], 1.0 / D, ALU.mult, [kss], [kss], s2=EPS, op1=ALU.add)
            ACT(ss[:rows], ss[:rows], AF.Sqrt, [kss], [kss])
            RECIP(ss[:rows], ss[:rows], [kss], [kss])
            STT("dve", ho[:rows], xt[:rows], ss[:rows, 0:1], Ab[:rows], ALU.mult, ALU.mult, [kx, kss, "modb"], [kh])
            if Bb is not None:
                TT("dve", ho[:rows], ho[:rows], Bb[:rows], ALU.add, [kh, "modb"], [kh])

        def load_bcast(dst, src_row, key):
            DMA("sp", dst, src_row.partition_broadcast(128), (), [key])

        def modulation(L):
            A.reset()
            wch = [A.alloc([128, 8, 512]) for _ in range(2)]
            brow = A.alloc([1, 6 * D])
            mrow = A.alloc([2, 6 * D])
            gm = A.alloc([2, D])
            gf = A.alloc([2, D])
            DMA("sp", brow[0:1], b_mod[L:L + 1, :], (), ["brow"])
            DMA("sp", gm[0:2], g_mix[L].partition_broadcast(2), (), ["gm"])
            DMA("sp", gf[0:2], g_ffn[L].partition_broadcast(2), (), ["gf"])
            wv = w_mod[L].rearrange("(a p) n -> p a n", p=128)
            for nb in range(12):
                wb = wch[nb % 2]
                DMA("sp", wb, wv[:, :, nb * 512:(nb + 1) * 512], (), [("wch", nb % 2)])
                b = 2 + nb % 2
                for c in range(8):
                    MM(PS[b][0:2, :], csil[:, c, :], wb[:, c, :], c == 0, False, [("wch", nb % 2), "csil"], [psk(b)])
                MM(PS[b][0:2, :], ones[0:1, 0:2], brow[0:1, nb * 512:(nb + 1) * 512], False, True, ["ones", "brow"], [psk(b)])
                CPY("act", mrow[0:2, nb * 512:(nb + 1) * 512], PS[b][0:2, :], [psk(b)], ["mrow"])
            STT("dve", mrow[0:2, D:2 * D], mrow[0:2, D:2 * D], 1.0, gm[0:2], ALU.add, ALU.mult, ["mrow", "gm"], ["mrow"])
            STT("dve", mrow[0:2, 4 * D:5 * D], mrow[0:2, 4 * D:5 * D], 1.0, gf[0:2], ALU.add, ALU.mult, ["mrow", "gf"], ["mrow"])
            DMA("sp", modd.rearrange("s k d -> s (k d)"), mrow[0:2, :], ["mrow"], ["modd"])
            P.barrier()

        MOD_SH1, MOD_A1, MOD_G1, MOD_SH2, MOD_A2, MOD_G2 = range(6)
        SIDX = {"l": 0, "c": 1}

        class PM:
            pass

        def pm_setup(L, s):
            pm = PM()
            pm.G1 = A.alloc([128, D]); pm.A2 = A.alloc([128, D]); pm.B2 = A.alloc([128, D])
            load_bcast(pm.G1, modd[SIDX[s], MOD_G1], "modb")
            load_bcast(pm.A2, modd[SIDX[s], MOD_A2], "modb")
            load_bcast(pm.B2, modd[SIDX[s], MOD_SH2], "modb")
            pm.xt = [A.alloc([128, D]) for _ in range(2)]
            pm.m = [A.alloc([128, D]) for _ in range(2)]
            pm.mT = A.alloc([128, 8, 128])
            pm.mb = [A.alloc([128, D], BF16) for _ in range(2)]
            pm.ss = A.alloc([128, 4])
            pm.ex = A.alloc([128, NE])
            DMA("sp", wr_t, moe_router[L].rearrange("(a p) e -> p a e", p=128), (), ["wr"])
            pm.n = 0
            return pm

        def post_mixer(pm, s, i, y, ykey, xrows=None, mrows=None, rr=128, affcol=None):
            k = pm.n % 2
            pm.n += 1
            xt, m = pm.xt[k], pm.m[k]
            kx, km = ("pmx", k), ("pmm", k)
            if xrows is None:
                rows = slice(i * 128, (i + 1) * 128)
                xrows = XS[s][rows, :]
                mrows = MB[s][rows, :]
            if affcol is None:
                affcol = AFF[s][:, :, i]
            DMA("sp", xt[:rr], xrows, (), [kx])
            TT("dve", y[:rr], y[:rr], pm.G1[:rr], ALU.mult, [ykey, "modb"], [ykey])
            TT("dve", xt[:rr], xt[:rr], y[:rr], ALU.add, [kx, ykey], [kx])
            DMA("sp", xrows, xt[:rr], [kx], [("xs", s, i)])
            norm_mod(xt, pm.A2, pm.B2, m, pm.ss[:, 0:1], kx, km, "pmss", rows=rr)
            CPY("act", pm.mb[k][:rr], m[:rr], [km], [("pmb", k)])
            DMA("sp", mrows, pm.mb[k][:rr], [("pmb", k)], [("ms", s, i)])
            transposes_to(m, rr, lambda c0, n: pm.mT[:, c0:c0 + n, :rr], km, "pmT", evac="act", pb=(0, 1))
            for c in range(8):
                MM(PS[7][:rr, 0:NE], pm.mT[:, c, :rr], wr_t[:, c, :], c == 0, c == 7, ["pmT", "wr"], [psk(7)])
            P.op("dve", lambda e: e.reduce_max(out=pm.ss[:rr, 1:2], in_=PS[7][:rr, 0:NE], axis=AX.X), [psk(7)], ["pmss2"])
            TSC("dve", pm.ss[:rr, 1:2], pm.ss[:rr, 1:2], -1.0, ALU.mult, ["pmss2"], ["pmss2"])
            ACT(pm.ex[:rr], PS[7][:rr, 0:NE], AF.Exp, [psk(7), "pmss2"], ["pmex", "pmss3"], bias=pm.ss[:rr, 1:2], accum=pm.ss[:rr, 2:3])
            RECIP(pm.ss[:rr, 2:3], pm.ss[:rr, 2:3], ["pmss3"], ["pmss3"])
            TSC("dve", affcol[:rr], pm.ex[:rr], pm.ss[:rr, 2:3], ALU.mult, ["pmex", "pmss3"], [("aff", s)])

        def conv_mixer(L, j, strs):
            A.reset()
            win = A.alloc([128, 8, 2 * D], BF16)
            bin_pp = A.alloc([128, 16])
            A1 = {}; B1 = {}
            for s in strs:
                A1[s] = A.alloc([128, D]); B1[s] = A.alloc([128, D])
                load_bcast(A1[s], modd[SIDX[s], MOD_A1], "modb")
                load_bcast(B1[s], modd[SIDX[s], MOD_SH1], "modb")
            xt = [A.alloc([128, D]) for _ in range(2)]
            h = [A.alloc([128, D]) for _ in range(2)]
            ss = A.alloc([128, 2])
            hT = [A.alloc([128, 8, 512], BF16) for _ in range(2)]
            sg = [A.alloc([128, 512]) for _ in range(2)]
            ub = [A.alloc([128, 512], BF16) for _ in range(2)]
            wv = conv_w_in[j].rearrange("(a p) n -> p a n", p=128)
            for c in range(8):
                DMA("pool", win[:, c, :], wv[:, c, :], (), ["win"])
            DMA("sp", bin_pp, conv_b_in_pp[j], (), ["binpp"])
            n = 0
            nb = 0
            for s in strs:
                Ts = TS[s]
                BW = min(512, Ts)
                for blk in range(Ts // BW):
                    hb = hT[nb % 2]
                    for ti in range(BW // 128):
                        i = blk * (BW // 128) + ti
                        k = n % 2
                        n += 1
                        DMA("sp", xt[k], XS[s][i * 128:(i + 1) * 128, :], (), [("xt", k)])
                        norm_mod(xt[k], A1[s], B1[s], h[k], ss[:, k:k + 1], ("xt", k), ("h", k), ("ss", k))
                        transposes_to(h[k], 128, lambda c0, nn, hb=hb, ti=ti: hb[:, c0:c0 + nn, ti * 128:(ti + 1) * 128],
                                      ("h", k), ("hT", nb % 2), evac="act", pb=(0, 1))
                    for fo in range(8):
                        pa, pg = 2 + (fo % 2) * 2, 3 + (fo % 2) * 2
                        for c in range(8):
                            MM(PS[pa][:, :BW], win[:, c, fo * 128:(fo + 1) * 128], hb[:, c, :BW], c == 0, c == 7, ["win", ("hT", nb % 2)], [psk(pa)])
                        for c in range(8):
                            MM(PS[pg][:, :BW], win[:, c, D + fo * 128:D + (fo + 1) * 128], hb[:, c, :BW], c == 0, c == 7, ["win", ("hT", nb % 2)], [psk(pg)])
                        q = fo % 2
                        ACT(sg[q][:, :BW], PS[pg][:, :BW], AF.Sigmoid, [psk(pg), "binpp"], [("sg", q)], bias=bin_pp[:, 8 + fo:9 + fo])
                        STT("dve", ub[q][:, :BW], PS[pa][:, :BW], bin_pp[:, fo:fo + 1], sg[q][:, :BW], ALU.add, ALU.mult, [psk(pa), ("sg", q), "binpp"], [("ub", q)])
                        DMA("sp", FA16[s][fo * 128:(fo + 1) * 128, blk * BW:(blk + 1) * BW], ub[q][:, :BW], [("ub", q)], [("fa", s)])
                    nb += 1
            P.barrier()
            A.reset()
            wout = A.alloc([128, 8, D], BF16)
            lng = A.alloc([128, 8]); lnb = A.alloc([128, 8])
            bout = A.alloc([128, D])
            wv = conv_w_out[j].rearrange("(a p) n -> p a n", p=128)
            for c in range(8):
                DMA("pool", wout[:, c, :], wv[:, c, :], (), ["wout"])
            DMA("sp", lng, conv_ln_g_pp[j], (), ["ln"])
            DMA("sp", lnb, conv_ln_b_pp[j], (), ["ln"])
            load_bcast(bout, conv_b_out[j], "bout")
            dw = A.alloc([128, 8, 31]); dwb = A.alloc([128, 8])
            DMA("sp", dw, conv_dw_pp[j], (), ["dw"])
            DMA("sp", dwb, conv_dw_b_pp[j], (), ["dw"])
            Dg = A.alloc([128, 8 * 31, 128], BF16)
            for c in range(8):
                for jj in range(31):
                    TSC("dve", Dg[:, c * 31 + jj, :], ident, dw[:, c, jj:jj + 1], ALU.mult, ["ident", "dw"], ["Dg"])
            u16 = A.alloc([128, 8, 512 + 30], BF16)
            v = [A.alloc([128, 8, 512])]
            sq = A.alloc([128, 8, 512])
            mean = A.alloc([128, 512]); var = A.alloc([128, 512])
            zT = [A.alloc([128, 8, 512], BF16) for _ in range(2)]
            ysb = [A.alloc([128, D]) for _ in range(2)]
            base = A.off
            nb = 0
            ny = 0
            for s in strs:
                A.off = base
                pm = pm_setup(L, s)
                Ts = TS[s]
                BW = min(512, Ts)
                fbv = FA16[s].rearrange("(a p) t -> p a t", p=128)
                for blk in range(Ts // BW):
                    q = nb % 2
                    nb += 1
                    vv = v[0]
                    kv, kz = ("v", 0), ("zT", q)
                    nblk_ = Ts // BW
                    t0 = blk * BW
                    if blk == 0:
                        MSET("pool", u16[:, :, 0:15], 0.0, ["u16"])
                    else:
                        DMA("sp", u16[:, :, 0:15], fbv[:, :, t0 - 15:t0], (), ["u16"])
                    if blk == nblk_ - 1:
                        MSET("pool", u16[:, :, 15 + BW:30 + BW], 0.0, ["u16"])
                    else:
                        DMA("sp", u16[:, :, 15 + BW:30 + BW], fbv[:, :, t0 + BW:t0 + BW + 15], (), ["u16"])
                    DMA("sp", u16[:, :, 15:15 + BW], fbv[:, :, t0:t0 + BW], (), ["u16"])
                    for c in range(8):
                        pc = (2, 3, 6)[c % 3]
                        for jj in range(31):
                            MM(PS[pc][:, :BW], Dg[:, c * 31 + jj, :], u16[:, c, jj:jj + BW], jj == 0, jj == 30, ["Dg", "u16"], [psk(pc)])
                        ACT(vv[:, c, :BW], PS[pc][:, :BW], AF.Identity, [psk(pc), "dw"], [kv], bias=dwb[:, c:c + 1])
                    ACT(sq[:, :, :BW], vv[:, :, :BW], AF.Square, [kv], ["sq"])
                    for c in range(8):
                        MM(PS[2][:, :BW], ones, vv[:, c, :BW], c == 0, c == 7, ["ones", kv], [psk(2)])
                    for c in range(8):
                        MM(PS[3][:, :BW], ones, sq[:, c, :BW], c == 0, c == 7, ["ones", "sq"], [psk(3)])
                    ACT(mean[:, :BW], PS[2][:, :BW], AF.Copy, [psk(2)], ["mean"], scale=1.0 / D)
                    TT("dve", var[:, :BW], mean[:, :BW], mean[:, :BW], ALU.mult, ["mean"], ["var"])
                    STT("dve", var[:, :BW], PS[3][:, :BW], 1.0 / D, var[:, :BW], ALU.mult, ALU.subtract, [psk(3), "var"], ["var"])
                    TSC("dve", var[:, :BW], var[:, :BW], EPS, ALU.add, ["var"], ["var"])
                    ACT(var[:, :BW], var[:, :BW], AF.Sqrt, ["var"], ["var"])
                    RECIP(var[:, :BW], var[:, :BW], ["var"], ["var"])
                    for c in range(8):
                        eng = "pool" if c % 2 else "dve"
                        TT(eng, sq[:, c, :BW], vv[:, c, :BW], mean[:, :BW], ALU.subtract, [kv, "mean", "sq"], [("sqc", c)])
                        TT(eng, sq[:, c, :BW], sq[:, c, :BW], var[:, :BW], ALU.mult, [("sqc", c), "var"], [("sqc", c)])
                        ACT(zT[q][:, c, :BW], sq[:, c, :BW], AF.Silu, [("sqc", c), "ln"], [kz], bias=lnb[:, c:c + 1], scale=lng[:, c:c + 1])
                    for c in range(8):
                        P.reads.setdefault("sq", {})
                    P.op("act", lambda e: e.activation(out=mean[:, 0:1], in_=mean[:, 0:1], func=AF.Copy), [("sqc", c) for c in range(8)] + ["mean"], ["sq", "mean"])
                    for ti in range(BW // 128):
                        i = blk * (BW // 128) + ti
                        yk = ny % 2
                        ny += 1
                        for nh in range(2):
                            b = 4 + nh
                            for c in range(8):
                                MM(PS[b], zT[q][:, c, ti * 128:(ti + 1) * 128], wout[:, c, nh * 512:(nh + 1) * 512], c == 0, c == 7, [kz, "wout"], [psk(b)])
                            TT("dve", ysb[yk][:, nh * 512:(nh + 1) * 512], PS[b], bout[:, nh * 512:(nh + 1) * 512], ALU.add, [psk(b), "bout"], [("ysb", yk)])
                        post_mixer(pm, s, i, ysb[yk], ("ysb", yk))
            P.barrier()

        def s5_mixer(L, strs, ctx_out):
            A.reset()
            A1 = {}; B1 = {}
            for s in strs:
                A1[s] = A.alloc([128, D]); B1[s] = A.alloc([128, D])
                load_bcast(A1[s], modd[SIDX[s], MOD_A1], "modb")
                load_bcast(B1[s], modd[SIDX[s], MOD_SH1], "modb")
            xt = [A.alloc([128, D]) for _ in range(2)]
            h = [A.alloc([128, D]) for _ in range(2)]
            ss = A.alloc([128, 2])
            hT = [A.alloc([128, 8, 512]) for _ in range(2)]
            n = 0
            nb = 0
            for s in strs:
                Ts = TS[s]
                BW = min(512, Ts)
                fav = FA[s].rearrange("(a p) t -> p a t", p=128)
                for blk in range(Ts // BW):
                    hb = hT[nb % 2]
                    for ti in range(BW // 128):
                        i = blk * (BW // 128) + ti
                        k = n % 2
                        n += 1
                        DMA("sp", xt[k], XS[s][i * 128:(i + 1) * 128, :], (), [("xt", k)])
                        norm_mod(xt[k], A1[s], B1[s], h[k], ss[:, k:k + 1], ("xt", k), ("h", k), ("ss", k))
                        transposes_to(h[k], 128, lambda c0, nn, hb=hb, ti=ti: hb[:, c0:c0 + nn, ti * 128:(ti + 1) * 128],
                                      ("h", k), ("hT", nb % 2), evac="act", pb=(0, 1))
                    DMA("sp", fav[:, :, blk * BW:(blk + 1) * BW], hb[:, :, :BW], [("hT", nb % 2)], [("fa", s)])
                    nb += 1
            P.barrier()
            A.reset()
            TBM = 2048
            NLV = 11
            lre = A.alloc([128, 32]); lim = A.alloc([128, 32]); ldt = A.alloc([128, 32])
            t1 = A.alloc([128, 32]); t2 = A.alloc([128, 32]); t3 = A.alloc([128, 32]); t4 = A.alloc([128, 32])
            ti32 = A.alloc([128, 32], I32)
            cre = A.alloc([128, 32]); cim = A.alloc([128, 32]); ncim = A.alloc([128, 32])
            pwr = A.alloc([128, NLV + 1, 32]); pwi = A.alloc([128, NLV + 1, 32]); npwi = A.alloc([128, NLV + 1, 32])
            bwr = A.alloc([128, 128]); bwi = A.alloc([128, 128])
            mre = A.alloc([128, 128]); mim = A.alloc([128, 128])
            LBr = A.alloc([128, 4, 128]); LBi = A.alloc([128, 4, 128])
            CWr = A.alloc([128, 4, 128]); CWi = A.alloc([128, 4, 128])
            ucb = [A.alloc([128, TBM]) for _ in range(2)]
            KA = [A.alloc([128, TBM]), A.alloc([128, TBM])]
            KB = [A.alloc([128, TBM]), A.alloc([128, TBM])]
            ysb = A.alloc([128, TBM])
            car = A.alloc([128, 4, 2])
            TWO_PI = 2.0 * math.pi
            for d in range(2):
                DMA("sp", lre, s5_lam_re[d], (), ["lre"])
                DMA("sp", lim, s5_lam_im[d], (), ["lim"])
                DMA("sp", ldt, s5_log_dt[d], (), ["ldt"])
                S = "s5s"
                ACT(ldt, ldt, AF.Exp, ["ldt"], ["ldt"])
                TT("dve", t1, lre, ldt, ALU.mult, ["lre", "ldt"], [S])
                ACT(t1, t1, AF.Exp, [S], [S])
                TT("dve", t2, lim, ldt, ALU.mult, ["lim", "ldt", S], [S])
                TSC("dve", t3, t2, 1.0 / TWO_PI, ALU.mult, [S], [S])
                CPY("dve", ti32, t3, [S], [S])
                CPY("dve", t3, ti32, [S], [S])
                STT("dve", t2, t3, -TWO_PI, t2, ALU.mult, ALU.add, [S], [S])
                TSC("dve", t3, t2, math.pi, ALU.is_gt, [S], [S])
                STT("dve", t2, t3, -TWO_PI, t2, ALU.mult, ALU.add, [S], [S])
                TSC("dve", t3, t2, -math.pi, ALU.is_lt, [S], [S])
                STT("dve", t2, t3, TWO_PI, t2, ALU.mult, ALU.add, [S], [S])
                ACT(t4, t2, AF.Sin, [S], [S])
                TSC("dve", t2, t2, math.pi / 2, ALU.add, [S], [S])
                TSC("dve", t3, t2, math.pi, ALU.is_gt, [S], [S])
                STT("dve", t2, t3, -TWO_PI, t2, ALU.mult, ALU.add, [S], [S])
                ACT(t3, t2, AF.Sin, [S], [S])
                TT("dve", pwr[:, 0, :], t1, t3, ALU.mult, [S], [S])
                TT("dve", pwi[:, 0, :], t1, t4, ALU.mult, [S], [S])
                for lv in range(NLV):
                    TT("dve", t1, pwr[:, lv, :], pwr[:, lv, :], ALU.mult, [S], [S])
                    TT("dve", t2, pwi[:, lv, :], pwi[:, lv, :], ALU.mult, [S], [S])
                    TT("dve", pwr[:, lv + 1, :], t1, t2, ALU.subtract, [S], [S])
                    TT("dve", t1, pwr[:, lv, :], pwi[:, lv, :], ALU.mult, [S], [S])
                    TSC("dve", pwi[:, lv + 1, :], t1, 2.0, ALU.mult, [S], [S])
                TSC("dve", npwi, pwi, -1.0, ALU.mult, [S], [S])
                TSC("dve", t1, pwr[:, 0, :], -1.0, ALU.add, [S], [S])
                TT("dve", t2, lre, lre, ALU.mult, [S, "lre"], [S])
                TT("dve", t3, lim, lim, ALU.mult, [S, "lim"], [S])
                TT("dve", t2, t2, t3, ALU.add, [S], [S])
                RECIP(t2, t2, [S], [S])
                TT("dve", t3, t1, lre, ALU.mult, [S], [S])
                TT("dve", t4, pwi[:, 0, :], lim, ALU.mult, [S], [S])
                TT("dve", t3, t3, t4, ALU.add, [S], [S])
                TT("dve", cre, t3, t2, ALU.mult, [S], [S])
                TT("dve", t3, pwi[:, 0, :], lre, ALU.mult, [S], [S])
                TT("dve", t4, t1, lim, ALU.mult, [S], [S])
                TT("dve", t3, t3, t4, ALU.subtract, [S], [S])
                TT("dve", cim, t3, t2, ALU.mult, [S], [S])
                TSC("dve", ncim, cim, -1.0, ALU.mult, [S], [S])
                nu = 0
                for c in range(8):
                    for ti in range(4):
                        it = 4 * c + ti
                        DMA("sp", bwr, s5_bw_re[d, it], (), ["bwr"])
                        DMA("sp", bwi, s5_bw_im[d, it], (), ["bwi"])
                        TSC("dve", mre, bwr, cre[:, it:it + 1], ALU.mult, ["bwr", S], ["mre"])
                        STT("dve", mre, bwi, ncim[:, it:it + 1], mre, ALU.mult, ALU.add, ["bwi", S, "mre"], ["mre"])
                        TSC("dve", mim, bwi, cre[:, it:it + 1], ALU.mult, ["bwi", S], ["mim"])
                        STT("dve", mim, bwr, cim[:, it:it + 1], mim, ALU.mult, ALU.add, ["bwr", S, "mim"], ["mim"])
                        P.op("pe", lambda e: e.transpose(out=PS[0][:, 0:128], in_=mre, identity=ident), ["mre", "ident"], [psk(0)])
                        CPY("act", LBr[:, ti, :], PS[0][:, 0:128], [psk(0)], ["LB"])
                        P.op("pe", lambda e: e.transpose(out=PS[1][:, 0:128], in_=mim, identity=ident), ["mim", "ident"], [psk(1)])
                        CPY("act", LBi[:, ti, :], PS[1][:, 0:128], [psk(1)], ["LB"])
                        DMA("sp", CWr[:, ti, :], s5_cw_re[d, it], (), ["CW"])
                        DMA("sp", CWi[:, ti, :], s5_cw_im[d, it], (), ["CW"])
                    TSC("dve", CWi, CWi, -1.0, ALU.mult, ["CW"], ["CW"])
                    first = True
                    for s in (("c", "l") if "c" in strs else ("l",)):
                        Ts = TS[s]
                        TB = min(TBM, Ts)
                        nlv = int(round(math.log2(TB)))
                        nblk = Ts // TB
                        need_y = (s == "l") or ctx_out
                        order = range(nblk) if d == 0 else range(nblk - 1, -1, -1)
                        for blk in order:
                            t0 = blk * TB
                            uk = nu % 2
                            nu += 1
                            u = ucb[uk]
                            DMA("sp", u[:, :TB], FA[s][c * 128:(c + 1) * 128, t0:t0 + TB], (), [("ucb", uk)])
                            ncb = (TB + 511) // 512
                            for ti in range(4):
                                it = 4 * c + ti
                                for cb in range(ncb):
                                    w = min(512, TB)
                                    cs = slice(cb * 512, cb * 512 + w)
                                    MM(PS[0][:, :w], LBr[:, ti, :], u[:, cs], True, True, ["LB", ("ucb", uk)], [psk(0)])
                                    CPY("act", KA[0][:, cs], PS[0][:, :w], [psk(0)], ["KA0"])
                                    MM(PS[1][:, :w], LBi[:, ti, :], u[:, cs], True, True, ["LB", ("ucb", uk)], [psk(1)])
                                    CPY("act", KA[1][:, cs], PS[1][:, :w], [psk(1)], ["KA1"])
                                ecol = 0 if d == 0 else TB - 1
                                if not first:
                                    ec = slice(ecol, ecol + 1)
                                    STT("dve", KA[0][:, ec], car[:, ti, 0:1], pwr[:, 0, it:it + 1], KA[0][:, ec], ALU.mult, ALU.add, ["car", S, "KA0"], ["KA0"])
                                    STT("dve", KA[0][:, ec], car[:, ti, 1:2], npwi[:, 0, it:it + 1], KA[0][:, ec], ALU.mult, ALU.add, ["car", S, "KA0"], ["KA0"])
                                    STT("dve", KA[1][:, ec], car[:, ti, 1:2], pwr[:, 0, it:it + 1], KA[1][:, ec], ALU.mult, ALU.add, ["car", S, "KA1"], ["KA1"])
                                    STT("dve", KA[1][:, ec], car[:, ti, 0:1], pwi[:, 0, it:it + 1], KA[1][:, ec], ALU.mult, ALU.add, ["car", S, "KA1"], ["KA1"])
                                src, dst = KA, KB
                                sk, dk = ("KA0", "KA1"), ("KB0", "KB1")
                                for lv in range(nlv):
                                    sh = 1 << lv
                                    if d == 0:
                                        S0, S1, SC = slice(0, TB - sh), slice(sh, TB), slice(0, sh)
                                    else:
                                        S0, S1, SC = slice(sh, TB), slice(0, TB - sh), slice(TB - sh, TB)
                                    pr = pwr[:, lv, it:it + 1]; pi_ = pwi[:, lv, it:it + 1]; npi = npwi[:, lv, it:it + 1]
                                    STT("dve", dst[0][:, S1], src[0][:, S0], pr, src[0][:, S1], ALU.mult, ALU.add, [sk[0], S], [dk[0]])
                                    STT("dve", dst[0][:, S1], src[1][:, S0], npi, dst[0][:, S1], ALU.mult, ALU.add, [sk[1], dk[0], S], [dk[0]])
                                    STT("dve", dst[1][:, S1], src[1][:, S0], pr, src[1][:, S1], ALU.mult, ALU.add, [sk[1], S], [dk[1]])
                                    STT("dve", dst[1][:, S1], src[0][:, S0], pi_, dst[1][:, S1], ALU.mult, ALU.add, [sk[0], dk[1], S], [dk[1]])
                                    CPY("act", dst[0][:, SC], src[0][:, SC], [sk[0], dk[0]], [dk[0]])
                                    CPY("act", dst[1][:, SC], src[1][:, SC], [sk[1], dk[1]], [dk[1]])
                                    src, dst = dst, src
                                    sk, dk = dk, sk
                                lcol = TB - 1 if d == 0 else 0
                                CPY("act", car[:, ti, 0:1], src[0][:, lcol:lcol + 1], [sk[0], "car"], ["car"])
                                CPY("act", car[:, ti, 1:2], src[1][:, lcol:lcol + 1], [sk[1], "car"], ["car"])
                                if need_y:
                                    for cb in range(ncb):
                                        w = min(512, TB)
                                        cs = slice(cb * 512, cb * 512 + w)
                                        MM(PS[4 + cb][:, :w], CWr[:, ti, :], src[0][:, cs], ti == 0, False, ["CW", sk[0]], [psk(4 + cb)])
                                        MM(PS[4 + cb][:, :w], CWi[:, ti, :], src[1][:, cs], False, ti == 3, ["CW", sk[1]], [psk(4 + cb)])
                                if src is not KA:
                                    pass
                            if need_y:
                                for cb in range(ncb):
                                    w = min(512, TB)
                                    CPY("act", ysb[:, cb * 512:cb * 512 + w], PS[4 + cb][:, :w], [psk(4 + cb)], ["ysb5"])
                                if d == 0:
                                    DMA("sp", FB[s][c * 128:(c + 1) * 128, t0:t0 + TB], ysb[:, :TB], ["ysb5"], [("fb", s)])
                                else:
                                    DMA("pool", FB[s][c * 128:(c + 1) * 128, t0:t0 + TB], ysb[:, :TB], ["ysb5"], [("fb", s)], accum_op=ALU.add)
                            first = False
                P.barrier()
            A.reset()
            wg = A.alloc([128, 8, 2 * D], BF16)
            dsk = A.alloc([128, 8])
            bg = A.alloc([128, 2 * D])
            wv = s5_w_glu.rearrange("(a p) n -> p a n", p=128)
            for c in range(8):
                DMA("pool", wg[:, c, :], wv[:, c, :], (), ["wg"])
            DMA("sp", dsk, s5_d_pp, (), ["dsk"])
            load_bcast(bg, s5_b_glu, "bg")
            uu = A.alloc([128, 8, 512]); yy = A.alloc([128, 8, 512]); x2 = A.alloc([128, 8, 512])
            gT = [A.alloc([128, 8, 512], BF16) for _ in range(2)]
            ysb = [A.alloc([128, D]) for _ in range(2)]
            tg = A.alloc([128, 512])
            base = A.off
            nb = 0
            ny = 0
            for s in (strs if ctx_out else ["l"]):
                A.off = base
                pm = pm_setup(L, s)
                Ts = TS[s]
                BW = min(512, Ts)
                fav = FA[s].rearrange("(a p) t -> p a t", p=128)
                fbv = FB[s].rearrange("(a p) t -> p a t", p=128)
                for blk in range(Ts // BW):
                    q = nb % 2
                    nb += 1
                    DMA("sp", uu[:, :, :BW], fav[:, :, blk * BW:(blk + 1) * BW], (), ["uu"])
                    DMA("sp", yy[:, :, :BW], fbv[:, :, blk * BW:(blk + 1) * BW], (), ["yy"])
                    for c in range(8):
                        STT("dve", yy[:, c, :BW], uu[:, c, :BW], dsk[:, c:c + 1], yy[:, c, :BW], ALU.mult, ALU.add, ["uu", "yy", "dsk"], ["yy"])
                    gelu_to(yy[:, :, :BW], x2[:, :, :BW], uu[:, :, :BW], gT[q][:, :, :BW], "yy", "x2", "uu", ("gT", q))
                    for ti in range(BW // 128):
                        i = blk * (BW // 128) + ti
                        yk = ny % 2
                        ny += 1
                        for nbk in range(4):
                            b = 2 + nbk
                            for c in range(8):
                                MM(PS[b], gT[q][:, c, ti * 128:(ti + 1) * 128], wg[:, c, nbk * 512:(nbk + 1) * 512], c == 0, c == 7, [("gT", q), "wg"], [psk(b)])
                        for nh in range(2):
                            TT("dve", tg, PS[4 + nh], bg[:, D + nh * 512:D + (nh + 1) * 512], ALU.add, [psk(4 + nh), "bg"], ["tg"])
                            ACT(tg, tg, AF.Sigmoid, ["tg"], ["tg"])
                            ysl = ysb[yk][:, nh * 512:(nh + 1) * 512]
                            TT("dve", ysl, PS[2 + nh], bg[:, nh * 512:(nh + 1) * 512], ALU.add, [psk(2 + nh), "bg"], [("ysb", yk)])
                            TT("dve", ysl, ysl, tg, ALU.mult, [("ysb", yk), "tg"], [("ysb", yk)])
                        post_mixer(pm, s, i, ysb[yk], ("ysb", yk))
            P.barrier()


        def s5_mixer2(L, strs, ctx_out):
            NCS = {s_: TS[s_] // 8 for s_ in strs}
            fag = {s_: FA[s_].rearrange("a t -> (a t)").rearrange("(g p c) -> p g c", g=64, p=128) for s_ in strs}
            fbg = {s_: FB[s_].rearrange("a t -> (a t)").rearrange("(g p c) -> p g c", g=64, p=128) for s_ in strs}
            A.reset()
            A1 = {}; B1 = {}
            for s in strs:
                A1[s] = A.alloc([128, D]); B1[s] = A.alloc([128, D])
                load_bcast(A1[s], modd[SIDX[s], MOD_A1], "modb")
                load_bcast(B1[s], modd[SIDX[s], MOD_SH1], "modb")
            xt = [A.alloc([128, D]) for _ in range(2)]
            junk = A.alloc([128, D])
            ss = A.alloc([128, 2])
            Ht = A.alloc([128, 64, 8, 16])
            Ub = [A.alloc([128, 4, 128]) for _ in range(2)]
            n = 0
            for s in strs:
                NC = NCS[s]
                xv = XS[s].rearrange("(c s) d -> s c d", s=8)
                for j in range((NC + 127) // 128):
                    rows = min(128, NC - j * 128)
                    for s8 in range(8):
                        k = n % 2
                        n += 1
                        kx, kss = ("xt", k), ("ss", k)
                        DMA("sp", xt[k][:rows], xv[s8, j * 128:j * 128 + rows, :], (), [kx])
                        ssk = ss[:, k:k + 1]
                        ACT(junk[:rows], xt[k][:rows], AF.Square, [kx], ["junk", kss], accum=ssk[:rows])
                        TSC("dve", ssk[:rows], ssk[:rows], 1.0 / D, ALU.mult, [kss], [kss], s2=EPS, op1=ALU.add)
                        ACT(ssk[:rows], ssk[:rows], AF.Sqrt, [kss], [kss])
                        RECIP(ssk[:rows], ssk[:rows], [kss], [kss])
                        hv = Ht[:rows, :, s8, :]
                        v3 = lambda ap_: ap_.rearrange("p (g k) -> p g k", g=64)
                        STT("dve", hv, v3(xt[k][:rows]), ssk[:rows, 0:1], v3(A1[s][:rows]), ALU.mult, ALU.mult, [kx, kss, "modb"], ["Ht"])
                        TT("dve", hv, hv, v3(B1[s][:rows]), ALU.add, ["Ht", "modb"], ["Ht"])
                    for g4 in range(16):
                        b = g4 % 2
                        for jj in range(4):
                            g = g4 * 4 + jj
                            P.op("pe", lambda e, g=g, jj=jj, b=b, rows=rows: e.transpose(out=PS[b][:, jj * 128:jj * 128 + rows],
                                 in_=Ht[:rows, g, :, :].rearrange("p s k -> p (s k)"), identity=ident[:rows, :rows]), ["Ht", "ident"], [psk(b)])
                        CPY("act" if b else "dve", Ub[b][:, :, :rows], PS[b].rearrange("p (a b) -> p a b", a=4)[:, :, :rows], [psk(b)], [("Ub", b)])
                        DMA("sp", fag[s][:, g4 * 4:(g4 + 1) * 4, j * 128:j * 128 + rows], Ub[b][:, :, :rows], [("Ub", b)], [("fa", s)])
            P.barrier()
            A.reset()
            NCL = NCS["l"]
            NCC = NCS.get("c", 0)
            NLVMAX = int(round(math.log2(NCL)))
            lre = A.alloc([128, 32]); lim = A.alloc([128, 32]); ldt = A.alloc([128, 32])
            t1 = A.alloc([128, 32]); t2 = A.alloc([128, 32]); t3 = A.alloc([128, 32]); t4 = A.alloc([128, 32])
            ti32 = A.alloc([128, 32], I32)
            cre = [A.alloc([128, 32]) for _ in range(2)]; cim = [A.alloc([128, 32]) for _ in range(2)]; ncim = [A.alloc([128, 32]) for _ in range(2)]
            Pp_r = A.alloc([128, 9, 32]); Pp_i = A.alloc([128, 9, 32])
            Pn_r = A.alloc([128, 8, 32]); Pn_i = A.alloc([128, 8, 32])
            Tn_r = [A.alloc([128, 32, 8]) for _ in range(2)]; Tn_i = [A.alloc([128, 32, 8]) for _ in range(2)]
            Tp_r = [A.alloc([128, 32, 8]) for _ in range(2)]; Tp_i = [A.alloc([128, 32, 8]) for _ in range(2)]
            L7r = [A.alloc([128, 32]) for _ in range(2)]; L7i = [A.alloc([128, 32]) for _ in range(2)]; nL7i = [A.alloc([128, 32]) for _ in range(2)]
            L1r = [A.alloc([128, 32]) for _ in range(2)]; L1i = [A.alloc([128, 32]) for _ in range(2)]
            nL1r = [A.alloc([128, 32]) for _ in range(2)]; nL1i = [A.alloc([128, 32]) for _ in range(2)]
            Qr = [A.alloc([128, NLVMAX + 1, 32]) for _ in range(2)]; Qi = [A.alloc([128, NLVMAX + 1, 32]) for _ in range(2)]; nQi = [A.alloc([128, NLVMAX + 1, 32]) for _ in range(2)]
            maskd = [A.alloc([128, 128]) for _ in range(2)]
            drep = A.alloc([128, 64])
            DMA("sp", drep, s5_d_rep, (), ["drep"])
            TWO_PI = 2.0 * math.pi
            S = "s5s"
            for d in range(2):
                MSET("pool", maskd[d], 1.0, [("mask", d)])
                if d == 0:
                    P.op("pool", lambda e, d=d: e.affine_select(out=maskd[d], in_=maskd[d], pattern=[[16, 8], [0, 16]], compare_op=ALU.is_ge, fill=0.0, base=15, channel_multiplier=-1), [("mask", d)], [("mask", d)])
                else:
                    P.op("pool", lambda e, d=d: e.affine_select(out=maskd[d], in_=maskd[d], pattern=[[-16, 8], [0, 16]], compare_op=ALU.is_ge, fill=0.0, base=0, channel_multiplier=1), [("mask", d)], [("mask", d)])
                DMA("sp", lre, s5_lam_re[d], [S], ["lre"])
                DMA("sp", lim, s5_lam_im[d], [S], ["lim"])
                DMA("sp", ldt, s5_log_dt[d], [S], ["ldt"])
                ACT(ldt, ldt, AF.Exp, ["ldt"], ["ldt"])
                TT("dve", t1, lre, ldt, ALU.mult, ["lre", "ldt"], [S])
                ACT(t1, t1, AF.Exp, [S], [S])
                TT("dve", t2, lim, ldt, ALU.mult, ["lim", "ldt", S], [S])
                TSC("dve", t3, t2, 1.0 / TWO_PI, ALU.mult, [S], [S])
                CPY("dve", ti32, t3, [S], [S])
                CPY("dve", t3, ti32, [S], [S])
                STT("dve", t2, t3, -TWO_PI, t2, ALU.mult, ALU.add, [S], [S])
                TSC("dve", t3, t2, math.pi, ALU.is_gt, [S], [S])
                STT("dve", t2, t3, -TWO_PI, t2, ALU.mult, ALU.add, [S], [S])
                TSC("dve", t3, t2, -math.pi, ALU.is_lt, [S], [S])
                STT("dve", t2, t3, TWO_PI, t2, ALU.mult, ALU.add, [S], [S])
                ACT(t4, t2, AF.Sin, [S], [S])
                TSC("dve", t2, t2, math.pi / 2, ALU.add, [S], [S])
                TSC("dve", t3, t2, math.pi, ALU.is_gt, [S], [S])
                STT("dve", t2, t3, -TWO_PI, t2, ALU.mult, ALU.add, [S], [S])
                ACT(t3, t2, AF.Sin, [S], [S])
                MSET("dve", Pp_r[:, 0, :], 1.0, [S]); MSET("dve", Pp_i[:, 0, :], 0.0, [S])
                TT("dve", Pp_r[:, 1, :], t1, t3, ALU.mult, [S], [S])
                TT("dve", Pp_i[:, 1, :], t1, t4, ALU.mult, [S], [S])

                def cmul(or_, oi_, ar, ai, br, bi):
                    TT("dve", t1, ar, br, ALU.mult, [S], [S])
                    TT("dve", t2, ai, bi, ALU.mult, [S], [S])
                    TT("dve", t3, ar, bi, ALU.mult, [S], [S])
                    TT("dve", t4, ai, br, ALU.mult, [S], [S])
                    TT("dve", or_, t1, t2, ALU.subtract, [S], [S])
                    TT("dve", oi_, t3, t4, ALU.add, [S], [S])

                for jx in range(2, 9):
                    cmul(Pp_r[:, jx, :], Pp_i[:, jx, :], Pp_r[:, jx - 1, :], Pp_i[:, jx - 1, :], Pp_r[:, 1, :], Pp_i[:, 1, :])
                MSET("dve", Pn_r[:, 0, :], 1.0, [S]); MSET("dve", Pn_i[:, 0, :], 0.0, [S])
                TT("dve", t1, Pp_r[:, 1, :], Pp_r[:, 1, :], ALU.mult, [S], [S])
                TT("dve", t2, Pp_i[:, 1, :], Pp_i[:, 1, :], ALU.mult, [S], [S])
                TT("dve", t1, t1, t2, ALU.add, [S], [S])
                RECIP(t1, t1, [S], [S])
                TT("dve", Pn_r[:, 1, :], Pp_r[:, 1, :], t1, ALU.mult, [S], [S])
                STT("dve", Pn_i[:, 1, :], Pp_i[:, 1, :], -1.0, t1, ALU.mult, ALU.mult, [S], [S])
                for jx in range(2, 8):
                    cmul(Pn_r[:, jx, :], Pn_i[:, jx, :], Pn_r[:, jx - 1, :], Pn_i[:, jx - 1, :], Pn_r[:, 1, :], Pn_i[:, 1, :])
                for jx in range(8):
                    jj = jx if d == 0 else 7 - jx
                    CPY("dve", Tn_r[d][:, :, jx], Pn_r[:, jj, :], [S], [S]); CPY("dve", Tn_i[d][:, :, jx], Pn_i[:, jj, :], [S], [S])
                    CPY("dve", Tp_r[d][:, :, jx], Pp_r[:, jj, :], [S], [S]); CPY("dve", Tp_i[d][:, :, jx], Pp_i[:, jj, :], [S], [S])
                CPY("dve", L7r[d], Pp_r[:, 7, :], [S], [S]); CPY("dve", L7i[d], Pp_i[:, 7, :], [S], [S])
                TSC("dve", nL7i[d], Pp_i[:, 7, :], -1.0, ALU.mult, [S], [S])
                CPY("dve", L1r[d], Pp_r[:, 1, :], [S], [S]); CPY("dve", L1i[d], Pp_i[:, 1, :], [S], [S])
                TSC("dve", nL1r[d], Pp_r[:, 1, :], -1.0, ALU.mult, [S], [S]); TSC("dve", nL1i[d], Pp_i[:, 1, :], -1.0, ALU.mult, [S], [S])
                CPY("dve", Qr[d][:, 0, :], Pp_r[:, 8, :], [S], [S]); CPY("dve", Qi[d][:, 0, :], Pp_i[:, 8, :], [S], [S])
                for lv in range(NLVMAX):
                    TT("dve", t1, Qr[d][:, lv, :], Qr[d][:, lv, :], ALU.mult, [S], [S])
                    TT("dve", t2, Qi[d][:, lv, :], Qi[d][:, lv, :], ALU.mult, [S], [S])
                    TT("dve", Qr[d][:, lv + 1, :], t1, t2, ALU.subtract, [S], [S])
                    TT("dve", t1, Qr[d][:, lv, :], Qi[d][:, lv, :], ALU.mult, [S], [S])
                    TSC("dve", Qi[d][:, lv + 1, :], t1, 2.0, ALU.mult, [S], [S])
                TSC("dve", nQi[d], Qi[d], -1.0, ALU.mult, [S], [S])
                TSC("dve", t1, Pp_r[:, 1, :], -1.0, ALU.add, [S], [S])
                TT("dve", t2, lre, lre, ALU.mult, [S, "lre"], [S])
                TT("dve", t3, lim, lim, ALU.mult, [S, "lim"], [S])
                TT("dve", t2, t2, t3, ALU.add, [S], [S])
                RECIP(t2, t2, [S], [S])
                TT("dve", t3, t1, lre, ALU.mult, [S], [S])
                TT("dve", t4, Pp_i[:, 1, :], lim, ALU.mult, [S], [S])
                TT("dve", t3, t3, t4, ALU.add, [S], [S])
                TT("dve", cre[d], t3, t2, ALU.mult, [S], [S])
                TT("dve", t3, Pp_i[:, 1, :], lre, ALU.mult, [S], [S])
                TT("dve", t4, t1, lim, ALU.mult, [S], [S])
                TT("dve", t3, t3, t4, ALU.subtract, [S], [S])
                TT("dve", cim[d], t3, t2, ALU.mult, [S], [S])
                TSC("dve", ncim[d], cim[d], -1.0, ALU.mult, [S, "lre", "lim"], [S])
            OPN = ("TzA", "TzB", "WZAr", "WZAi", "WZBr", "WZBi", "WYAr", "WYAi", "WYBr", "WYBi")
            OPS = [[{nm: A.alloc([128, 128]) for nm in OPN} for _ in range(4)] for _ in range(2)]
            SB = {}
            for s in strs:
                SB[s] = [[[A.alloc([128, NCS[s] + 1]) for _ in range(2)] for _ in range(4)] for _ in range(2)]
            bn = [A.alloc([128, 16]) for _ in range(2)]; cn = [A.alloc([128, 16]) for _ in range(2)]
            Bb = [A.alloc([128, 16]) for _ in range(2)]
            VBr = A.alloc([128, 8, 16]); VBi = A.alloc([128, 8, 16]); nVBi = A.alloc([128, 8, 16])
            VCr = A.alloc([128, 8, 16]); VCi = A.alloc([128, 8, 16])
            VCm = [A.alloc([128, 128]) for _ in range(4)]
            wt = [A.alloc([128, 128]) for _ in range(4)]
            Ug = [A.alloc([128, NCL]) for _ in range(4)]
            KPAD = NCL // 2
            KA = [A.alloc([128, NCL + KPAD]), A.alloc([128, NCL + KPAD])]
            KB = [A.alloc([128, NCL + KPAD]), A.alloc([128, NCL + KPAD])]
            car = A.alloc([128, 2, 4, 2])
            yt = [A.alloc([128, 512]) for _ in range(2)]
            gx2 = A.alloc([128, 512]); gtm = A.alloc([128, 512])
            go = [A.alloc([128, 512]) for _ in range(2)]
            f2 = lambda ap_: ap_.rearrange("p a b -> p (a b)")
            nug = 0
            ngo = 0
            for c8 in range(8):
                for d in range(2):
                    for ti in range(4):
                        it = 4 * c8 + ti
                        O = OPS[d][ti]
                        ko = ("ops", d, ti)
                        DMA("sp", bn[0], s5_bn_re[d, it], (), ["bn"]); DMA("sp", bn[1], s5_bn_im[d, it], (), ["bn"])
                        DMA("sp", cn[0], s5_cn_re[d, it], (), ["cn"]); DMA("sp", cn[1], s5_cn_im[d, it], (), ["cn"])
                        W_ = "s5w"
                        TSC("dve", Bb[0], bn[0], cre[d][:, it:it + 1], ALU.mult, ["bn", S], [W_])
                        STT("dve", Bb[0], bn[1], ncim[d][:, it:it + 1], Bb[0], ALU.mult, ALU.add, ["bn", S, W_], [W_])
                        TSC("dve", Bb[1], bn[1], cre[d][:, it:it + 1], ALU.mult, ["bn", S, W_], [W_])
                        STT("dve", Bb[1], bn[0], cim[d][:, it:it + 1], Bb[1], ALU.mult, ALU.add, ["bn", S, W_], [W_])
                        bc_k = lambda ap_: ap_.rearrange("p (o k) -> p o k", o=1).to_broadcast([128, 8, 16])
                        bc_s = lambda ap_: ap_.rearrange("p (s o) -> p s o", o=1).to_broadcast([128, 8, 16])
                        tA = wt[0].rearrange("p (a b) -> p a b", a=8); tB = wt[1].rearrange("p (a b) -> p a b", a=8)
                        TT("dve", VBr, bc_k(Bb[0]), bc_s(Tn_r[d][:, it, :]), ALU.mult, [W_, S], [W_])
                        TT("dve", tA, bc_k(Bb[1]), bc_s(Tn_i[d][:, it, :]), ALU.mult, [W_, S], [W_])
                        TT("dve", VBr, VBr, tA, ALU.subtract, [W_], [W_])
                        TT("dve", VBi, bc_k(Bb[0]), bc_s(Tn_i[d][:, it, :]), ALU.mult, [W_, S], [W_])
                        TT("dve", tA, bc_k(Bb[1]), bc_s(Tn_r[d][:, it, :]), ALU.mult, [W_, S], [W_])
                        TT("dve", VBi, VBi, tA, ALU.add, [W_], [W_])
                        TSC("dve", nVBi, VBi, -1.0, ALU.mult, [W_], [W_])
                        TT("dve", VCr, bc_k(cn[0]), bc_s(Tp_r[d][:, it, :]), ALU.mult, ["cn", W_, S], [W_])
                        TT("dve", tA, bc_k(cn[1]), bc_s(Tp_i[d][:, it, :]), ALU.mult, ["cn", W_, S], [W_])
                        TT("dve", VCr, VCr, tA, ALU.subtract, [W_], [W_])
                        TT("dve", VCi, bc_k(cn[0]), bc_s(Tp_i[d][:, it, :]), ALU.mult, ["cn", W_, S], [W_])
                        TT("dve", tA, bc_k(cn[1]), bc_s(Tp_r[d][:, it, :]), ALU.mult, ["cn", W_, S], [W_])
                        TT("dve", VCi, VCi, tA, ALU.add, [W_], [W_])
                        TSC("dve", VCm[0], f2(VCr), rmA[:, 0:1], ALU.mult, [W_, "rm"], [W_])
                        TSC("dve", VCm[1], f2(VCi), rmA[:, 0:1], ALU.mult, [W_, "rm"], [W_])
                        TSC("dve", VCm[2], f2(VCr), rmB[:, 0:1], ALU.mult, [W_, "rm"], [W_])
                        TSC("dve", VCm[3], f2(VCi), rmB[:, 0:1], ALU.mult, [W_, "rm"], [W_])
                        for gi, nm in ((0, "TzA"), (1, "TzB")):
                            MM(PS[0][:, 0:128], f2(VBr), VCm[2 * gi], True, False, [W_], [psk(0)])
                            MM(PS[0][:, 0:128], f2(nVBi), VCm[2 * gi + 1], False, True, [W_], [psk(0)])
                            TT("dve", O[nm], PS[0][:, 0:128], maskd[d], ALU.mult, [psk(0), ("mask", d)], [ko])
                        TSC("dve", wt[2], f2(VBr), L7r[d][:, it:it + 1], ALU.mult, [W_, S], [W_])
                        STT("dve", wt[2], f2(VBi), nL7i[d][:, it:it + 1], wt[2], ALU.mult, ALU.add, [W_, S], [W_])
                        TSC("dve", wt[3], f2(VBr), L7i[d][:, it:it + 1], ALU.mult, [W_, S], [W_])
                        STT("dve", wt[3], f2(VBi), L7r[d][:, it:it + 1], wt[3], ALU.mult, ALU.add, [W_, S], [W_])
                        for q_, (na, nb_) in ((2, ("WZAr", "WZBr")), (3, ("WZAi", "WZBi"))):
                            P.op("pe", lambda e, q_=q_: e.transpose(out=PS[1][:, 0:128], in_=wt[q_], identity=ident), [W_, "ident"], [psk(1)])
                            MSET("pool", O[na][:, 64:128], 0.0, [ko]); MSET("pool", O[nb_][:, 0:64], 0.0, [ko])
                            CPY("act", O[na][:, 0:64], PS[1][:, 0:64], [psk(1)], [ko])
                            CPY("act", O[nb_][:, 64:128], PS[1][:, 64:128], [psk(1)], [ko])
                        TSC("dve", wt[0], f2(VCr), L1r[d][:, it:it + 1], ALU.mult, [W_, S], [W_])
                        STT("dve", wt[0], f2(VCi), nL1i[d][:, it:it + 1], wt[0], ALU.mult, ALU.add, [W_, S], [W_])
                        TSC("dve", wt[1], f2(VCr), nL1i[d][:, it:it + 1], ALU.mult, [W_, S], [W_])
                        STT("dve", wt[1], f2(VCi), nL1r[d][:, it:it + 1], wt[1], ALU.mult, ALU.add, [W_, S], [W_])
                        TSC("dve", O["WYAr"], wt[0], rmA[:, 0:1], ALU.mult, [W_, "rm"], [ko]); TSC("dve", O["WYBr"], wt[0], rmB[:, 0:1], ALU.mult, [W_, "rm"], [ko])
                        TSC("dve", O["WYAi"], wt[1], rmA[:, 0:1], ALU.mult, [W_, "rm"], [ko]); TSC("dve", O["WYBi"], wt[1], rmB[:, 0:1], ALU.mult, [W_, "rm"], [ko])
                for si, s in enumerate(("c", "l") if "c" in strs else ("l",)):
                    NC = NCS[s]
                    nlv = int(round(math.log2(NC)))
                    first = (si == 0)
                    for ti in range(4):
                        it = 4 * c8 + ti
                        ua = Ug[nug % 4]; ka_ = ("Ug", nug % 4); nug += 1
                        ub_ = Ug[nug % 4]; kb_ = ("Ug", nug % 4); nug += 1
                        DMA("sp", ua[:, :NC], fag[s][:, 2 * it, :], (), [ka_])
                        DMA("sp", ub_[:, :NC], fag[s][:, 2 * it + 1, :], (), [kb_])
                        for d in range(2):
                            O = OPS[d][ti]
                            ko = ("ops", d, ti)
                            msh = max(NC // 2, 1)
                            D0 = KPAD if d == 0 else 0
                            PZ = slice(KPAD - msh, KPAD) if d == 0 else slice(NC, NC + msh)
                            for bi_, (buf, kk) in enumerate(((KA[0], "KA0"), (KA[1], "KA1"), (KB[0], "KB0"), (KB[1], "KB1"))):
                                MSET("pool", buf[:, PZ], 0.0, [kk])
                            for cb in range((NC + 511) // 512):
                                w = min(512, NC - cb * 512)
                                cs = slice(cb * 512, cb * 512 + w)
                                ds_ = slice(D0 + cb * 512, D0 + cb * 512 + w)
                                MM(PS[2][:, :w], O["WZAr"], ua[:, cs], True, False, [ko, ka_], [psk(2)])
                                MM(PS[2][:, :w], O["WZBr"], ub_[:, cs], False, True, [ko, kb_], [psk(2)])
                                CPY("act", KA[0][:, ds_], PS[2][:, :w], [psk(2)], ["KA0"])
                                MM(PS[3][:, :w], O["WZAi"], ua[:, cs], True, False, [ko, ka_], [psk(3)])
                                MM(PS[3][:, :w], O["WZBi"], ub_[:, cs], False, True, [ko, kb_], [psk(3)])
                                CPY("act", KA[1][:, ds_], PS[3][:, :w], [psk(3)], ["KA1"])
                            ecol = D0 if d == 0 else D0 + NC - 1
                            cr_ = car[:, d, ti, 0:1]; ci_ = car[:, d, ti, 1:2]
                            kc = ("car", d, ti)
                            if not first:
                                ec = slice(ecol, ecol + 1)
                                STT("dve", KA[0][:, ec], cr_, Qr[d][:, 0, it:it + 1], KA[0][:, ec], ALU.mult, ALU.add, [kc, S, "KA0"], ["KA0"])
                                STT("dve", KA[0][:, ec], ci_, nQi[d][:, 0, it:it + 1], KA[0][:, ec], ALU.mult, ALU.add, [kc, S, "KA0"], ["KA0"])
                                STT("dve", KA[1][:, ec], ci_, Qr[d][:, 0, it:it + 1], KA[1][:, ec], ALU.mult, ALU.add, [kc, S, "KA1"], ["KA1"])
                                STT("dve", KA[1][:, ec], cr_, Qi[d][:, 0, it:it + 1], KA[1][:, ec], ALU.mult, ALU.add, [kc, S, "KA1"], ["KA1"])
                            src, dst = KA, KB
                            sk, dk = ("KA0", "KA1"), ("KB0", "KB1")
                            S1 = slice(D0, D0 + NC)
                            for lv in range(nlv):
                                sh = 1 << lv
                                S0 = slice(D0 - sh, D0 - sh + NC) if d == 0 else slice(D0 + sh, D0 + sh + NC)
                                pr = Qr[d][:, lv, it:it + 1]; pi_ = Qi[d][:, lv, it:it + 1]; npi = nQi[d][:, lv, it:it + 1]
                                STT("dve", dst[0][:, S1], src[0][:, S0], pr, src[0][:, S1], ALU.mult, ALU.add, [sk[0], S], [dk[0]])
                                STT("dve", dst[1][:, S1], src[1][:, S0], pr, src[1][:, S1], ALU.mult, ALU.add, [sk[1], S], [dk[1]])
                                STT("dve", dst[0][:, S1], src[1][:, S0], npi, dst[0][:, S1], ALU.mult, ALU.add, [sk[1], dk[0], S], [dk[0]])
                                STT("dve", dst[1][:, S1], src[0][:, S0], pi_, dst[1][:, S1], ALU.mult, ALU.add, [sk[0], dk[1], S], [dk[1]])
                                src, dst = dst, src
                                sk, dk = dk, sk
                            sbr, sbi = SB[s][d][ti]
                            ksb = ("SB", s, d, ti)
                            off = 1 if d == 0 else 0
                            icol = 0 if d == 0 else NC
                            CPY("act", sbr[:, off:off + NC], src[0][:, D0:D0 + NC], [sk[0]], [ksb])
                            CPY("act", sbi[:, off:off + NC], src[1][:, D0:D0 + NC], [sk[1]], [ksb])
                            if first:
                                MSET("pool", sbr[:, icol:icol + 1], 0.0, [ksb]); MSET("pool", sbi[:, icol:icol + 1], 0.0, [ksb])
                            else:
                                CPY("act", sbr[:, icol:icol + 1], cr_, [kc], [ksb]); CPY("act", sbi[:, icol:icol + 1], ci_, [kc], [ksb])
                            lcol = D0 + NC - 1 if d == 0 else D0
                            CPY("act", cr_, src[0][:, lcol:lcol + 1], [sk[0], ksb], [kc])
                            CPY("act", ci_, src[1][:, lcol:lcol + 1], [sk[1], ksb], [kc])
                    if s == "l" or ctx_out:
                        for gl in range(8):
                            g = 8 * c8 + gl
                            ti = gl // 2
                            AB = "A" if gl % 2 == 0 else "B"
                            ug_ = Ug[nug % 4]; ku = ("Ug", nug % 4); nug += 1
                            DMA("sp", ug_[:, :NC], fag[s][:, g, :], (), [ku])
                            for cb in range((NC + 511) // 512):
                                w = min(512, NC - cb * 512)
                                c0 = cb * 512
                                pb_ = 4 + (ngo % 2)
                                first_mm = True
                                for d in range(2):
                                    O = OPS[d][ti]
                                    ko = ("ops", d, ti)
                                    sbr, sbi = SB[s][d][ti]
                                    ksb = ("SB", s, d, ti)
                                    so = c0 if d == 0 else c0 + 1
                                    MM(PS[pb_][:, :w], O["Tz" + AB], ug_[:, c0:c0 + w], first_mm, False, [ko, ku], [psk(pb_)])
                                    first_mm = False
                                    MM(PS[pb_][:, :w], O["WY" + AB + "r"], sbr[:, so:so + w], False, False, [ko, ksb], [psk(pb_)])
                                    MM(PS[pb_][:, :w], O["WY" + AB + "i"], sbi[:, so:so + w], False, d == 1, [ko, ksb], [psk(pb_)])
                                q = ngo % 2
                                ngo += 1
                                STT("dve", yt[q][:, :w], ug_[:, c0:c0 + w], drep[:, g:g + 1], PS[pb_][:, :w], ALU.mult, ALU.add, [ku, "drep", psk(pb_)], [("yt", q)])
                                gelu_to(yt[q][:, :w], gx2[:, :w], gtm[:, :w], go[q][:, :w], ("yt", q), "gx2", "gtm", ("go", q))
                                DMA("sp", fbg[s][:, g, c0:c0 + w], go[q][:, :w], [("go", q)], [("fb", s)])
            P.barrier()
            A.reset()
            wg = A.alloc([128, 8, 2 * D], BF16)
            bg = A.alloc([128, 2 * D])
            wv = s5_w_glu.rearrange("(a p) n -> p a n", p=128)
            for c in range(8):
                DMA("pool", wg[:, c, :], wv[:, c, :], (), ["wg"])
            load_bcast(bg, s5_b_glu, "bg")
            Gb = A.alloc([128, 64, 128])
            Gtok = A.alloc([128, 8, D], BF16)
            gT = [A.alloc([128, 8, 128], BF16) for _ in range(2)]
            ysb = [A.alloc([128, D]) for _ in range(2)]
            tg = A.alloc([128, 512])
            base = A.off
            ny = 0
            ngt = 0
            for s in (strs if ctx_out else ["l"]):
                A.off = base
                pm = pm_setup(L, s)
                NC = NCS[s]
                xv = XS[s].rearrange("(c s) d -> s c d", s=8)
                mv = MB[s].rearrange("(c s) d -> s c d", s=8)
                if s == "c":
                    MSET("dve", AFFC5, -1.0, [("aff", s)])
                for j in range((NC + 127) // 128):
                    rows = min(128, NC - j * 128)
                    DMA("sp", Gb[:, :, :rows], fbg[s][:, :, j * 128:j * 128 + rows], (), ["Gb"])
                    for g4 in range(16):
                        b = g4 % 2
                        for jj in range(4):
                            g = g4 * 4 + jj
                            P.op("pe", lambda e, g=g, jj=jj, b=b, rows=rows: e.transpose(out=PS[b][:rows, jj * 128:(jj + 1) * 128],
                                 in_=Gb[:, g, :rows], identity=ident), ["Gb", "ident"], [psk(b)])
                        for jj in range(4):
                            g = g4 * 4 + jj
                            CPY("act" if jj % 2 else "dve", Gtok[:rows, :, 16 * g:16 * g + 16],
                                PS[b][:rows, jj * 128:(jj + 1) * 128].rearrange("p (t k) -> p t k", t=8), [psk(b)], ["Gtok"])
                    for tau in range(8):
                        q = ngt % 2
                        ngt += 1
                        for g2_ in range(2):
                            b = g2_
                            pb16 = PS[b].bitcast(BF16)
                            for jj in range(4):
                                c = g2_ * 4 + jj
                                P.op("pe", lambda e, c=c, jj=jj, pb16=pb16, rows=rows, tau=tau: e.transpose(out=pb16[:, jj * 128:jj * 128 + rows],
                                     in_=Gtok[:rows, tau, c * 128:(c + 1) * 128], identity=identb[:rows, :rows]), ["Gtok", "identb"], [psk(b)])
                            CPY("act" if g2_ else "dve", gT[q][:, g2_ * 4:g2_ * 4 + 4, :rows],
                                pb16[:, 0:512].rearrange("p (a b) -> p a b", a=4)[:, :, :rows], [psk(b)], [("gT", q)])
                        yk = ny % 2
                        ny += 1
                        for nbk in range(4):
                            b = 2 + nbk
                            for c in range(8):
                                MM(PS[b][:rows, :], gT[q][:, c, :rows], wg[:, c, nbk * 512:(nbk + 1) * 512], c == 0, c == 7, [("gT", q), "wg"], [psk(b)])
                        for nh in range(2):
                            TT("dve", tg[:rows], PS[4 + nh][:rows, :], bg[:rows, D + nh * 512:D + (nh + 1) * 512], ALU.add, [psk(4 + nh), "bg"], ["tg"])
                            ACT(tg[:rows], tg[:rows], AF.Sigmoid, ["tg"], ["tg"])
                            ysl = ysb[yk][:rows, nh * 512:(nh + 1) * 512]
                            TT("dve", ysl, PS[2 + nh][:rows, :], bg[:rows, nh * 512:(nh + 1) * 512], ALU.add, [psk(2 + nh), "bg"], [("ysb", yk)])
                            TT("dve", ysl, ysl, tg[:rows], ALU.mult, [("ysb", yk), "tg"], [("ysb", yk)])
                        if s == "l":
                            affcol = AFF["l"][:, :, 8 * j + tau]
                        else:
                            affcol = AFFC5[:, :, tau]
                        post_mixer(pm, s, ("s5", j, tau), ysb[yk], ("ysb", yk),
                                   xrows=xv[tau, j * 128:j * 128 + rows, :], mrows=mv[tau, j * 128:j * 128 + rows, :], rr=rows, affcol=affcol)
            RCFG["l"] = (AFF["l"], NT, tokid_l5)
            if ctx_out and "c" in strs:
                RCFG["c"] = (AFFC5, 8, tokid_c5)
            P.barrier()

        def gelu_to(y, x2, tmp, outp, ky, kx2, ktmp, kout):
            ACT(x2, y, AF.Square, [ky], [kx2])
            TSC("dve", x2, x2, 0.044715, ALU.mult, [kx2], [kx2], s2=1.0, op1=ALU.add)
            TT("dve", x2, x2, y, ALU.mult, [kx2, ky], [kx2])
            ACT(tmp, x2, AF.Sigmoid, [kx2], [ktmp], scale=1.5957691216057308)
            TT("dve", outp, tmp, y, ALU.mult, [ktmp, ky], [kout])

        def lru_mixer(L, strs, ctx_out):
            A.reset()
            wx = A.alloc([128, 8, D], BF16); wy = A.alloc([128, 8, D], BF16)
            bx = A.alloc([128, 8]); by = A.alloc([128, 8])
            for nm, wt, src_w in (("wx", wx, lru_w_x), ("wy", wy, lru_w_y)):
                wv = src_w.rearrange("(a p) n -> p a n", p=128)
                for c in range(8):
                    DMA("pool", wt[:, c, :], wv[:, c, :], (), [nm])
            DMA("sp", bx, lru_b_x_pp, (), ["bx"])
            DMA("sp", by, lru_b_y_pp, (), ["by"])
            A1 = {}; B1 = {}
            for s in strs:
                A1[s] = A.alloc([128, D]); B1[s] = A.alloc([128, D])
                load_bcast(A1[s], modd[SIDX[s], MOD_A1], "modb")
                load_bcast(B1[s], modd[SIDX[s], MOD_SH1], "modb")
            xt = [A.alloc([128, D]) for _ in range(2)]
            h = [A.alloc([128, D]) for _ in range(2)]
            ss = A.alloc([128, 2])
            hT = [A.alloc([128, 8, 512], BF16) for _ in range(2)]
            xo = [A.alloc([128, 512]) for _ in range(2)]
            go = [A.alloc([128, 512]) for _ in range(2)]
            gx2 = A.alloc([128, 512]); gtm = A.alloc([128, 512]); gy = A.alloc([128, 512])
            n = 0
            nb = 0
            for s in strs:
                Ts = TS[s]
                BW = min(512, Ts)
                for blk in range(Ts // BW):
                    hb = hT[nb % 2]
                    for ti in range(BW // 128):
                        i = blk * (BW // 128) + ti
                        k = n % 2
                        n += 1
                        DMA("sp", xt[k], XS[s][i * 128:(i + 1) * 128, :], (), [("xt", k)])
                        norm_mod(xt[k], A1[s], B1[s], h[k], ss[:, k:k + 1], ("xt", k), ("h", k), ("ss", k))
                        transposes_to(h[k], 128, lambda c0, nn, hb=hb, ti=ti: hb[:, c0:c0 + nn, ti * 128:(ti + 1) * 128],
                                      ("h", k), ("hT", nb % 2), evac="act", pb=(0, 1))
                    for oc in range(8):
                        q = oc % 2
                        pa, pg = 2 + q * 2, 3 + q * 2
                        for c in range(8):
                            MM(PS[pa][:, :BW], wx[:, c, oc * 128:(oc + 1) * 128], hb[:, c, :BW], c == 0, c == 7, ["wx", ("hT", nb % 2)], [psk(pa)])
                        ACT(xo[q][:, :BW], PS[pa][:, :BW], AF.Identity, [psk(pa), "bx"], [("xo", q)], bias=bx[:, oc:oc + 1])
                        DMA("sp", FA[s][oc * 128:(oc + 1) * 128, blk * BW:(blk + 1) * BW], xo[q][:, :BW], [("xo", q)], [("fa", s)])
                        if s == "l":
                            for c in range(8):
                                MM(PS[pg][:, :BW], wy[:, c, oc * 128:(oc + 1) * 128], hb[:, c, :BW], c == 0, c == 7, ["wy", ("hT", nb % 2)], [psk(pg)])
                            ACT(gy[:, :BW], PS[pg][:, :BW], AF.Identity, [psk(pg), "by"], ["gy"], bias=by[:, oc:oc + 1])
                            gelu_to(gy[:, :BW], gx2[:, :BW], gtm[:, :BW], go[q][:, :BW], "gy", "gx2", "gtm", ("go", q))
                            DMA("sp", FC[s][oc * 128:(oc + 1) * 128, blk * BW:(blk + 1) * BW], go[q][:, :BW], [("go", q)], [("fc", s)])
                    nb += 1
            P.barrier()
            A.reset()
            TBM = 2048
            cw = A.alloc([128, 8, 4]); cb_ = A.alloc([128, 8])
            ba = A.alloc([128, 2, 8]); bi = A.alloc([128, 2, 8]); nsp = A.alloc([128, 2, 8])
            wa = A.alloc([128, 128]); wi = A.alloc([128, 128])
            DMA("sp", cw, lru_conv_w_pp, (), ["cw"])
            DMA("sp", cb_, lru_conv_b_pp, (), ["cw"])
            for d in range(2):
                DMA("sp", ba[:, d, :], lru_b_a_pp[d], (), ["ba"])
                DMA("sp", bi[:, d, :], lru_b_i_pp[d], (), ["bi"])
                DMA("sp", nsp[:, d, :], lru_lam_pp[d], (), ["nsp"])
            ACT(nsp, nsp, AF.Exp, ["nsp"], ["nsp"], scale=-1.0)
            ACT(nsp, nsp, AF.Ln, ["nsp"], ["nsp"], bias=1.0)
            TSC("dve", nsp, nsp, -8.0, ALU.mult, ["nsp"], ["nsp"])
            xb = [A.alloc([128, TBM + 3]) for _ in range(2)]
            xc2 = [A.alloc([128, TBM]) for _ in range(2)]
            av2 = [A.alloc([128, TBM]) for _ in range(2)]; bv2 = [A.alloc([128, TBM]) for _ in range(2)]; ig2 = [A.alloc([128, TBM]) for _ in range(2)]
            hb2 = [A.alloc([128, TBM]) for _ in range(2)]
            car = A.alloc([128, 1])
            nx = 0
            nh2 = 0
            for d in range(2):
                for c in range(8):
                    DMA("sp", wa, lru_w_a[d, c], (), ["wa"])
                    DMA("sp", wi, lru_w_i[d, c], (), ["wi"])
                    first = True
                    for s in (("c", "l") if "c" in strs else ("l",)):
                        Ts = TS[s]
                        TB = min(TBM, Ts)
                        nblk = Ts // TB
                        need_y = (s == "l") or ctx_out
                        order = range(nblk) if d == 0 else range(nblk - 1, -1, -1)
                        for blk in order:
                            t0 = blk * TB
                            k = nx % 2
                            nx += 1
                            xbk = xb[k]
                            kx = ("xb", k)
                            xc, av, bv, ig = xc2[k], av2[k], bv2[k], ig2[k]
                            kxc, kav, kbv, kig = ("xc", k), ("av", k), ("bv", k), ("ig", k)
                            rowsl = slice(c * 128, (c + 1) * 128)
                            if blk == 0:
                                MSET("dve", xbk[:, 0:2], 0.0, [kx])
                            else:
                                DMA("sp", xbk[:, 0:2], FA[s][rowsl, t0 - 2:t0], (), [kx])
                            if blk == nblk - 1:
                                MSET("dve", xbk[:, 2 + TB:3 + TB], 0.0, [kx])
                            else:
                                DMA("sp", xbk[:, 2 + TB:3 + TB], FA[s][rowsl, t0 + TB:t0 + TB + 1], (), [kx], allow_slow_non_contiguous=True)
                            DMA("sp", xbk[:, 2:2 + TB], FA[s][rowsl, t0:t0 + TB], (), [kx])
                            TSC("dve", xc[:, :TB], xbk[:, 0:TB], cw[:, c, 0:1], ALU.mult, [kx, "cw"], [kxc], s2=cb_[:, c:c + 1], op1=ALU.add)
                            for jj in range(1, 4):
                                STT("dve", xc[:, :TB], xbk[:, jj:jj + TB], cw[:, c, jj:jj + 1], xc[:, :TB], ALU.mult, ALU.add, [kx, kxc, "cw"], [kxc])
                            ncb = (TB + 511) // 512
                            for cbk in range(ncb):
                                w = min(512, TB)
                                cs = slice(cbk * 512, cbk * 512 + w)
                                pa, pg = 2 + (cbk % 2) * 2, 3 + (cbk % 2) * 2
                                MM(PS[pa][:, :w], wa, xc[:, cs], True, True, ["wa", kxc], [psk(pa)])
                                MM(PS[pg][:, :w], wi, xc[:, cs], True, True, ["wi", kxc], [psk(pg)])
                                ACT(av[:, cs], PS[pa][:, :w], AF.Sigmoid, [psk(pa), "ba"], [kav], bias=ba[:, d, c:c + 1])
                                ACT(ig[:, cs], PS[pg][:, :w], AF.Sigmoid, [psk(pg), "bi"], [kig], bias=bi[:, d, c:c + 1])
                            ACT(av[:, :TB], av[:, :TB], AF.Exp, [kav, "nsp"], [kav], scale=nsp[:, d, c:c + 1])
                            TT("dve", bv[:, :TB], av[:, :TB], av[:, :TB], ALU.mult, [kav], [kbv])
                            ACT(bv[:, :TB], bv[:, :TB], AF.Sqrt, [kbv], [kbv], bias=1.0, scale=-1.0)
                            TT("dve", ig[:, :TB], ig[:, :TB], xc[:, :TB], ALU.mult, [kig, kxc], [kig])
                            TT("dve", bv[:, :TB], bv[:, :TB], ig[:, :TB], ALU.mult, [kbv, kig], [kbv])
                            hk = nh2 % 2
                            nh2 += 1
                            hh = hb2[hk]
                            init = 0.0 if first else car[:, 0:1]
                            if d == 0:
                                P.op("dve", lambda e, hh=hh, init=init, TB=TB, av=av, bv=bv: e.tensor_tensor_scan(out=hh[:, :TB], data0=av[:, :TB], data1=bv[:, :TB], initial=init, op0=ALU.mult, op1=ALU.add),
                                     [kav, kbv, "car"], [("hb2", hk)])
                                lcol = TB - 1
                            else:
                                P.op("dve", lambda e, hh=hh, init=init, TB=TB, av=av, bv=bv: e.tensor_tensor_scan(out=hh[:, TB - 1::-1] if False else hh[:, :TB][:, ::-1], data0=av[:, :TB][:, ::-1], data1=bv[:, :TB][:, ::-1], initial=init, op0=ALU.mult, op1=ALU.add),
                                     [kav, kbv, "car"], [("hb2", hk)])
                                lcol = 0
                            CPY("dve", car[:, 0:1], hh[:, lcol:lcol + 1], [("hb2", hk), "car"], ["car"])
                            first = False
                            if need_y:
                                if d == 0:
                                    DMA("sp", FB[s][rowsl, t0:t0 + TB], hh[:, :TB], [("hb2", hk)], [("fb", s)])
                                else:
                                    DMA("pool", FB[s][rowsl, t0:t0 + TB], hh[:, :TB], [("hb2", hk)], [("fb", s)], accum_op=ALU.add)
                P.barrier()
            A.reset()
            wo = A.alloc([128, 8, D], BF16)
            bout = A.alloc([128, D])
            wv = lru_w_out.rearrange("(a p) n -> p a n", p=128)
            for c in range(8):
                DMA("pool", wo[:, c, :], wv[:, c, :], (), ["wo"])
            load_bcast(bout, lru_b_out, "bout")
            rr = A.alloc([128, 8, 512]); gg = A.alloc([128, 8, 512])
            zT = [A.alloc([128, 8, 512], BF16) for _ in range(2)]
            ysb = [A.alloc([128, D]) for _ in range(2)]
            base = A.off
            nb = 0
            ny = 0
            for s in (strs if ctx_out else ["l"]):
                A.off = base
                pm = pm_setup(L, s)
                Ts = TS[s]
                BW = min(512, Ts)
                fbv = FB[s].rearrange("(a p) t -> p a t", p=128)
                fcv = FC[s].rearrange("(a p) t -> p a t", p=128)
                for blk in range(Ts // BW):
                    q = nb % 2
                    nb += 1
                    DMA("sp", rr[:, :, :BW], fbv[:, :, blk * BW:(blk + 1) * BW], (), ["rr"])
                    DMA("sp", gg[:, :, :BW], fcv[:, :, blk * BW:(blk + 1) * BW], (), ["gg"])
                    TT("dve", zT[q][:, :, :BW], rr[:, :, :BW], gg[:, :, :BW], ALU.mult, ["rr", "gg"], [("zT", q)])
                    for ti in range(BW // 128):
                        i = blk * (BW // 128) + ti
                        yk = ny % 2
                        ny += 1
                        for nh in range(2):
                            b = 4 + nh
                            for c in range(8):
                                MM(PS[b], zT[q][:, c, ti * 128:(ti + 1) * 128], wo[:, c, nh * 512:(nh + 1) * 512], c == 0, c == 7, [("zT", q), "wo"], [psk(b)])
                            TT("dve", ysb[yk][:, nh * 512:(nh + 1) * 512], PS[b], bout[:, nh * 512:(nh + 1) * 512], ALU.add, [psk(b), "bout"], [("ysb", yk)])
                        post_mixer(pm, s, i, ysb[yk], ("ysb", yk))
            P.barrier()

        def moe(L, strs):
            def route(s):
                A.reset()
                cap = CAPS[s]
                aff, nt, tokid = RCFG.get(s, (AFF[s], NTS[s], tokid_std))
                lo = A.alloc([128, NE]); hi = A.alloc([128, NE]); mid = A.alloc([128, NE]); tq = A.alloc([128, NE])
                ge = A.alloc([128, NE], I32); nge = A.alloc([128, NE], I32)
                cnt = A.alloc([128, NE])
                cmp_ = A.alloc([128, NE, nt])
                R = "rt"
                MSET("dve", lo, 0.0, [R]); MSET("dve", hi, 1.0, [R]); MSET("dve", mid, 0.5, [R])
                for it in range(34):
                    TT("dve", cmp_, aff, mid.rearrange("p (e o) -> p e o", o=1).to_broadcast([128, NE, nt]), ALU.is_gt, [R, ("aff", s)], [R])
                    P.op("dve", lambda e: e.reduce_sum(out=cnt, in_=cmp_, axis=AX.X), [R], [R])
                    MM(PS[0][:, 0:NE], ones, cnt, True, True, ["ones", R], [psk(0)])
                    TSC("dve", ge, PS[0][:, 0:NE], cap - 0.5, ALU.is_gt, [psk(0), R], [R])
                    TSC("dve", nge, PS[0][:, 0:NE], cap - 0.5, ALU.is_lt, [psk(0), R], [R])
                    P.op("dve", lambda e: e.copy_predicated(lo, ge, mid), [R], [R])
                    P.op("dve", lambda e: e.copy_predicated(hi, nge, mid), [R], [R])
                    TSC("dve", tq, lo, 0.5, ALU.mult, [R], [R])
                    STT("dve", mid, hi, 0.5, tq, ALU.mult, ALU.add, [R], [R])
                sel = cmp_
                TT("dve", sel, aff, lo.rearrange("p (e o) -> p e o", o=1).to_broadcast([128, NE, nt]), ALU.is_gt, [R, ("aff", s)], [R])
                onesb = A.alloc([128, NE * nt]); csum = A.alloc([128, NE, nt])
                base_ = A.alloc([128, NE]); tot = A.alloc([128, NE]); adj = A.alloc([128, NE])
                posf = A.alloc([128, NE, nt]); posi = A.alloc([128, NE, nt], I32)
                pair = A.alloc([128, NE, nt, 2])
                pair_i = pair.bitcast(I32)
                posi2 = posi.rearrange("p e t -> p (e t)")
                pair2 = pair_i.rearrange("p e t c -> p (e t c)")
                MSET("dve", onesb, 1.0, [R])
                P.op("dve", lambda e: e.tensor_tensor_scan(out=csum.rearrange("p e t -> p (e t)"), data0=onesb, data1=sel.rearrange("p e t -> p (e t)"), initial=0.0, op0=ALU.mult, op1=ALU.add), [R], [R])
                MSET("dve", base_[:, 0:1], 0.0, [R])
                CPY("dve", base_[:, 1:NE], csum[:, 0:NE - 1, nt - 1], [R], [R])
                TT("dve", tot, csum[:, :, nt - 1], base_, ALU.subtract, [R], [R])
                MM(PS[1][:, 0:NE], Umat, tot, True, True, ["Umat", R], [psk(1)])
                TT("dve", adj, PS[1][:, 0:NE], base_, ALU.subtract, [psk(1), R], [R])
                TSC("dve", adj, adj, -1.0, ALU.add, [R], [R])
                TT("dve", posf, csum, adj.rearrange("p (e o) -> p e o", o=1).to_broadcast([128, NE, nt]), ALU.add, [R], [R])
                STT("dve", posf, posf, -BIG, sel, ALU.add, ALU.mult, [R], [R])
                TSC("dve", posf, posf, BIG, ALU.add, [R], [R])
                CPY("dve", posi, posf, [R], [R])
                CPY("dve", pair_i[:, :, :, 0], tokid[:, 0:nt].rearrange("p (o t) -> p o t", o=1).to_broadcast([128, NE, nt]), [R, "tokid"], [R])
                CPY("dve", pair[:, :, :, 1], aff, [R, ("aff", s)], [R])
                for e_ in range(NE):
                    for jt in range(nt):
                        P.dma("pool", lambda e, e_=e_, jt=jt: e.indirect_dma_start(
                            out=LST[s][e_][:, :], out_offset=bass.IndirectOffsetOnAxis(ap=posi2[:, e_ * nt + jt:e_ * nt + jt + 1], axis=0),
                            in_=pair2[:, 2 * (e_ * nt + jt):2 * (e_ * nt + jt) + 2], in_offset=None, bounds_check=getreg(e, cap - 1), oob_is_err=False),
                            [R], [("lst", s, e_, jt)])
                P.barrier()
            for s in strs:
                route(s)
            A.reset()
            NSLOT = 6
            W = [A.alloc([128, 8, D], BF16) for _ in range(NSLOT)]
            has_c = "c" in strs
            NCOL = CAP + (CAPC if has_c else 0)
            xs = A.alloc([128, 8, D], BF16)
            xsc = A.alloc([128, D], BF16)
            xsT = A.alloc([128, 8, NCOL], BF16)
            hT = A.alloc([128, 8, NCOL], BF16)
            ysb = [A.alloc([128, D]) for _ in range(2)]
            sa = [A.alloc([128, 512]) for _ in range(2)]
            G2 = {}
            lt = {}
            for s in strs:
                G2[s] = A.alloc([128, D])
                load_bcast(G2[s], modd[SIDX[s], MOD_G2], ("g2", s))
                lt[s] = [A.alloc([128, 8, 2], I32) for _ in range(2)]
            wsrc = (moe_w1, moe_w3, moe_w2)
            tiles = [("l", jt, min(CAP, 128), jt * 128) for jt in range((CAP + 127) // 128)]
            if has_c:
                tiles.append(("c", 0, CAPC, CAP))

            def xs_of(s, jt):
                return xs[:, jt, :] if s == "l" else xsc

            def load_w(e_):
                for m in range(3):
                    slot = (3 * e_ + m) % NSLOT
                    wv = wsrc[m][L, e_].rearrange("(a p) n -> p a n", p=128)
                    for c in range(0, 8, 4):
                        DMA("pool", W[slot][:, c:c + 4, :], wv[:, c:c + 4, :], (), [("W", slot)])

            def gathers(e_):
                for s in strs:
                    cap = CAPS[s]
                    rows = min(cap, 128)
                    ntile = (cap + 127) // 128
                    DMA("sp", lt[s][e_ % 2][:rows, :ntile, :], LST[s][e_].rearrange("(j p) c -> p j c", p=rows), (), [("lt", s, e_ % 2)])
                for (s, jt, rows, c0) in tiles:
                    P.dma("pool", lambda e, s=s, jt=jt, rows=rows, e_=e_: e.indirect_dma_start(
                        out=xs_of(s, jt)[:rows, :], out_offset=None, in_=MB[s],
                        in_offset=bass.IndirectOffsetOnAxis(ap=lt[s][e_ % 2][:rows, jt, 0:1], axis=0),
                        bounds_check=getreg(e, TS[s] - 1), oob_is_err=False),
                        [("lt", s, e_ % 2)], ["INDIRECT", ("xs", s, jt)])

            load_w(0)
            gathers(0)
            load_w(1)
            ny = 0
            nsa = 0
            ntr = 0
            for e_ in range(NE):
                w1, w3, w2 = (3 * e_) % NSLOT, (3 * e_ + 1) % NSLOT, (3 * e_ + 2) % NSLOT
                for (s, jt, rows, c0) in tiles:
                    src = xs_of(s, jt)
                    for g in range(2):
                        b = ntr % 2
                        ntr += 1
                        pb16 = PS[b].bitcast(BF16)
                        for j in range(4):
                            c = g * 4 + j
                            P.op("pe", lambda e, c=c, j=j, pb16=pb16, src=src, rows=rows: e.transpose(
                                out=pb16[:, j * 128:j * 128 + rows], in_=src[:rows, c * 128:(c + 1) * 128], identity=identb[:rows, :rows]),
                                [("xs", s, jt), "identb"], [psk(b)])
                        CPY("act" if ntr % 2 else "dve", xsT[:, g * 4:g * 4 + 4, c0:c0 + rows],
                            pb16[:, 0:512].rearrange("p (a b) -> p a b", a=4)[:, :, :rows], [psk(b)], ["xsT"])
                if e_ + 1 < NE:
                    gathers(e_ + 1)
                for fc in range(8):
                    for hb_ in range((NCOL + 511) // 512):
                        w = min(512, NCOL - hb_ * 512)
                        cs = slice(hb_ * 512, hb_ * 512 + w)
                        q = nsa % 2
                        nsa += 1
                        pa, pb_ = 2 + q * 2, 3 + q * 2
                        for c in range(8):
                            MM(PS[pa][:, :w], W[w1][:, c, fc * 128:(fc + 1) * 128], xsT[:, c, cs], c == 0, c == 7, [("W", w1), "xsT"], [psk(pa)])
                        for c in range(8):
                            MM(PS[pb_][:, :w], W[w3][:, c, fc * 128:(fc + 1) * 128], xsT[:, c, cs], c == 0, c == 7, [("W", w3), "xsT"], [psk(pb_)])
                        ACT(sa[q][:, :w], PS[pa][:, :w], AF.Silu, [psk(pa)], [("sa", q)])
                        TT("dve", hT[:, fc, cs], sa[q][:, :w], PS[pb_][:, :w], ALU.mult, [("sa", q), psk(pb_)], ["hT"])
                for (s, jt, rows, c0) in tiles:
                    yk = ny % 2
                    ny += 1
                    ltf = lt[s][e_ % 2].bitcast(F32)
                    for nh in range(2):
                        b = 6 + nh
                        for fc in range(8):
                            MM(PS[b][:rows, :], hT[:, fc, c0:c0 + rows], W[w2][:, fc, nh * 512:(nh + 1) * 512], fc == 0, fc == 7, ["hT", ("W", w2)], [psk(b)])
                        STT("dve", ysb[yk][:rows, nh * 512:(nh + 1) * 512], PS[b][:rows, :], ltf[:rows, jt, 1:2], G2[s][:rows, nh * 512:(nh + 1) * 512],
                            ALU.mult, ALU.mult, [psk(b), ("lt", s, e_ % 2), ("g2", s)], [("ysbm", yk)])
                    P.dma("pool", lambda e, s=s, jt=jt, rows=rows, yk=yk, e_=e_: e.indirect_dma_start(
                        out=XS[s], out_offset=bass.IndirectOffsetOnAxis(ap=lt[s][e_ % 2][:rows, jt, 0:1], axis=0),
                        in_=ysb[yk][:rows, :], in_offset=None, compute_op=ALU.add, bounds_check=getreg(e, TS[s] - 1), oob_is_err=True),
                        [("ysbm", yk), ("lt", s, e_ % 2)], ["INDIRECT", ("xsd", s)])
                if e_ + 2 < NE:
                    load_w(e_ + 2)
            P.barrier()


        A.reset()
        xt = [A.alloc([128, D]) for _ in range(2)]
        om = A.alloc([128, 256]); posc = A.alloc([128, 512])
        prow = [A.alloc([128, 512]) for _ in range(2)]
        arg = A.alloc([128, 256]); tq_ = A.alloc([128, 256]); tqi = A.alloc([128, 256], I32)
        pf = A.alloc([128, 1]); phi = A.alloc([128, 1]); colv = A.alloc([128, 1]); rowv = A.alloc([128, 2])
        TWO_PI0 = 2.0 * math.pi
        G0 = "st0"
        P.op("pool", lambda e: e.iota(om, pattern=[[1, 256]], base=0, channel_multiplier=0, allow_small_or_imprecise_dtypes=True), (), [G0])
        P.op("pool", lambda e: e.iota(pf, pattern=[[0, 1]], base=0, channel_multiplier=1, allow_small_or_imprecise_dtypes=True), (), [G0])
        ACT(om, om, AF.Exp, [G0], [G0], scale=-math.log(10000.0) / 256.0)
        TSC("dve", phi, pf, 63.5, ALU.is_gt, [G0], [G0])
        STT("dve", colv, phi, -64.0, pf, ALU.mult, ALU.add, [G0], [G0])

        def sincos(dst, scal, kd):
            TSC("dve", arg, om, scal, ALU.mult, [G0, ("rowv", 0), ("rowv", 1)], [G0])
            TSC("dve", tq_, arg, 1.0 / TWO_PI0, ALU.mult, [G0], [G0])
            CPY("dve", tqi, tq_, [G0], [G0])
            CPY("dve", tq_, tqi, [G0], [G0])
            STT("dve", arg, tq_, -TWO_PI0, arg, ALU.mult, ALU.add, [G0], [G0])
            TSC("dve", tq_, arg, math.pi, ALU.is_gt, [G0], [G0])
            STT("dve", arg, tq_, -TWO_PI0, arg, ALU.mult, ALU.add, [G0], [G0])
            TSC("dve", tq_, arg, -math.pi, ALU.is_lt, [G0], [G0])
            STT("dve", arg, tq_, TWO_PI0, arg, ALU.mult, ALU.add, [G0], [G0])
            ACT(dst[:, 0:256], arg, AF.Sin, [G0], [kd])
            TSC("dve", arg, arg, math.pi / 2, ALU.add, [G0, kd], [G0])
            TSC("dve", tq_, arg, math.pi, ALU.is_gt, [G0], [G0])
            STT("dve", arg, tq_, -TWO_PI0, arg, ALU.mult, ALU.add, [G0], [G0])
            ACT(dst[:, 256:512], arg, AF.Sin, [G0], [kd])

        sincos(posc, colv[:, 0:1], "posc")
        for i in range(NT):
            k = i % 2
            DMA("sp", xt[k], x_in[i * 128:(i + 1) * 128, :], (), [("xt", k)])
            TSC("dve", rowv[:, k:k + 1], phi, float(2 * i), ALU.add, [G0], [("rowv", k)])
            sincos(prow[k], rowv[:, k:k + 1], ("prow", k))
            TT("dve", xt[k][:, 0:512], xt[k][:, 0:512], prow[k], ALU.add, [("xt", k), ("prow", k)], [("xt", k)])
            TT("dve", xt[k][:, 512:1024], xt[k][:, 512:1024], posc, ALU.add, [("xt", k), "posc"], [("xt", k)])
            DMA("sp", XS["l"][i * 128:(i + 1) * 128, :], xt[k], [("xt", k)], [("xs", "l", i)])
        for i in range(NTC):
            k = i % 2
            DMA("sp", xt[k], ctx_in[i * 128:(i + 1) * 128, :], (), [("xt", k)])
            DMA("sp", XS["c"][i * 128:(i + 1) * 128, :], xt[k], [("xt", k)], [("xs", "c", i)])
        P.barrier()

        for L in range(nlayers):
            kind = L % 3
            j = L // 3
            ctx_inL = L <= 2
            ctx_out = L < 2
            strs = ["l"] + (["c"] if ctx_inL else [])
            modulation(L)
            if kind == 0:
                conv_mixer(L, j, ["l"] + (["c"] if ctx_out else []))
            elif kind == 1:
                s5_mixer2(L, strs, ctx_out)
            else:
                lru_mixer(L, strs, ctx_out)
            moe(L, ["l"] + (["c"] if ctx_out else []))
            RCFG.clear()

        A.reset()
        fg = A.alloc([128, D])
        load_bcast(fg, final_g, "modb")
        xt = [A.alloc([128, D]) for _ in range(2)]
        ho = [A.alloc([128, D]) for _ in range(2)]
        ss = A.alloc([128, 2])
        for i in range(NT):
            k = i % 2
            DMA("sp", xt[k], XS["l"][i * 128:(i + 1) * 128, :], (), [("xt", k)])
            norm_mod(xt[k], fg, None, ho[k], ss[:, k:k + 1], ("xt", k), ("h", k), ("ss", k))
            DMA("sp", out[i * 128:(i + 1) * 128, :], ho[k], [("h", k)], [("out", i)])
        P.barrier()
        P.build()
    return nc, P


def _pp(v):
    v = np.asarray(v)
    return np.ascontiguousarray(v.reshape(-1, 128).T)


def _grid_pos(n, d):
    rows = n // 64
    quarter = d // 4
    omega = (1.0 / (10000.0 ** (np.arange(quarter, dtype=np.float32) / np.float32(quarter)))).astype(np.float32)
    r = np.arange(rows, dtype=np.float32)[:, None] * omega
    cc = np.arange(64, dtype=np.float32)[:, None] * omega
    row_emb = np.concatenate([np.sin(r), np.cos(r)], axis=-1)
    col_emb = np.concatenate([np.sin(cc), np.cos(cc)], axis=-1)
    emb = np.concatenate([np.broadcast_to(row_emb[:, None, :], (rows, 64, d // 2)),
                          np.broadcast_to(col_emb[None, :, :], (rows, 64, d // 2))], axis=-1)
    return np.ascontiguousarray(emb.reshape(rows * 64, d).astype(np.float32))


def prepare_shared(inp):
    f = lambda k: np.ascontiguousarray(np.asarray(inp[k], dtype=np.float32))
    sh = {}
    for k in ("w_mod", "b_mod", "g_mix", "g_ffn", "final_g", "moe_router", "moe_w1", "moe_w3", "moe_w2",
              "conv_w_in", "conv_w_out", "conv_b_out", "lru_w_y", "lru_w_x", "lru_w_out", "lru_b_out"):
        a = f(k)
        if k.startswith("lru_") :
            a = a[0]
        sh[k] = np.ascontiguousarray(a)
    na = f("conv_b_in").shape[0]
    def pad2(a):
        if a.shape[0] == 2:
            return a
        return np.ascontiguousarray(np.concatenate([a, a], axis=0)[:2])
    sh["conv_w_in"] = pad2(sh["conv_w_in"]); sh["conv_w_out"] = pad2(sh["conv_w_out"]); sh["conv_b_out"] = pad2(sh["conv_b_out"])
    sh["conv_b_in_pp"] = pad2(np.stack([_pp(v) for v in f("conv_b_in")]))
    sh["conv_dw_pp"] = pad2(np.stack([np.ascontiguousarray(w.T.reshape(8, 128, 31).transpose(1, 0, 2)) for w in f("conv_dw")]))
    sh["conv_dw_b_pp"] = pad2(np.stack([_pp(v) for v in f("conv_dw_b")]))
    sh["conv_ln_g_pp"] = pad2(np.stack([_pp(v) for v in f("conv_ln_g")]))
    sh["conv_ln_b_pp"] = pad2(np.stack([_pp(v) for v in f("conv_ln_b")]))
    lam_re = f("s5_lam_re")[0]; lam_im = f("s5_lam_im")[0]; log_dt = f("s5_log_dt")[0]
    sh["s5_lam_re"] = np.stack([_pp(lam_re[d].reshape(-1)) for d in range(2)])
    sh["s5_lam_im"] = np.stack([_pp(lam_im[d].reshape(-1)) for d in range(2)])
    sh["s5_log_dt"] = np.stack([_pp(np.repeat(log_dt[d], 64)) for d in range(2)])
    b_re = f("s5_b_re")[0]; b_im = f("s5_b_im")[0]; c_re = f("s5_c_re")[0]; c_im = f("s5_c_im")[0]
    def blockB(b):
        o = np.zeros((2, 32, 128, 128), np.float32)
        for g in range(64):
            it, gl = g // 2, g % 2
            o[:, it, gl * 64:(gl + 1) * 64, (g % 8) * 16:(g % 8) * 16 + 16] = b[:, g]
        return o
    def blockC(c):
        o = np.zeros((2, 32, 128, 128), np.float32)
        for g in range(64):
            it, gl = g // 2, g % 2
            o[:, it, gl * 64:(gl + 1) * 64, (g % 8) * 16:(g % 8) * 16 + 16] = c[:, g].transpose(0, 2, 1)
        return o
    sh["s5_bw_re"] = blockB(b_re); sh["s5_bw_im"] = blockB(b_im)
    sh["s5_cw_re"] = blockC(c_re); sh["s5_cw_im"] = blockC(c_im)
    sh["s5_d_pp"] = _pp(f("s5_d")[0])
    sh["s5_bn_re"] = np.ascontiguousarray(b_re.reshape(2, 32, 128, 16)); sh["s5_bn_im"] = np.ascontiguousarray(b_im.reshape(2, 32, 128, 16))
    sh["s5_cn_re"] = np.ascontiguousarray(c_re.transpose(0, 1, 3, 2).reshape(2, 32, 128, 16)); sh["s5_cn_im"] = np.ascontiguousarray(c_im.transpose(0, 1, 3, 2).reshape(2, 32, 128, 16))
    sh["s5_d_rep"] = np.ascontiguousarray(np.tile(f("s5_d")[0].reshape(64, 1, 16), (1, 8, 1)).reshape(64, 128).T)
    sh["s5_w_glu"] = f("s5_w_glu")[0]
    sh["s5_b_glu"] = f("s5_b_glu")[0]
    sh["lru_b_y_pp"] = _pp(f("lru_b_y")[0]); sh["lru_b_x_pp"] = _pp(f("lru_b_x")[0])
    sh["lru_conv_w_pp"] = np.ascontiguousarray(f("lru_conv_w")[0].T.reshape(8, 128, 4).transpose(1, 0, 2))
    sh["lru_conv_b_pp"] = _pp(f("lru_conv_b")[0])
    sh["lru_w_a"] = f("lru_w_a")[0]; sh["lru_w_i"] = f("lru_w_i")[0]
    sh["lru_b_a_pp"] = np.stack([_pp(v) for v in f("lru_b_a")[0]])
    sh["lru_b_i_pp"] = np.stack([_pp(v) for v in f("lru_b_i")[0]])
    sh["lru_lam_pp"] = np.stack([_pp(v) for v in f("lru_lam")[0]])
    return sh


def run_module(inp, nlayers=4):
    x = np.asarray(inp["x"], dtype=np.float32)
    B, T, _ = x.shape
    ctx = np.asarray(inp["ctx"], dtype=np.float32)
    TC = ctx.shape[1]
    c = np.asarray(inp["c"], dtype=np.float32)
    c_ctx = np.asarray(inp["c_ctx"], dtype=np.float32)
    sh = prepare_shared(inp)
    nc, P = build_program(T, TC, nlayers)
    in_maps = []
    for b in range(B):
        m = dict(sh)
        m["x"] = np.ascontiguousarray(x[b])
        m["ctx"] = np.ascontiguousarray(ctx[b])
        m["c2T"] = np.ascontiguousarray(np.stack([_pp(c[b]), _pp(c_ctx)], axis=-1))
        in_maps.append(m)
    res = run_bass_kernel_spmd(nc, in_maps, core_ids=list(range(B)))
    return np.stack([np.asarray(r["out"]) for r in res.results], axis=0).astype(np.float32)


def kernel(**inputs):
    return run_module(inputs, 4)
```
